# Optimizing a Trainium2 kernel written in Bass

```python
import math
import jax, jax.numpy as jnp
from jax import lax
import numpy as np


D_MODEL = 1024
BATCH = 8
SEQ = 4096
DEPTH = 1

N_HEADS_SWA = 8
N_KV_HEADS_SWA = 2
N_HEADS_FOX = 8
HEAD_DIM = 64
WINDOW = 128
BLOCK = 128
NUM_BUCKETS = 32
MAX_DISTANCE = 128
N_GROUPS = 4
EXPERTS_PER_GROUP = 8
TOP_K = 2
D_FF_EXPERT = 256
FORGET_BIAS_INIT = 2.0
EPS = 1e-6
NEG_INF = -1e30

Q_A = N_HEADS_SWA * HEAD_DIM
KV_A = N_KV_HEADS_SWA * HEAD_DIM
W_B = N_HEADS_FOX * HEAD_DIM
IN_SPLITS = (Q_A, KV_A, KV_A, W_B, W_B, W_B, N_HEADS_FOX, 2 * D_MODEL)
IN_COLS = Q_A + 2 * KV_A + 3 * W_B + N_HEADS_FOX + 2 * D_MODEL

kernel_name = "hybrid_swa_sink_fox_hmoe_adaln_block"


def _rmsnorm(x, g):
    x32 = x.astype(jnp.float32)
    y = x32 * lax.rsqrt(jnp.mean(x32 * x32, axis=-1, keepdims=True) + EPS)
    return (y * g.astype(jnp.float32)).astype(x.dtype)


def _modulate(h, shift, scale):
    return h * (1 + scale[:, None, :]) + shift[:, None, :]


def _t5_bucket(dist):
    n = jnp.maximum(dist, 0)
    max_exact = NUM_BUCKETS // 2
    nf = jnp.maximum(n, 1).astype(jnp.float32)
    large = max_exact + (jnp.log(nf / max_exact) / math.log(MAX_DISTANCE / max_exact)
                         * (NUM_BUCKETS - max_exact)).astype(jnp.int32)
    large = jnp.minimum(large, NUM_BUCKETS - 1)
    return jnp.where(n < max_exact, n, large)


def _sliding_window_attention(q, k, v, sinks, rel_table):
    b, s, _ = q.shape
    nb = s // BLOCK
    grp = N_HEADS_SWA // N_KV_HEADS_SWA
    qb = q.reshape(b, nb, BLOCK, N_KV_HEADS_SWA, grp, HEAD_DIM)
    k = k.reshape(b, s, N_KV_HEADS_SWA, HEAD_DIM)
    v = v.reshape(b, s, N_KV_HEADS_SWA, HEAD_DIM)
    pad = ((0, 0), (BLOCK, 0), (0, 0), (0, 0))
    kp = jnp.pad(k, pad)
    vp = jnp.pad(v, pad)
    shp = (b, nb, BLOCK, N_KV_HEADS_SWA, HEAD_DIM)
    kb = jnp.concatenate([kp[:, :s].reshape(shp), kp[:, BLOCK:].reshape(shp)], axis=2)
    vb = jnp.concatenate([vp[:, :s].reshape(shp), vp[:, BLOCK:].reshape(shp)], axis=2)
    scores = jnp.einsum('bnqhgd,bnkhd->bnhgqk', qb, kb).astype(jnp.float32) * (HEAD_DIM ** -0.5)
    qi = jnp.arange(BLOCK)[:, None]
    kj = jnp.arange(2 * BLOCK)[None, :]
    dist = qi - kj + BLOCK
    band = (dist >= 0) & (dist < WINDOW)
    key_pos = jnp.arange(nb)[:, None, None] * BLOCK - BLOCK + kj[None]
    valid = band[None] & (key_pos >= 0)
    bias = rel_table[_t5_bucket(dist)].astype(jnp.float32)
    bias = jnp.transpose(bias, (2, 0, 1)).reshape(N_KV_HEADS_SWA, grp, BLOCK, 2 * BLOCK)
    scores = jnp.where(valid[None, :, None, None], scores + bias, NEG_INF)
    sink = jnp.broadcast_to(sinks.astype(jnp.float32).reshape(N_KV_HEADS_SWA, grp, 1, 1),
                            scores.shape[:-1] + (1,))
    probs = jax.nn.softmax(jnp.concatenate([scores, sink], axis=-1), axis=-1)[..., :-1]
    out = jnp.einsum('bnhgqk,bnkhd->bnqhgd', probs.astype(v.dtype), vb)
    return out.reshape(b, s, Q_A)


def _forgetting_attention(q, k, v, f_logit, b_forget):
    b, s, _ = q.shape
    nb = s // BLOCK
    q = q.reshape(b, s, N_HEADS_FOX, HEAD_DIM)
    k = k.reshape(b, s, N_HEADS_FOX, HEAD_DIM)
    v = v.reshape(b, s, N_HEADS_FOX, HEAD_DIM)
    log_f = jax.nn.log_sigmoid(f_logit.astype(jnp.float32) + b_forget.astype(jnp.float32))
    cum = jnp.cumsum(log_f, axis=1)
    cum_k = jnp.transpose(cum, (0, 2, 1))[:, :, None, :]
    qb = q.reshape(b, nb, BLOCK, N_HEADS_FOX, HEAD_DIM).transpose(1, 0, 2, 3, 4)
    cqb = cum.reshape(b, nb, BLOCK, N_HEADS_FOX).transpose(1, 0, 3, 2)
    kpos = jnp.arange(s)
    scale = HEAD_DIM ** -0.5

    def block(args):
        idx, qblk, cq = args
        sc = jnp.einsum('bqhd,bkhd->bhqk', qblk, k).astype(jnp.float32) * scale
        sc = sc + cq[..., None] - cum_k
        qpos = idx * BLOCK + jnp.arange(BLOCK)
        causal = kpos[None, :] <= qpos[:, None]
        p = jax.nn.softmax(jnp.where(causal, sc, NEG_INF), axis=-1)
        return jnp.einsum('bhqk,bkhd->bqhd', p.astype(v.dtype), v)

    out = lax.map(block, (jnp.arange(nb), qb, cqb))
    return out.transpose(1, 0, 2, 3, 4).reshape(b, s, W_B)


def _hierarchical_moe(h, w_rg, b_rg, w_re, b_re, w_g, w_u, w_d):
    bsz, s, d = h.shape
    t = h.reshape(-1, d)
    g_logits = (t @ w_rg + b_rg).astype(jnp.float32)
    g_prob = jax.nn.softmax(g_logits, axis=-1)
    gp, gi = lax.top_k(g_prob, 1)
    e_logits = (jnp.einsum('nd,gde->nge', t, w_re) + b_re).astype(jnp.float32)
    e_sel = jnp.take_along_axis(e_logits, gi[:, :, None], axis=1)[:, 0]
    ev, ei = lax.top_k(e_sel, TOP_K)
    ew = jax.nn.softmax(ev, axis=-1) * gp
    within = jnp.sum(jax.nn.one_hot(ei, EXPERTS_PER_GROUP, dtype=jnp.float32) * ew[..., None], axis=1)
    combine = jax.nn.one_hot(gi[:, 0], N_GROUPS, dtype=jnp.float32)[:, :, None] * within[:, None, :]
    y = jnp.zeros_like(t)
    for g in range(N_GROUPS):
        a = jnp.einsum('nd,edf->nef', t, w_g[g])
        u = jnp.einsum('nd,edf->nef', t, w_u[g])
        hid = jax.nn.silu(a) * u * combine[:, g, :, None].astype(t.dtype)
        y = y + jnp.einsum('nef,efd->nd', hid, w_d[g])
    return y.reshape(bsz, s, d)


def setup_inputs(seed: int = 0) -> dict:
    key = jax.random.key(seed)
    ks = jax.random.split(key, 22)

    def nrm(k, shape, scale):
        return jax.random.normal(k, shape, jnp.float32) * scale

    D, G, E, F = D_MODEL, N_GROUPS, EXPERTS_PER_GROUP, D_FF_EXPERT
    return {
        "x": nrm(ks[0], (BATCH, SEQ, D), 1.0),
        "c": nrm(ks[1], (BATCH, D), 1.0),
        "w_ada": nrm(ks[2], (DEPTH, D, 6 * D), 0.5 * D ** -0.5),
        "b_ada": nrm(ks[3], (DEPTH, 6 * D), 0.02),
        "g_norm_mix": 1.0 + nrm(ks[4], (DEPTH, D), 0.05),
        "g_norm_ffn": 1.0 + nrm(ks[5], (DEPTH, D), 0.05),
        "w_in": nrm(ks[6], (DEPTH, D, IN_COLS), D ** -0.5),
        "sinks": nrm(ks[7], (DEPTH, N_HEADS_SWA), 0.5),
        "b_forget": FORGET_BIAS_INIT + nrm(ks[8], (DEPTH, N_HEADS_FOX), 0.5),
        "w_proj_swa": nrm(ks[9], (DEPTH, Q_A, D), Q_A ** -0.5),
        "w_proj_fox": nrm(ks[10], (DEPTH, W_B, D), W_B ** -0.5),
        "w_out": nrm(ks[11], (DEPTH, D, D), D ** -0.5),
        "rel_bias_table": nrm(ks[12], (NUM_BUCKETS, N_HEADS_SWA), 0.5),
        "w_router_group": nrm(ks[13], (DEPTH, D, G), D ** -0.5),
        "b_router_group": nrm(ks[14], (DEPTH, G), 0.01),
        "w_router_expert": nrm(ks[15], (DEPTH, G, D, E), D ** -0.5),
        "b_router_expert": nrm(ks[16], (DEPTH, G, E), 0.01),
        "w_gate_exp": nrm(ks[17], (DEPTH, G, E, D, F), D ** -0.5),
        "w_up_exp": nrm(ks[18], (DEPTH, G, E, D, F), D ** -0.5),
        "w_down_exp": nrm(ks[19], (DEPTH, G, E, F, D), F ** -0.5),
        "g_final": 1.0 + nrm(ks[20], (D,), 0.05),
    }


def reference(x, c, w_ada, b_ada, g_norm_mix, g_norm_ffn, w_in, sinks, b_forget,
              w_proj_swa, w_proj_fox, w_out, rel_bias_table, w_router_group, b_router_group,
              w_router_expert, b_router_expert, w_gate_exp, w_up_exp, w_down_exp, g_final):
    offsets = []
    acc = 0
    for n in IN_SPLITS[:-1]:
        acc += n
        offsets.append(acc)
    c_act = jax.nn.silu(c)
    for l in range(DEPTH):
        mod = c_act @ w_ada[l] + b_ada[l]
        shift_m, scale_m, gate_m, shift_f, scale_f, gate_f = jnp.split(mod, 6, axis=-1)

        h = _modulate(_rmsnorm(x, g_norm_mix[l]), shift_m, scale_m)
        proj = h @ w_in[l]
        qa, ka, va, qb, kb, vb, f_logit, gates = jnp.split(proj, offsets, axis=-1)
        o_a = _sliding_window_attention(qa, ka, va, sinks[l], rel_bias_table)
        o_b = _forgetting_attention(qb, kb, vb, f_logit, b_forget[l])
        gate_a = jax.nn.sigmoid(gates[..., :D_MODEL])
        gate_b = jax.nn.sigmoid(gates[..., D_MODEL:])
        merged = gate_a * (o_a @ w_proj_swa[l]) + gate_b * (o_b @ w_proj_fox[l])
        x = x + gate_m[:, None, :] * (merged @ w_out[l])

        h2 = _modulate(_rmsnorm(x, g_norm_ffn[l]), shift_f, scale_f)
        y = _hierarchical_moe(h2, w_router_group[l], b_router_group[l], w_router_expert[l],
                              b_router_expert[l], w_gate_exp[l], w_up_exp[l], w_down_exp[l])
        x = x + gate_f[:, None, :] * y
    return _rmsnorm(x, g_final)
```

```python
import contextlib
import math
import numpy as np
import concourse.bass as bass
import concourse.mybir as mybir
from concourse.bass_utils import run_bass_kernel_spmd

F32 = mybir.dt.float32
BF16 = mybir.dt.bfloat16
I32 = mybir.dt.int32
AF = mybir.ActivationFunctionType
ALU = mybir.AluOpType
AX = mybir.AxisListType

D = 1024
EPS = 1e-6
NEG = -1e30


class Op:
    __slots__ = ("eng", "fn", "deps", "flag", "semval", "dma", "sem")

    def __init__(self, eng, fn, dma):
        self.eng = eng
        self.fn = fn
        self.deps = []
        self.flag = False
        self.semval = 0
        self.dma = dma
        self.sem = None


class Sched:
    ENGS = ["sync", "scalar", "vector", "gpsimd", "tensor"]

    def __init__(self, nc, n_dma_sems=48):
        self.nc = nc
        self.streams = {e: [] for e in self.ENGS}
        self.res = {}
        self.n_dma_sems = n_dma_sems
        self.dma_count = 0
        self.dma_count_sw = 0
        self.dma_last = [None] * n_dma_sems
        self.dma_uses = [0] * n_dma_sems
        self.nbar = 0

    def op(self, eng, fn, r=(), w=(), dma=False, extra=()):
        o = Op(eng, fn, dma)
        deps = set(extra)
        for k in r:
            st = self.res.get(k)
            if st is None:
                st = self.res[k] = [None, []]
            if st[0] is not None:
                deps.add(st[0])
        for k in w:
            st = self.res.get(k)
            if st is None:
                st = self.res[k] = [None, []]
            if st[0] is not None:
                deps.add(st[0])
            for rd in st[1]:
                deps.add(rd)
        if dma:
            half = self.n_dma_sems // 2
            if eng == "gpsimd":
                i = half + self.dma_count_sw % half
                self.dma_count_sw += 1
            else:
                i = self.dma_count % half
                self.dma_count += 1
            prev = self.dma_last[i]
            if prev is not None:
                deps.add(prev)
            self.dma_last[i] = o
            self.dma_uses[i] += 1
            o.sem = i
            o.semval = 16 * self.dma_uses[i]
        deps.discard(o)
        for d in deps:
            if d.dma:
                o.deps.append(d)
            elif d.eng == eng:
                if eng == "tensor":
                    continue
                o.deps.append(d)
                d.flag = True
            else:
                o.deps.append(d)
                d.flag = True
        for k in r:
            self.res[k][1].append(o)
        for k in w:
            st = self.res[k]
            st[0] = o
            st[1] = []
        self.streams[eng].append(o)
        return o

    def mm(self, out, lhsT, rhs, start, stop, r, w):
        return self.op("tensor", lambda e: e.matmul(out, lhsT=lhsT, rhs=rhs, start=start, stop=stop), r, w)

    def tr(self, out, in_, ident, r, w):
        return self.op("tensor", lambda e: e.transpose(out=out, in_=in_, identity=ident), r, w)

    def act(self, out, in_, func, r, w, bias=None, scale=None, accum=None):
        kw = {}
        if bias is not None:
            kw["bias"] = bias
        if scale is not None:
            kw["scale"] = scale
        if accum is not None:
            kw["accum_out"] = accum
        return self.op("scalar", lambda e: e.activation(out=out, in_=in_, func=func, **kw), r, w)

    def dma(self, eng, out, in_, r, w):
        return self.op(eng, lambda e: e.dma_start(out=out, in_=in_), r, w, dma=True)

    def tt(self, eng, out, in0, in1, op, r, w):
        return self.op(eng, lambda e: e.tensor_tensor(out=out, in0=in0, in1=in1, op=op), r, w)

    def ts(self, eng, out, in0, s1, s2, op0, op1, r, w):
        if op1 is None:
            return self.op(eng, lambda e: e.tensor_scalar(out=out, in0=in0, scalar1=s1, scalar2=None, op0=op0), r, w)
        return self.op(eng, lambda e: e.tensor_scalar(out=out, in0=in0, scalar1=s1, scalar2=s2, op0=op0, op1=op1), r, w)

    def stt(self, out, in0, scalar, in1, op0, op1, r, w):
        return self.op("vector", lambda e: e.scalar_tensor_tensor(out=out, in0=in0, scalar=scalar, in1=in1, op0=op0, op1=op1), r, w)

    def cp(self, eng, out, in_, r, w):
        if eng == "scalar":
            return self.op("scalar", lambda e: e.activation(out=out, in_=in_, func=AF.Copy), r, w)
        return self.op(eng, lambda e: e.tensor_copy(out=out, in_=in_), r, w)

    def memset(self, eng, ap, val, w):
        return self.op(eng, lambda e: e.memset(ap, val), (), w)

    def barrier(self, scratch):
        n = self.nbar
        self.nbar += 1
        outstanding = [o for o in self.dma_last if o is not None]
        pe_last = [self.streams["tensor"][-1]] if self.streams["tensor"] else []
        self.op("scalar", lambda eng: eng.activation(out=scratch[:, 0:1], in_=scratch[:, 15:16], func=AF.Copy),
                ["barscr"], [("bar", n, "scalar")])
        self.op("vector", lambda eng: eng.memset(scratch[:, 1:2], 0.0), ["barscr"], [("bar", n, "vector")])
        self.op("gpsimd", lambda eng: eng.memset(scratch[:, 2:3], 0.0), ["barscr"], [("bar", n, "gpsimd")], extra=outstanding)
        self.op("sync", lambda eng: eng.dma_start(out=scratch[:, 8:10], in_=scratch[:, 12:14]),
                ["barscr"], [("bar", n, "sync")], dma=True, extra=outstanding)
        allk = [("bar", n, e) for e in ["scalar", "vector", "gpsimd", "sync"]]
        self.op("scalar", lambda eng: eng.activation(out=scratch[:, 4:5], in_=scratch[:, 15:16], func=AF.Copy),
                allk, [("bar2", n, "scalar")], extra=pe_last)
        self.op("vector", lambda eng: eng.memset(scratch[:, 5:6], 0.0), allk, [("bar2", n, "vector")], extra=pe_last)
        self.op("gpsimd", lambda eng: eng.memset(scratch[:, 6:7], 0.0), allk, [("bar2", n, "gpsimd")], extra=pe_last)
        self.op("sync", lambda eng: eng.dma_start(out=scratch[:, 10:12], in_=scratch[:, 12:14]),
                allk, [("bar2", n, "sync")], dma=True, extra=pe_last)

    def reg(self, e, val):
        cache = self.__dict__.setdefault("_regs", {})
        if val not in cache:
            cache[val] = e.to_reg(val)
        return cache[val]

    def recip(self, out, in_, r, w):
        return self.op("vector", lambda e: e.reciprocal(out=out, in_=in_), r, w)

    def rmax(self, out, in_, r, w):
        return self.op("vector", lambda e: e.reduce_max(out=out, in_=in_, axis=AX.X), r, w)

    def rsum(self, out, in_, r, w):
        return self.op("vector", lambda e: e.reduce_sum(out=out, in_=in_, axis=AX.X), r, w)

    def max8(self, out, in_, r, w):
        return self.op("vector", lambda e: e.max(out=out, in_=in_), r, w)

    def emit(self):
        nc = self.nc
        for e in self.ENGS:
            c = 0
            for o in self.streams[e]:
                if o.dma:
                    continue
                if o.flag:
                    c += 1
                    o.semval = c
        with contextlib.ExitStack() as es:
            esem = {e: es.enter_context(nc.semaphore("c_" + e)) for e in self.ENGS}
            dsem = [es.enter_context(nc.semaphore("d_%d" % i)) for i in range(self.n_dma_sems)]
            block = es.enter_context(nc.Block())

            def run_stream(ename, eng):
                waited = {}
                for o in self.streams[ename]:
                    for d in o.deps:
                        if d.dma:
                            key = ("d", d.sem)
                            sem = dsem[d.sem]
                        else:
                            key = ("e", d.eng)
                            sem = esem[d.eng]
                        if waited.get(key, 0) >= d.semval:
                            continue
                        waited[key] = d.semval
                        eng.wait_ge(sem, d.semval)
                    ins = o.fn(eng)
                    if o.dma:
                        ins.then_inc(dsem[o.sem], 16)
                    elif o.flag:
                        ins.then_inc(esem[ename], 1)
                for o in self.streams[ename]:
                    if o.dma and self.dma_last[o.sem] is o:
                        if waited.get(("d", o.sem), 0) < o.semval:
                            eng.wait_ge(dsem[o.sem], o.semval)

            @block.sync
            def _(eng):
                run_stream("sync", eng)

            @block.scalar
            def _(eng):
                run_stream("scalar", eng)

            @block.vector
            def _(eng):
                run_stream("vector", eng)

            @block.gpsimd
            def _(eng):
                run_stream("gpsimd", eng)

            @block.tensor
            def _(eng):
                run_stream("tensor", eng)


def bcast_rows(ap, nparts):
    pat = [list(p) for p in ap.ap]
    return bass.AP(ap.tensor, ap.offset, [[0, nparts]] + pat[1:])


def build_nc(S, debug=False):
    NT = S // 128
    NCH = S // 512
    NMT = 2 * NT + 32
    nc = bass.Bass("TRN2", target_bir_lowering=False)

    def din(name, shape, dt=F32):
        return nc.dram_tensor(name, shape, dt, kind="ExternalInput").ap()

    x = din("x", [S, D])
    ccol = din("ccol", [128, 8])
    w_ada = din("w_ada", [D, 6 * D])
    b_ada = din("b_ada", [1, 6 * D])
    g_mix = din("g_mix", [1, D])
    g_ffn = din("g_ffn", [1, D])
    g_fin = din("g_fin", [1, D])
    w_in = din("w_in", [D, 4360])
    sinks = din("sinks", [1, 8])
    b_forget = din("b_forget", [1, 8])
    rel_tab = din("rel_tab", [32, 8])
    selb = din("selb", [32, 128])
    wp_a = din("wp_a", [512, D])
    wp_b = din("wp_b", [512, D])
    w_out = din("w_out", [D, D])
    wr = din("wr", [D, 36])
    br = din("br", [1, 36])
    wg = din("wg", [4096, 2048])
    wu = din("wu", [4096, 2048])
    wd = din("wd", [4096, 2048])
    out = nc.dram_tensor("out", [S, D], F32, kind="ExternalOutput").ap()

    def dscr(name, shape, dt):
        return nc.dram_tensor(name, shape, dt, kind="ExternalOutput" if debug else "Internal").ap()

    QB = dscr("QB", [8, 70, S], BF16)
    KB = dscr("KB", [8, 70, S], BF16)
    VB = dscr("VB", [8, 128, NT * 65], BF16)
    OaT = dscr("OaT", [512, S], BF16)
    ObT = dscr("ObT", [512, S], BF16)
    GT = dscr("GT", [2048, S], BF16)
    X1 = dscr("X1", [S, D], F32)
    H2P = dscr("H2P", [S, D], BF16)
    H2S = dscr("H2S", [NMT * 128, D], BF16)
    YS = dscr("YS", [NMT * 128, D], F32)
    L2 = dscr("L2", [8, 384], F32)
    MODS = dscr("MODS", [4, 128, D], F32)
    WXB = [nc.dram_tensor(nm, [4096, 2048], BF16, kind="Internal").ap() for nm in ("WGB", "WUB", "WDB")]

    S_ = Sched(nc)
    top = contextlib.ExitStack()
    with top:
        def sbt(stack, name, shape, dt):
            return stack.enter_context(nc.sbuf_tensor(name, shape, dt))

        identb = sbt(top, "identb", [128, 128], BF16)
        identf = sbt(top, "identf", [128, 128], F32)
        onesf = sbt(top, "onesf", [128, 128], F32)
        onesb = sbt(top, "onesb", [128, 128], BF16)
        barscr = sbt(top, "barscr", [128, 16], F32)
        M1 = sbt(top, "M1", [128, NT, 32], F32)
        M2 = sbt(top, "M2", [128, NT, 32], F32)
        W1 = sbt(top, "W1", [128, NT], F32)
        W2 = sbt(top, "W2", [128, NT], F32)
        R1 = sbt(top, "R1", [128, NT], F32)
        R2 = sbt(top, "R2", [128, NT], F32)
        Macc = sbt(top, "Macc", [128, 32], BF16)
        psb = [top.enter_context(nc.psum_tensor("psb%d" % i, [128, 512], F32)) for i in range(8)]
        NCB = 2
        cst = [sbt(top, "cst%d" % i, [128, 2048], BF16) for i in range(NCB)]

        def conv_gen():
            srcs = (wg, wu, wd)
            prev = None
            for jn in range(96):
                e_, m_ = jn // 3, jn % 3
                S_.dma("gpsimd", cst[jn % NCB][:], srcs[m_][e_ * 128:(e_ + 1) * 128, :], [], [("cst", jn % NCB)])
                if prev is not None:
                    pe_, pm_, pj = prev
                    S_.dma("sync", WXB[pm_][pe_ * 128:(pe_ + 1) * 128, :], cst[pj % NCB][:], [("cst", pj % NCB)], [("WB", pj)])
                prev = (e_, m_, jn)
                yield
            pe_, pm_, pj = prev
            S_.dma("sync", WXB[pm_][pe_ * 128:(pe_ + 1) * 128, :], cst[pj % NCB][:], [("cst", pj % NCB)], [("WB", pj)])
            yield

        conv = conv_gen()

        def PS(i):
            return ("ps", i)

        S_.memset("vector", barscr[:], 0.0, ["barscr"])
        S_.memset("gpsimd", identb[:], 1.0, ["identb"])
        S_.op("gpsimd", lambda e: e.affine_select(out=identb[:], in_=identb[:], pattern=[[-1, 128]],
                                                  compare_op=ALU.is_equal, fill=0.0, base=0, channel_multiplier=1),
              ["identb"], ["identb"])
        S_.memset("gpsimd", identf[:], 1.0, ["identf"])
        S_.op("gpsimd", lambda e: e.affine_select(out=identf[:], in_=identf[:], pattern=[[-1, 128]],
                                                  compare_op=ALU.is_equal, fill=0.0, base=0, channel_multiplier=1),
              ["identf"], ["identf"])
        S_.memset("vector", onesf[:], 1.0, ["onesf"])
        S_.memset("vector", onesb[:], 1.0, ["onesb"])
        S_.memset("vector", Macc[:], 0.0, ["Macc"])

        pab = contextlib.ExitStack()
        A1 = sbt(pab, "A1", [128, D], F32)
        B1 = sbt(pab, "B1", [128, D], F32)
        biasT = sbt(pab, "biasT", [128, 8, 256], F32)
        Win = sbt(pab, "Win", [128, 8, 4360], BF16)
        Wka2 = sbt(pab, "Wka2", [128, 8, 128], BF16)
        w_in_v = w_in.rearrange("(k p) n -> p k n", p=128)
        for (c0, c1) in ((0, 2048), (2048, 4096), (4096, 4360)):
            S_.dma("gpsimd", Win[:, :, c0:c1], w_in_v[:, :, c0:c1], [], [("Win", c0)])
        WIN = [("Win", 0), ("Win", 2048), ("Win", 4096)]
        WKA2 = ["Wka2a", "Wka2b"]
        pro = contextlib.ExitStack()
        with pro:
            cact = sbt(pro, "cact", [128, 8], F32)
            A2 = sbt(pro, "A2", [128, D], F32)
            B2 = sbt(pro, "B2", [128, D], F32)
            Gm = sbt(pro, "Gm", [128, D], F32)
            Gf = sbt(pro, "Gf", [128, D], F32)
            CB = sbt(pro, "CB", [128, 8, 128], F32)
            wa = [sbt(pro, "wa%d" % i, [128, 8, 512], F32) for i in range(2)]
            badaB = sbt(pro, "badaB", [128, 6 * D], F32)
            gmixB = sbt(pro, "gmixB", [128, D], F32)
            gffnB = sbt(pro, "gffnB", [128, D], F32)
            S_.dma("sync", cact[:], ccol, [], ["cact"])
            S_.dma("sync", badaB[:], bcast_rows(b_ada, 128), [], ["badaB"])
            S_.dma("sync", gmixB[:], bcast_rows(g_mix, 128), [], ["gmixB"])
            S_.dma("sync", gffnB[:], bcast_rows(g_ffn, 128), [], ["gffnB"])
            S_.act(cact[:], cact[:], AF.Silu, ["cact"], ["cact"])
            for k in range(8):
                S_.cp("vector", CB[:, k, :], cact[:, k:k + 1].to_broadcast([128, 128]), ["cact"], [("CB", k)])
            dests = [B1, A1, Gm, B2, A2, Gf]
            w_ada_v = w_ada.rearrange("(k p) n -> p k n", p=128)
            for cc in range(12):
                wb = wa[cc % 2]
                S_.dma("sync", wb[:], w_ada_v[:, :, cc * 512:(cc + 1) * 512], [], [("wa", cc % 2)])
                pb = psb[cc % 2]
                for k in range(8):
                    S_.mm(pb[:], CB[:, k, :], wb[:, k, :], k == 0, k == 7,
                          [("CB", k), ("wa", cc % 2)], [PS(cc % 2)])
                dst = dests[cc // 2]
                S_.tt("vector", dst[:, (cc % 2) * 512:(cc % 2 + 1) * 512], pb[:], badaB[:, cc * 512:(cc + 1) * 512],
                      ALU.add, [PS(cc % 2), "badaB"], [(dst.name, cc % 2)])
            for (At, gB) in ((A1, gmixB), (A2, gffnB)):
                for hh in range(2):
                    sl = slice(hh * 512, (hh + 1) * 512)
                    S_.stt(At[:, sl], At[:, sl], 1.0, gB[:, sl], ALU.add, ALU.mult,
                           [(At.name, hh), gB.name], [(At.name, hh)])
            for q, tl in enumerate((A2, B2, Gm, Gf)):
                S_.dma("sync", MODS[q], tl[:], [(tl.name, 0), (tl.name, 1)], [("MODS", q)])
            bt = contextlib.ExitStack()
            with bt:
                relsb = sbt(bt, "relsb", [32, 8], F32)
                selsb = sbt(bt, "selsb", [32, 128], F32)
                tvec = sbt(bt, "tvec", [128, 8], F32)
                line = sbt(bt, "line", [8, 384], F32)
                antiJ = sbt(bt, "antiJ", [128, 2, 256], F32)
                brT = sbt(bt, "brT", [128, 2, 128], F32)
                S_.dma("sync", relsb[:], rel_tab, [], ["relsb"])
                S_.dma("sync", selsb[:], selb, [], ["selsb"])
                S_.mm(psb[2][:, 0:8], selsb[:], relsb[:], True, True, ["relsb", "selsb"], [PS(2)])
                S_.cp("vector", tvec[:], psb[2][:, 0:8], [PS(2)], ["tvec"])
                S_.op("tensor", lambda e: e.transpose(out=psb[3][0:8, 0:128], in_=tvec[:], identity=identf[:]),
                      ["tvec", "identf"], [PS(3)])
                S_.memset("vector", line[:], NEG, ["line"])
                S_.cp("vector", line[:, 127:255], psb[3][0:8, 0:128], [PS(3), "line"], ["line"])
                S_.dma("sync", L2, line[:], ["line"], ["L2"])
                S_.memset("gpsimd", antiJ[:], 1.0, ["antiJ"])
                for hf in range(2):
                    S_.op("gpsimd", (lambda hf: lambda e: e.affine_select(
                        out=antiJ[:, hf, :], in_=antiJ[:, hf, :], pattern=[[1, 256]],
                        compare_op=ALU.is_equal, fill=0.0, base=hf * 128 - 255, channel_multiplier=1))(hf),
                        ["antiJ"], ["antiJ"])
                for h in range(8):
                    for hf in range(2):
                        src = bass.AP(L2.tensor, h * 384 + hf * 128, [[1, 128], [1, 128]])
                        S_.dma("sync", brT[:, hf, :], src, ["L2"], [("brT", hf)])
                    for hf in range(2):
                        S_.mm(psb[4][:, 0:256], brT[:, hf, :], antiJ[:, hf, :], hf == 0, hf == 1,
                              [("brT", hf), "antiJ"], [PS(4)])
                    S_.cp("vector", biasT[:, h, :], psb[4][:, 0:256], [PS(4)], [("biasT", h)])

        S_.barrier(barscr)

        def full(t):
            return [(t.name, 0), (t.name, 1)]

        pa = contextlib.ExitStack()
        with pa:
            xt = [sbt(pa, "xt%d" % i, [128, D], F32) for i in range(2)]
            tmpf = [sbt(pa, "tmpf%d" % i, [128, D], F32) for i in range(1)]
            hb = [sbt(pa, "hb%d" % i, [128, D], BF16) for i in range(2)]
            hT = [sbt(pa, "hT%d" % i, [128, 8, 512], BF16) for i in range(1)]
            ssA = sbt(pa, "ssA", [128, NT], F32)
            rsA = sbt(pa, "rsA", [128, NT], F32)
            junk = sbt(pa, "junk", [128, D], BF16)
            Qtm = [sbt(pa, "Qtm%d" % i, [128, 8, 70], BF16) for i in range(2)]
            Ktm = [sbt(pa, "Ktm%d" % i, [128, 8, 70], BF16) for i in range(2)]
            QBst = [sbt(pa, "QBst%d" % i, [70, 8, 512], BF16) for i in range(1)]
            KBst = [sbt(pa, "KBst%d" % i, [70, 8, 512], BF16) for i in range(1)]
            Vst = [sbt(pa, "Vst%d" % i, [128, 8, 4, 65], BF16) for i in range(2)]
            Va = sbt(pa, "Va", [128, 12, 128], BF16)
            QTa = [sbt(pa, "QTa%d" % i, [128, 4, 512], BF16) for i in range(2)]
            KTa = sbt(pa, "KTa", [128, 2, 1536], BF16)
            Gst = [sbt(pa, "Gst%d" % i, [128, 4, 512], BF16) for i in range(2)]
            bfB = sbt(pa, "bfB", [128, 8], F32)
            sinkB = sbt(pa, "sinkB", [128, 8], F32)
            carryB = sbt(pa, "carryB", [128, 8], F32)
            tri = sbt(pa, "tri", [128, 128], F32)
            fz = [sbt(pa, "fz%d" % i, [128, 8], F32) for i in range(2)]
            cumt = [sbt(pa, "cumt%d" % i, [128, 8], F32) for i in range(2)]
            r1t = [sbt(pa, "r1t%d" % i, [128, 8], F32) for i in range(2)]
            ssb = [sbt(pa, "ssb%d" % i, [128, 256], F32) for i in range(5)]
            pbf = [sbt(pa, "pbf%d" % i, [128, 256], BF16) for i in range(5)]
            pTs = [sbt(pa, "pTs%d" % i, [128, 2, 128], BF16) for i in range(5)]
            swst = sbt(pa, "swst", [128, NT * 8, 6], F32)
            rinvA = [sbt(pa, "rinvA%d" % i, [128, 8], F32) for i in range(2)]
            Oatm = [sbt(pa, "Oatm%d" % i, [128, 512], BF16) for i in range(2)]
            OaTst = [sbt(pa, "OaTst%d" % i, [128, 4, 512], BF16) for i in range(1)]

            S_.cp("vector", Wka2[:, :, 0:64], Win[:, :, 576:640], WIN, ["Wka2a"])
            S_.cp("vector", Wka2[:, :, 64:128], Win[:, :, 512:576], WIN, ["Wka2b"])
            S_.dma("sync", bfB[:], bcast_rows(b_forget, 128), [], ["bfB"])
            S_.dma("sync", sinkB[:], bcast_rows(sinks, 128), [], ["sinkB"])
            S_.memset("vector", carryB[:], 0.0, ["carryB"])
            S_.memset("vector", ssA[:], 0.0, ["ssA"])
            S_.memset("vector", swst[:], 0.0, ["swst"])
            S_.memset("gpsimd", tri[:], 1.0, ["tri"])
            S_.op("gpsimd", lambda e: e.affine_select(out=tri[:], in_=tri[:], pattern=[[1, 128]],
                                                      compare_op=ALU.is_ge, fill=0.0, base=0, channel_multiplier=-1),
                  ["tri"], ["tri"])
            for i in range(2):
                S_.memset("vector", Qtm[i][:], 1.0, [("Qtm", i)])
                S_.memset("gpsimd", Ktm[i][:], 1.0, [("Ktm", i)])
                S_.memset("gpsimd", Vst[i][:], 1.0, [("Vst", i)])

            scale_q = 0.125
            psbf = [p[:].bitcast(BF16) for p in psb]

            def emit_H1(i):
                if i >= NT:
                    return
                xb = xt[i % 2]
                S_.dma("sync", xb[:], x[i * 128:(i + 1) * 128, :], [], [("xt", i % 2)])
                if i < 32:
                    next(conv, None)
                S_.act(junk[:], xb[:], AF.Square, [("xt", i % 2), "ssA"], ["junk", ("ss", i)], accum=ssA[:, i:i + 1])

            def emit_H2(i):
                if i >= NT:
                    return
                xb = xt[i % 2]
                S_.ts("vector", rsA[:, i:i + 1], ssA[:, i:i + 1], 1.0 / D, EPS, ALU.mult, ALU.add, [("ss", i)], [("rs", i)])
                S_.act(rsA[:, i:i + 1], rsA[:, i:i + 1], AF.Ln, [("rs", i)], [("rs", i)])
                S_.act(rsA[:, i:i + 1], rsA[:, i:i + 1], AF.Exp, [("rs", i)], [("rs", i)], scale=-0.5)
                tf = tmpf[0]
                S_.stt(tf[:], xb[:], rsA[:, i:i + 1], A1[:], ALU.mult, ALU.mult,
                       [("xt", i % 2), ("rs", i)] + full(A1), [("tmpf", 0)])
                S_.tt("gpsimd", hb[i % 2][:], tf[:], B1[:], ALU.add, [("tmpf", 0)] + full(B1), [("hb", i % 2)])

            def emit_T(c, t):
                i = 4 * c + t
                h_ = hb[i % 2]
                for k in range(8):
                    S_.tr(psbf[0][:, k * 128:(k + 1) * 128], h_[:, k * 128:(k + 1) * 128], identb[:],
                          [("hb", i % 2), "identb"], [PS(0)])
                S_.cp("scalar" if t % 2 == 0 else "vector", hT[0][:, :, t * 128:(t + 1) * 128],
                      psbf[0][:].rearrange("p (k n) -> p k n", k=8), [PS(0)], [("hT", 0, t)])

            def emit_P(c, t):
                i = 4 * c + t
                hTc = hT[0]
                rhT = [("hT", 0, t)]

                def tokproj(ps_ap, key, c0, c1):
                    for k in range(8):
                        S_.mm(ps_ap, hTc[:, k, t * 128:(t + 1) * 128], Win[:, k, c0:c1],
                              k == 0, k == 7, rhT + WIN, [key])
                q_ = Qtm[i % 2]
                k_ = Ktm[i % 2]
                f_ = fz[i % 2]
                tokproj(psb[3][:, 0:8], PS(3), 2304, 2312)
                S_.tt("vector", f_[:], psb[3][:, 0:8], bfB[:], ALU.add, [PS(3), "bfB"], [("fz", i % 2)])
                S_.act(f_[:], f_[:], AF.Exp, [("fz", i % 2)], [("fz", i % 2)], scale=-1.0)
                S_.act(f_[:], f_[:], AF.Ln, [("fz", i % 2)], [("fz", i % 2)], bias=1.0)
                tokproj(psb[1][:], PS(1), 768, 1280)
                S_.act(q_[:, :, 0:64], psb[1][:].rearrange("p (h d) -> p h d", h=8), AF.Copy,
                       [PS(1)], [("Qtm", i % 2)], scale=scale_q)
                tokproj(psb[2][:], PS(2), 1280, 1792)
                S_.cp("vector", k_[:, :, 0:64], psb[2][:].rearrange("p (h d) -> p h d", h=8), [PS(2)], [("Ktm", i % 2)])
                emit_H2(i + 1)
                tokproj(psb[3][:], PS(3), 1792, 2304)
                vs = Vst[c % 2]
                S_.cp("scalar", vs[:, :, t, 0:64], psb[3][:].rearrange("p (h d) -> p h d", h=8), [PS(3)], [("Vst", c % 2)])
                tokproj(psb[1][:, 0:128], PS(1), 640, 768)
                S_.cp("vector", Va[:, i % 12, :], psb[1][:, 0:128], [PS(1)], [("Va", i % 12)])
                S_.mm(psb[2][:, 0:8], tri[:], f_[:], True, True, ["tri", ("fz", i % 2)], [PS(2)])
                S_.mm(psb[2][:, 8:16], onesf[:], f_[:], True, True, ["onesf", ("fz", i % 2)], [PS(2)])
                cm = cumt[i % 2]
                S_.tt("vector", cm[:], carryB[:], psb[2][:, 0:8], ALU.subtract, ["carryB", PS(2)], [("cumt", i % 2)])
                S_.tt("vector", carryB[:], carryB[:], psb[2][:, 8:16], ALU.subtract, ["carryB", PS(2)], ["carryB"])
                r1 = r1t[i % 2]
                S_.cp("vector", q_[:, :, 64], cm[:], [("cumt", i % 2)], [("Qtm", i % 2)])
                S_.tt("vector", r1[:], cm[:], q_[:, :, 64], ALU.subtract, [("cumt", i % 2), ("Qtm", i % 2)], [("r1t", i % 2)])
                S_.cp("vector", q_[:, :, 65], r1[:], [("r1t", i % 2)], [("Qtm", i % 2)])
                S_.tt("vector", r1[:], r1[:], q_[:, :, 65], ALU.subtract, [("r1t", i % 2), ("Qtm", i % 2)], [("r1t", i % 2)])
                S_.cp("vector", q_[:, :, 66], r1[:], [("r1t", i % 2)], [("Qtm", i % 2)])
                S_.ts("vector", k_[:, :, 67:70], q_[:, :, 64:67], -1.0, None, ALU.mult, None,
                      [("Qtm", i % 2)], [("Ktm", i % 2)])

            def emit_C2(c, t):
                i = 4 * c + t
                q_ = Qtm[i % 2]
                k_ = Ktm[i % 2]
                for (src, dstst, key, bank, ceng) in ((q_, QBst[0], "QBst", 4, "scalar"), (k_, KBst[0], "KBst", 6, "vector")):
                    for h in range(8):
                        S_.tr(psbf[bank][0:70, h * 128:(h + 1) * 128], src[:, h, :], identb[:],
                              [("Qtm" if key == "QBst" else "Ktm", i % 2), "identb"], [PS(bank)])
                    S_.cp(ceng, dstst[:, :, t * 128:(t + 1) * 128],
                          psbf[bank][0:70, :].rearrange("p (h n) -> p h n", h=8), [PS(bank)], [(key, 0)])

            def emit_chunk_stores(c):
                for h in range(8):
                    pass
                S_.dma("gpsimd", QB[:, :, c * 512:(c + 1) * 512].rearrange("h r n -> r h n"), QBst[0][:],
                       [("QBst", 0)], [("QB", c)])
                S_.dma("gpsimd", KB[:, :, c * 512:(c + 1) * 512].rearrange("h r n -> r h n"), KBst[0][:],
                       [("KBst", 0)], [("KB", c)])
                S_.dma("gpsimd", VB[:, :, c * 260:(c + 1) * 260].rearrange("h p n -> p h n"),
                       Vst[c % 2][:].rearrange("p h t d -> p h (t d)"), [("Vst", c % 2)], [("VB", c)])

            def emit_featgroups(c, swa_gen):
                hTc = hT[0]
                rh = [("hT", 0, t) for t in range(4)]
                groups = []
                for j in range(4):
                    groups.append(("qa", j))
                groups.append(("ka", 0))
                groups.append(("ka", 1))
                for m in range(16):
                    groups.append(("g", m))
                for gi, (kind, j) in enumerate(groups):
                    bank = 1 + gi % 3
                    if kind == "qa":
                        lw = lambda k: Win[:, k, j * 128:(j + 1) * 128]
                        rw = WIN
                    elif kind == "ka":
                        lw = (lambda k: Win[:, k, 512:640]) if j == 0 else (lambda k: Wka2[:, k, :])
                        rw = WIN if j == 0 else WKA2
                    else:
                        lw = lambda k: Win[:, k, 2312 + j * 128:2312 + (j + 1) * 128]
                        rw = WIN
                    for k in range(8):
                        S_.mm(psb[bank][:], lw(k), hTc[:, k, :], k == 0, k == 7, rh + rw, [PS(bank)])
                    if kind == "qa":
                        S_.act(QTa[c % 2][:, j, :], psb[bank][:], AF.Copy, [PS(bank)], [("QTa", c % 2, j)], scale=scale_q)
                    elif kind == "ka":
                        S_.cp("vector", KTa[:, j, (c % 3) * 512:(c % 3 + 1) * 512], psb[bank][:], [PS(bank)], [("KTa", j, c % 3)])
                    else:
                        gb = Gst[(j // 4) % 2]
                        S_.act(gb[:, j % 4, :], psb[bank][:], AF.Tanh, [PS(bank)], [("Gst", (j // 4) % 2)], scale=0.5)
                        if j % 4 == 3:
                            S_.dma("gpsimd", GT[(j - 3) * 128:(j + 1) * 128, c * 512:(c + 1) * 512].rearrange("(m p) n -> p m n", p=128),
                                   gb[:], [("Gst", (j // 4) % 2)], [("GT", c, j // 4)])
                    if swa_gen is not None:
                        for _ in range(5):
                            next(swa_gen, None)

            def swa_chunk(c):
                units = [(t, hq) for t in range(4) for hq in range(8)]
                st1 = {}

                def stage1(u):
                    t, hq = units[u]
                    i = 4 * c + t
                    j, b = hq // 2, 64 * (hq % 2)
                    kv = hq // 4
                    var = 0 if (kv == 0) == (b == 0) else 1
                    nk = 256 if i > 0 else 128
                    k0 = (i - 1) * 128 if i > 0 else 0
                    sl = u % 5
                    tiles_ = ([i - 1] if i > 0 else []) + [i]
                    for bk, ti in enumerate(tiles_):
                        off = ((ti // 4) % 3) * 512 + (ti % 4) * 128
                        S_.mm(psb[5][:, bk * 128:(bk + 1) * 128], QTa[c % 2][b:b + 64, j, t * 128:(t + 1) * 128],
                              KTa[b:b + 64, var, off:off + 128], True, True,
                              [("QTa", c % 2, j), ("KTa", var, (ti // 4) % 3)], [PS(5)])
                    col = i * 8 + hq
                    S_.tt("vector", ssb[sl][:, 0:nk], psb[5][:, 0:nk], biasT[:, hq, 256 - nk:256], ALU.add,
                          [PS(5), ("biasT", hq)], [("ssb", sl)])
                    S_.rmax(swst[:, col, 0:1], ssb[sl][:, 0:nk], [("ssb", sl), "swst"], [("swst", col)])
                    S_.tt("vector", swst[:, col, 1:2], swst[:, col, 0:1], sinkB[:, hq:hq + 1], ALU.max,
                          [("swst", col), "sinkB"], [("swst", col)])
                    S_.ts("vector", swst[:, col, 2:3], swst[:, col, 1:2], -1.0, None, ALU.mult, None,
                          [("swst", col)], [("swst", col)])
                    S_.act(pbf[sl][:, 0:nk], ssb[sl][:, 0:nk], AF.Exp, [("ssb", sl), ("swst", col)],
                           [("pbf", sl), ("swst", col)], bias=swst[:, col, 2:3], accum=swst[:, col, 3:4])
                    S_.act(swst[:, col, 4:5], sinkB[:, hq:hq + 1], AF.Exp, ["sinkB", ("swst", col)], [("swst", col)],
                           bias=swst[:, col, 2:3])
                    st1[u] = (nk, sl)

                def stage2(u):
                    t, hq = units[u]
                    nk, sl = st1[u]
                    nb = nk // 128
                    i = 4 * c + t
                    col = i * 8 + hq
                    S_.tt("vector", swst[:, col, 5:6], swst[:, col, 3:4], swst[:, col, 4:5], ALU.add,
                          [("swst", col)], [("swst", col)])
                    S_.recip(rinvA[i % 2][:, hq:hq + 1], swst[:, col, 5:6], [("swst", col)], [("rinvA", i % 2, hq)])
                    for bk in range(nb):
                        S_.tr(psbf[6][:, bk * 128:(bk + 1) * 128], pbf[sl][:, bk * 128:(bk + 1) * 128], identb[:],
                              [("pbf", sl), "identb"], [PS(6)])
                    S_.cp("scalar" if u % 2 else "vector", pTs[sl][:, 0:nb, :],
                          psbf[6][:, 0:nb * 128].rearrange("p (b n) -> p b n", b=nb), [PS(6)], [("pTs", sl)])

                def stage3(u):
                    t, hq = units[u]
                    i = 4 * c + t
                    nk, sl = st1[u]
                    nb = nk // 128
                    kv = hq // 4
                    for bk in range(nb):
                        ktile = i - (nb - 1) + bk
                        S_.mm(psb[7][:, hq * 64:(hq + 1) * 64], pTs[sl][:, bk, :], Va[:, ktile % 12, kv * 64:(kv + 1) * 64],
                              bk == 0, bk == nb - 1, [("pTs", sl), ("Va", ktile % 12)], [("ps7", hq)])
                    if hq == 7:
                        oa = Oatm[i % 2]
                        S_.tt("vector", oa[:].rearrange("p (h d) -> p h d", h=8),
                              psb[7][:].rearrange("p (h d) -> p h d", h=8),
                              rinvA[i % 2][:].unsqueeze(2).to_broadcast([128, 8, 64]), ALU.mult,
                              [("ps7", h) for h in range(8)] + [("rinvA", i % 2, h) for h in range(8)], [("Oatm", i % 2)])
                        for j in range(4):
                            S_.tr(psbf[4][:, j * 128:(j + 1) * 128], oa[:, j * 128:(j + 1) * 128], identb[:],
                                  [("Oatm", i % 2), "identb"], [PS(4)])
                        S_.cp("scalar", OaTst[0][:, :, t * 128:(t + 1) * 128],
                              psbf[4][:, 0:512].rearrange("p (j n) -> p j n", j=4), [PS(4)], [("OaTst", 0)])
                        if t == 3:
                            S_.dma("gpsimd", OaT[:, c * 512:(c + 1) * 512].rearrange("(j p) n -> p j n", p=128),
                                   OaTst[0][:], [("OaTst", 0)], [("OaT", c)])

                n = len(units)
                for step in range(n + 4):
                    if step < n:
                        stage1(step)
                        yield
                    if 0 <= step - 2 < n:
                        stage2(step - 2)
                        yield
                    if 0 <= step - 4 < n:
                        stage3(step - 4)
                        yield

            prev_swa = None
            emit_H1(0)
            emit_H2(0)
            for c in range(NCH):
                for t in range(4):
                    emit_T(c, t)
                    emit_H1(4 * c + t + 1)
                    if t > 0:
                        emit_C2(c, t - 1)
                    emit_P(c, t)
                emit_C2(c, 3)
                emit_chunk_stores(c)
                emit_featgroups(c, prev_swa)
                if prev_swa is not None:
                    for _ in prev_swa:
                        pass
                prev_swa = swa_chunk(c)
            for _ in prev_swa:
                pass
        S_.barrier(barscr)
        pab.close()

        pbx = contextlib.ExitStack()
        with pbx:
            KBh = [sbt(pbx, "KBh%d" % i, [70, S], BF16) for i in range(2)]
            QBh = [sbt(pbx, "QBh%d" % i, [70, S], BF16) for i in range(2)]
            VBh = [sbt(pbx, "VBh%d" % i, [128, NT * 65], BF16) for i in range(2)]
            PTt = [sbt(pbx, "PTt%d" % i, [128, 512], BF16) for i in range(6)]
            cmask = sbt(pbx, "cmask", [128, 128], BF16)
            otf = [sbt(pbx, "otf%d" % i, [64, 512], F32) for i in range(2)]
            rinv = [sbt(pbx, "rinv%d" % i, [65, 512], F32) for i in range(2)]
            obst = [sbt(pbx, "obst%d" % i, [64, 512], BF16) for i in range(2)]
            S_.memset("gpsimd", cmask[:], -30000.0, ["cmask"])
            S_.op("gpsimd", lambda e: e.affine_select(out=cmask[:], in_=cmask[:], pattern=[[-1, 128]],
                                                      compare_op=ALU.is_gt, fill=0.0, base=0, channel_multiplier=1),
                  ["cmask"], ["cmask"])
            units = []
            for h in range(8):
                for c in range(NCH):
                    for kt in range(4 * c + 4):
                        units.append((h, c, kt))
            loaded = set()
            STB = [0, 1, 2, 3, 4]

            def load_head(h):
                if h in loaded or h >= 8:
                    return
                loaded.add(h)
                S_.dma("sync", KBh[h % 2][:], KB[h], [("KB", c) for c in range(NCH)], [("KBh", h % 2)])
                S_.dma("sync", QBh[h % 2][:], QB[h], [("QB", c) for c in range(NCH)], [("QBh", h % 2)])
                S_.dma("sync", VBh[h % 2][:], VB[h], [("VB", c) for c in range(NCH)], [("VBh", h % 2)])

            def st_mm(u):
                h, c, kt = units[u]
                j = kt - 4 * c
                q0 = 128 * j if j > 0 else 0
                bank = STB[u % 5]
                S_.mm(psb[bank][:, q0:512], KBh[h % 2][:, kt * 128:(kt + 1) * 128],
                      QBh[h % 2][:, c * 512 + q0:(c + 1) * 512], True, j < 0,
                      [("KBh", h % 2), ("QBh", h % 2)], [PS(bank)])
                if j >= 0:
                    S_.mm(psb[bank][:, q0:q0 + 128], identb[:], cmask[:], False, True, ["identb", "cmask"], [PS(bank)])

            def exp_pv(u):
                h, c, kt = units[u]
                j = kt - 4 * c
                q0 = 128 * j if j > 0 else 0
                bank = STB[u % 5]
                pt = PTt[u % 6]
                S_.act(pt[:, q0:512], psb[bank][:, q0:512], AF.Exp, [PS(bank)], [("PTt", u % 6)])
                ob = 5 + (h * NCH + c) % 2
                last = kt == 4 * c + 3
                S_.mm(psb[ob][0:65, q0:512], VBh[h % 2][:, kt * 65:(kt + 1) * 65], pt[:, q0:512], kt == 0, last,
                      [("VBh", h % 2), ("PTt", u % 6)], [PS(ob)])
                if last:
                    pp = (h * NCH + c) % 2

                    def normalize(h=h, c=c, pp=pp, ob=ob):
                        S_.act(rinv[pp][64:65, :], psb[ob][64:65, :], AF.Ln, [PS(ob)], [("rinv", pp)])
                        S_.act(rinv[pp][64:65, :], rinv[pp][64:65, :], AF.Exp, [("rinv", pp)], [("rinv", pp)], scale=-1.0)
                        S_.mm(psb[7][0:64, :], onesf[64:65, 0:64], rinv[pp][64:65, :], True, True,
                              ["onesf", ("rinv", pp)], [PS(7)])
                        S_.cp("scalar", otf[pp][:], psb[ob][0:64, :], [PS(ob)], [("otf", pp)])
                        S_.tt("vector", obst[pp][:], otf[pp][:], psb[7][0:64, :], ALU.mult,
                              [("otf", pp), PS(7)], [("obst", pp)])
                        S_.dma("gpsimd", ObT[h * 64:(h + 1) * 64, c * 512:(c + 1) * 512], obst[pp][:],
                               [("obst", pp)], [("ObT", c)])
                    pendB.append([3, normalize])

            pendB = []

            def tickB():
                for p_ in [q_ for q_ in pendB if q_[0] <= 0]:
                    pendB.remove(p_)
                    p_[1]()
                for p_ in pendB:
                    p_[0] -= 1

            load_head(0)
            load_head(1)
            n = len(units)
            LOOK = 4
            for u in range(n + LOOK):
                if u < n:
                    st_mm(u)
                if u - LOOK >= 0:
                    tickB()
                    exp_pv(u - LOOK)
                    hh, cc, kk = units[u - LOOK]
                    if kk == 4 * cc + 3 and (hh * NCH + cc) % 2 == 0:
                        next(conv, None)
                    if cc == NCH - 1 and kk == 4 * cc + 3:
                        load_head(hh + 2)
            while pendB:
                tickB()
        S_.barrier(barscr)

        pc = contextlib.ExitStack()
        with pc:
            Wpa = sbt(pc, "Wpa", [128, 4, D], BF16)
            Wpb = sbt(pc, "Wpb", [128, 4, D], BF16)
            Wout = sbt(pc, "Wout", [128, 8, D], BF16)
            Wr = sbt(pc, "Wr", [128, 8, 36], F32)
            brB = sbt(pc, "brB", [128, 36], F32)
            oaC = [sbt(pc, "oaC%d" % i, [128, 4, 512], BF16) for i in range(2)]
            obC = [sbt(pc, "obC%d" % i, [128, 4, 512], BF16) for i in range(2)]
            gtC = [sbt(pc, "gtC%d" % i, [128, 16, 512], BF16) for i in range(2)]
            xc = [sbt(pc, "xc%d" % i, [128, D], F32) for i in range(2)]
            x1t = [sbt(pc, "x1t%d" % i, [128, D], F32) for i in range(2)]
            h2f = [sbt(pc, "h2f%d" % i, [128, D], F32) for i in range(4)]
            h2b = [sbt(pc, "h2b%d" % i, [128, D], BF16) for i in range(2)]
            h2T = [sbt(pc, "h2T%d" % i, [128, 8, 128], F32) for i in range(2)]
            mT = [sbt(pc, "mT%d" % i, [128, 8, 512], BF16) for i in range(2)]
            ga = [sbt(pc, "ga%d" % i, [128, 512], F32) for i in range(2)]
            gb_ = [sbt(pc, "gb%d" % i, [128, 512], F32) for i in range(2)]
            ss2 = sbt(pc, "ss2", [128, NT], F32)
            rs2 = sbt(pc, "rs2", [128, NT], F32)
            junk2 = sbt(pc, "junk2", [128, D], BF16)
            lg = [sbt(pc, "lg%d" % i, [128, 36], F32) for i in range(2)]
            rt = sbt(pc, "rt", [128, NT, 16], F32)
            msk = [sbt(pc, "msk%d" % i, [128, 32], F32) for i in range(2)]
            top8 = [sbt(pc, "top8%d" % i, [128, 8], F32) for i in range(2)]
            gex = [sbt(pc, "gex%d" % i, [128, 4], F32) for i in range(2)]
            Mb = [sbt(pc, "Mb%d" % i, [128, 32], BF16) for i in range(2)]
            lstrict = sbt(pc, "lstrict", [128, 128], BF16)
            rk = [sbt(pc, "rk%d" % i, [128, 32], F32) for i in range(2)]
            rj = [sbt(pc, "rj%d" % i, [128, 32], F32) for i in range(2)]

            A2 = sbt(pc, "A2c", [128, D], F32)
            B2 = sbt(pc, "B2c", [128, D], F32)
            Gm = sbt(pc, "Gmc", [128, D], F32)
            for q, tl in ((0, A2), (1, B2), (2, Gm)):
                S_.dma("sync", tl[:], MODS[q], [("MODS", q)], [(tl.name, 0), (tl.name, 1)])
            S_.dma("gpsimd", Wpa[:], wp_a.rearrange("(j p) n -> p j n", p=128), [], ["Wpa"])
            S_.dma("gpsimd", Wpb[:], wp_b.rearrange("(j p) n -> p j n", p=128), [], ["Wpb"])
            for k in range(8):
                S_.dma("sync", xc[k % 2][:], w_out[k * 128:(k + 1) * 128, :], [], [("xc", k % 2)])
                S_.stt(Wout[:, k, :], xc[k % 2][:], 0.5, Gm[:], ALU.mult, ALU.mult, [("xc", k % 2)] + full(Gm), [("Wout", k)])
            WOUT = [("Wout", k) for k in range(8)]
            S_.dma("sync", Wr[:], wr.rearrange("(k p) n -> p k n", p=128), [], ["Wr"])
            S_.dma("sync", brB[:], bcast_rows(br, 128), [], ["brB"])
            S_.memset("vector", ss2[:], 0.0, ["ss2"])
            S_.memset("vector", rt[:], 0.0, ["rt"])
            S_.memset("gpsimd", lstrict[:], 1.0, ["lstrict"])
            S_.op("gpsimd", lambda e: e.affine_select(out=lstrict[:], in_=lstrict[:], pattern=[[1, 128]],
                                                      compare_op=ALU.is_gt, fill=0.0, base=0, channel_multiplier=-1),
                  ["lstrict"], ["lstrict"])

            def load_chunk(c):
                if c >= NCH:
                    return
                S_.dma("sync", oaC[c % 2][:], OaT[:, c * 512:(c + 1) * 512].rearrange("(j p) n -> p j n", p=128),
                       [("OaT", c)], [("oaC", c % 2)])
                S_.dma("sync", obC[c % 2][:], ObT[:, c * 512:(c + 1) * 512].rearrange("(j p) n -> p j n", p=128),
                       [("ObT", c)], [("obC", c % 2)])
                S_.dma("sync", gtC[c % 2][:], GT[:, c * 512:(c + 1) * 512].rearrange("(m p) n -> p m n", p=128),
                       [("GT", c, q) for q in range(4)], [("gtC", c % 2)])

            def c_merge(c, m):
                pa_, pb_ = (0, 1) if m % 2 == 0 else (2, 3)
                for j in range(4):
                    S_.mm(psb[pa_][:], Wpa[:, j, m * 128:(m + 1) * 128], oaC[c % 2][:, j, :], j == 0, j == 3,
                          ["Wpa", ("oaC", c % 2)], [PS(pa_)])
                for j in range(4):
                    S_.mm(psb[pb_][:], Wpb[:, j, m * 128:(m + 1) * 128], obC[c % 2][:, j, :], j == 0, j == 3,
                          ["Wpb", ("obC", c % 2)], [PS(pb_)])
                S_.stt(ga[m % 2][:], gtC[c % 2][:, m, :], 1.0, psb[pa_][:], ALU.add, ALU.mult,
                       [PS(pa_), ("gtC", c % 2)], [("ga", m % 2)])
                S_.stt(gb_[m % 2][:], gtC[c % 2][:, 8 + m, :], 1.0, psb[pb_][:], ALU.add, ALU.mult,
                       [PS(pb_), ("gtC", c % 2)], [("gb", m % 2)])
                S_.tt("gpsimd", mT[c % 2][:, m, :], ga[m % 2][:], gb_[m % 2][:], ALU.add,
                      [("ga", m % 2), ("gb", m % 2)], [("mT", c % 2, m)])

            def c_W(c, t):
                i = 4 * c + t
                xb = xc[i % 2]
                S_.dma("sync", xb[:], x[i * 128:(i + 1) * 128, :], [], [("xc", i % 2)])
                next(conv, None)
                x1 = x1t[i % 2]
                for hf in range(2):
                    bank = 4 + hf
                    for m in range(8):
                        S_.mm(psb[bank][:], mT[c % 2][:, m, t * 128:(t + 1) * 128], Wout[:, m, hf * 512:(hf + 1) * 512],
                              m == 0, m == 7, [("mT", c % 2, m), ("Wout", m)], [PS(bank)])
                    S_.tt("vector", x1[:, hf * 512:(hf + 1) * 512], psb[bank][:], xb[:, hf * 512:(hf + 1) * 512],
                          ALU.add, [PS(bank), ("xc", i % 2)], [("x1t", i % 2, hf)])
                X1K = [("x1t", i % 2, 0), ("x1t", i % 2, 1)]
                S_.dma("gpsimd", X1[i * 128:(i + 1) * 128, :], x1[:], X1K, [("X1", i)])
                S_.act(junk2[:], x1[:], AF.Square, X1K + ["ss2"], ["junk2", ("ss2", i)], accum=ss2[:, i:i + 1])
                S_.ts("vector", rs2[:, i:i + 1], ss2[:, i:i + 1], 1.0 / D, EPS, ALU.mult, ALU.add, [("ss2", i)], [("rs2", i)])
                S_.act(rs2[:, i:i + 1], rs2[:, i:i + 1], AF.Ln, [("rs2", i)], [("rs2", i)])
                S_.act(rs2[:, i:i + 1], rs2[:, i:i + 1], AF.Exp, [("rs2", i)], [("rs2", i)], scale=-0.5)
                hf_ = h2f[i % 4]
                S_.stt(hf_[:], x1[:], rs2[:, i:i + 1], A2[:], ALU.mult, ALU.mult, X1K + [("rs2", i)] + full(A2), [("h2f", i % 4)])
                S_.tt("gpsimd", hf_[:], hf_[:], B2[:], ALU.add, [("h2f", i % 4)] + full(B2), [("h2f", i % 4)])
                S_.cp("gpsimd", h2b[i % 2][:].rearrange("t (k p) -> t k p", k=8),
                      hf_[:].rearrange("t (p k) -> t k p", k=8), [("h2f", i % 4)], [("h2b", i % 2)])
                S_.dma("gpsimd", H2P[i * 128:(i + 1) * 128, :], h2b[i % 2][:], [("h2b", i % 2)], [("H2P", i)])

            def c_R1(i):
                hf_ = h2f[i % 4]
                for rnd in range(2):
                    for kk in range(4):
                        k = rnd * 4 + kk
                        S_.tr(psb[6][:, kk * 128:(kk + 1) * 128], hf_[:, k * 128:(k + 1) * 128], identf[:],
                              [("h2f", i % 4), "identf"], [PS(6)])
                    S_.cp("scalar" if rnd == 0 else "vector", h2T[i % 2][:, rnd * 4:rnd * 4 + 4, :],
                          psb[6][:].rearrange("p (k n) -> p k n", k=4), [PS(6)], [("h2T", i % 2, rnd)])
                for k in range(8):
                    S_.mm(psb[7][:, 0:36], h2T[i % 2][:, k, :], Wr[:, k, :], k == 0, k == 7,
                          [("h2T", i % 2, k // 4), "Wr"], [("ps7", "lg")])
                L = lg[i % 2]
                LK = ("lg", i % 2)
                S_.tt("vector", L[:], psb[7][:, 0:36], brB[:], ALU.add, [("ps7", "lg"), "brB"], [LK])
                RTK = ("rt", i)
                S_.rmax(rt[:, i, 0:1], L[:, 0:4], [LK, "rt"], [RTK])
                S_.ts("vector", rt[:, i, 1:2], rt[:, i, 0:1], -1.0, None, ALU.mult, None, [RTK], [RTK])
                S_.act(gex[i % 2][:], L[:, 0:4], AF.Exp, [LK, RTK], [("gex", i % 2), RTK], bias=rt[:, i, 1:2], accum=rt[:, i, 2:3])
                S_.recip(rt[:, i, 3:4], rt[:, i, 2:3], [RTK], [RTK])
                S_.ts("vector", gex[i % 2][:], L[:, 0:4], rt[:, i, 0:1], None, ALU.is_equal, None, [LK, RTK, ("gex", i % 2)], [("gex", i % 2)])
                S_.ts("vector", gex[i % 2][:], gex[i % 2][:], -1.0, 1e30, ALU.add, ALU.mult, [("gex", i % 2)], [("gex", i % 2)])
                mk = msk[i % 2]
                S_.tt("vector", mk[:].rearrange("p (g e) -> p g e", g=4), L[:, 4:36].rearrange("p (g e) -> p g e", g=4),
                      gex[i % 2][:].unsqueeze(2).to_broadcast([128, 4, 8]), ALU.add, [LK, ("gex", i % 2)], [("msk", i % 2)])
                S_.max8(top8[i % 2][:], mk[:], [("msk", i % 2)], [("top8", i % 2)])
                S_.ts("vector", M1[:, i, :], mk[:], top8[i % 2][:, 0:1], None, ALU.is_equal, None, [("msk", i % 2), ("top8", i % 2)], [("M1", i)])
                S_.ts("vector", M2[:, i, :], mk[:], top8[i % 2][:, 1:2], None, ALU.is_equal, None, [("msk", i % 2), ("top8", i % 2)], [("M2", i)])
                S_.tt("vector", rt[:, i, 4:5], top8[i % 2][:, 1:2], top8[i % 2][:, 0:1], ALU.subtract, [("top8", i % 2), RTK], [RTK])
                S_.act(rt[:, i, 5:6], rt[:, i, 4:5], AF.Exp, [RTK], [RTK])
                S_.ts("vector", rt[:, i, 6:7], rt[:, i, 5:6], 1.0, None, ALU.add, None, [RTK], [RTK])
                S_.recip(rt[:, i, 6:7], rt[:, i, 6:7], [RTK], [RTK])
                S_.tt("vector", W1[:, i:i + 1], rt[:, i, 6:7], rt[:, i, 3:4], ALU.mult, [RTK], [("W1", i)])
                S_.tt("vector", W2[:, i:i + 1], rt[:, i, 3:4], W1[:, i:i + 1], ALU.subtract, [RTK, ("W1", i)], [("W2", i)])
                mb = Mb[i % 2]
                S_.tt("vector", mb[:], M1[:, i, :], M2[:, i, :], ALU.add, [("M1", i), ("M2", i)], [("Mb", i % 2)])

            def c_R2(i):
                mb = Mb[i % 2]
                S_.mm(psb[7][:, 64:96], lstrict[:], mb[:], True, False, ["lstrict", ("Mb", i % 2)], [("ps7", "rk")])
                S_.mm(psb[7][:, 64:96], onesb[:], Macc[:], False, True, ["onesb", "Macc"], [("ps7", "rk")])
                S_.cp("vector", rk[i % 2][:], psb[7][:, 64:96], [("ps7", "rk")], [("rk", i % 2)])
                S_.tt("gpsimd", Macc[:], Macc[:], mb[:], ALU.add, ["Macc", ("Mb", i % 2)], ["Macc"])
                for (Mx, Rx, nm) in ((M1, R1, "R1"), (M2, R2, "R2")):
                    S_.tt("vector", rj[i % 2][:], Mx[:, i, :], rk[i % 2][:], ALU.mult,
                          [("M1" if nm == "R1" else "M2", i), ("rk", i % 2)], [("rj", i % 2)])
                    S_.rsum(Rx[:, i:i + 1], rj[i % 2][:], [("rj", i % 2)], [(nm, i)])

            pend = []

            def tick():
                due = [p_ for p_ in pend if p_[0] <= 0]
                for p_ in due:
                    pend.remove(p_)
                    p_[1](p_[2])
                for p_ in pend:
                    p_[0] -= 1

            load_chunk(0)
            for c in range(NCH):
                load_chunk(c + 1)
                for m in range(8):
                    c_merge(c, m)
                    tick()
                for t in range(4):
                    i = 4 * c + t
                    c_W(c, t)
                    tick()
                    pend.append([1, c_R1, i])
                    pend.append([2, c_R2, i])
            while pend:
                tick()
        S_.barrier(barscr)

        pd = contextlib.ExitStack()
        with pd:
            cnt = sbt(pd, "cnt", [128, 32], F32)
            thr = sbt(pd, "thr", [128, 32], F32)
            cmp_ = sbt(pd, "cmp", [128, 32, 32], F32)
            ntl = sbt(pd, "ntl", [128, 32], F32)
            incl = sbt(pd, "incl", [128, 32], F32)
            base = sbt(pd, "base", [128, 32], F32)
            ones32 = sbt(pd, "ones32", [128, 32], F32)
            tio = sbt(pd, "tio", [128, NMT], F32)
            cmp2 = sbt(pd, "cmp2", [128, NMT, 32], F32)
            etf = sbt(pd, "etf", [128, NMT], F32)
            pio = sbt(pd, "pio", [128, 1], F32)
            widx = sbt(pd, "widx", [128, NMT], I32)
            posf = sbt(pd, "posf", [128, 2, NT], F32)
            posi = sbt(pd, "posi", [128, 2, NT], I32)
            mbig = sbt(pd, "mbig", [128, NT, 32], F32)
            h2g = [sbt(pd, "h2g%d" % i, [128, D], BF16) for i in range(4)]
            hs = [sbt(pd, "hs%d" % i, [128, D], BF16) for i in range(3)]
            hsT = [sbt(pd, "hsT%d" % i, [128, 8, 128], BF16) for i in range(2)]
            Wg_ = [sbt(pd, "Wg%d" % i, [128, 8, 256], BF16) for i in range(3)]
            Wu_ = [sbt(pd, "Wu%d" % i, [128, 8, 256], BF16) for i in range(3)]
            Wd_ = [sbt(pd, "Wd%d" % i, [128, 2, D], BF16) for i in range(3)]
            sa = [sbt(pd, "sa%d" % i, [128, 256], F32) for i in range(2)]
            hid = [sbt(pd, "hid%d" % i, [128, 256], BF16) for i in range(2)]
            hidT = [sbt(pd, "hidT%d" % i, [128, 2, 128], BF16) for i in range(2)]
            yt = [sbt(pd, "yt%d" % i, [128, D], F32) for i in range(2)]
            y1 = [sbt(pd, "y1_%d" % i, [128, D], F32) for i in range(5)]
            y2 = [sbt(pd, "y2_%d" % i, [128, D], F32) for i in range(5)]
            xf = [sbt(pd, "xf%d" % i, [128, D], F32) for i in range(5)]
            ssf = sbt(pd, "ssf", [128, NT], F32)
            rsf = sbt(pd, "rsf", [128, NT], F32)
            junk3 = sbt(pd, "junk3", [128, D], BF16)

            Gf = sbt(pd, "Gfd", [128, D], F32)
            Gfin = sbt(pd, "Gfin", [128, D], F32)
            S_.dma("sync", Gf[:], MODS[3], [("MODS", 3)], [("Gfd", 0), ("Gfd", 1)])
            S_.dma("sync", Gfin[:], bcast_rows(g_fin, 128), [], ["Gfin"])
            for _ in conv:
                pass
            WBK = [("WB", jn) for jn in range(96)]
            allM = [("M1", i) for i in range(NT)] + [("M2", i) for i in range(NT)]
            S_.mm(psb[0][:, 0:32], onesb[:], Macc[:], True, True, ["onesb", "Macc"], [PS(0)])
            S_.cp("vector", cnt[:], psb[0][:, 0:32], [PS(0)], ["cnt"])
            S_.op("gpsimd", lambda e: e.iota(thr[:], pattern=[[128, 32]], base=0, channel_multiplier=0,
                                             allow_small_or_imprecise_dtypes=True), [], ["thr"])
            S_.op("gpsimd", lambda e: e.iota(tio[:], pattern=[[1, NMT]], base=0, channel_multiplier=0,
                                             allow_small_or_imprecise_dtypes=True), [], ["tio"])
            S_.op("gpsimd", lambda e: e.iota(pio[:], pattern=[[0, 1]], base=0, channel_multiplier=1,
                                             allow_small_or_imprecise_dtypes=True), [], ["pio"])
            S_.memset("vector", ones32[:], 1.0, ["ones32"])
            S_.tt("vector", cmp_[:], cnt[:].unsqueeze(2).to_broadcast([128, 32, 32]),
                  thr[:].unsqueeze(1).to_broadcast([128, 32, 32]), ALU.is_gt, ["cnt", "thr"], ["cmp"])
            S_.rsum(ntl[:], cmp_[:], ["cmp"], ["ntl"])
            S_.op("vector", lambda e: e.tensor_tensor_scan(out=incl[:], data0=ones32[:], data1=ntl[:], initial=0.0,
                                                           op0=ALU.mult, op1=ALU.add), ["ones32", "ntl"], ["incl"])
            S_.tt("vector", base[:], incl[:], ntl[:], ALU.subtract, ["incl", "ntl"], ["base"])
            S_.ts("vector", base[:], base[:], 128.0, None, ALU.mult, None, ["base"], ["base"])
            S_.tt("vector", cmp2[:], incl[:].unsqueeze(1).to_broadcast([128, NMT, 32]),
                  tio[:].unsqueeze(2).to_broadcast([128, NMT, 32]), ALU.is_le, ["incl", "tio"], ["cmp2"])
            S_.rsum(etf[:], cmp2[:], ["cmp2"], ["etf"])
            S_.ts("vector", etf[:], etf[:], 128.0, None, ALU.mult, None, ["etf"], ["etf"])
            S_.ts("vector", etf[:], etf[:], pio[:, 0:1], None, ALU.add, None, ["etf", "pio"], ["etf"])
            S_.cp("vector", widx[:], etf[:], ["etf"], ["widx"])
            for q, (Mx, Rx, nm) in enumerate(((M1, R1, "R1"), (M2, R2, "R2"))):
                S_.tt("vector", mbig[:], Mx[:], base[:].unsqueeze(1).to_broadcast([128, NT, 32]), ALU.mult,
                      [("M1" if q == 0 else "M2", i) for i in range(NT)] + ["base"], ["mbig"])
                S_.rsum(posf[:, q, :], mbig[:], ["mbig"], [("posf", q)])
                S_.tt("vector", posf[:, q, :], posf[:, q, :], Rx[:], ALU.add, [("posf", q)] + [(nm, i) for i in range(NT)], [("posf", q)])
                S_.cp("vector", posi[:, q, :], posf[:, q, :], [("posf", q)], [("posi", q)])
            for i in range(NT):
                S_.dma("sync", h2g[i % 4][:], H2P[i * 128:(i + 1) * 128, :], [("H2P", i)], [("h2g", i % 4)])
                for q in range(2):
                    S_.op("gpsimd", (lambda i, q: lambda e: e.indirect_dma_start(
                        out=H2S, out_offset=bass.IndirectOffsetOnAxis(ap=posi[:, q, i:i + 1], axis=0),
                        in_=h2g[i % 4][:], in_offset=None))(i, q),
                        [("h2g", i % 4), ("posi", q)], [("H2S", i, q)], dma=True)
            H2SK = [("H2S", i, q) for i in range(NT) for q in range(2)]
            YSK = [("YS", t) for t in range(NMT)]
            def d_load(t):
                if t >= NMT:
                    return
                S_.dma("sync", hs[t % 3][:], H2S[t * 128:(t + 1) * 128, :], H2SK, [("hs", t % 3)])
                wb3 = t % 3
                for (Wt, src, nm) in ((Wg_, WXB[0], "Wg"), (Wu_, WXB[1], "Wu"), (Wd_, WXB[2], "Wd")):
                    dst = Wt[wb3][:].rearrange("p a f -> p (a f)")
                    S_.op("gpsimd", (lambda dst, src, t: lambda e: e.indirect_dma_start(
                        out=dst, out_offset=None, in_=src,
                        in_offset=bass.IndirectOffsetOnAxis(ap=widx[:, t:t + 1], axis=0),
                        bounds_check=S_.reg(e, 4095), oob_is_err=False))(dst, src, t),
                        ["widx"] + (WBK if t == 0 else []), [(nm, wb3)], dma=True)

            def d_trA(t):
                if t >= NMT:
                    return
                b = t % 2
                for k in range(8):
                    S_.tr(psbf[b][:, k * 128:(k + 1) * 128], hs[t % 3][:, k * 128:(k + 1) * 128], identb[:],
                          [("hs", t % 3), "identb"], [PS(b)])
                S_.cp("vector", hsT[b][:], psbf[b][:].rearrange("p (k n) -> p k n", k=8), [PS(b)], [("hsT", b)])

            def d_au(t):
                b = t % 2
                wb3 = t % 3
                bank = 2 + b
                for k in range(8):
                    S_.mm(psb[bank][:, 0:256], hsT[b][:, k, :], Wg_[wb3][:, k, :], k == 0, k == 7,
                          [("hsT", b), ("Wg", wb3)], [PS(bank)])
                for k in range(8):
                    S_.mm(psb[bank][:, 256:512], hsT[b][:, k, :], Wu_[wb3][:, k, :], k == 0, k == 7,
                          [("hsT", b), ("Wu", wb3)], [PS(bank)])
                S_.act(sa[b][:], psb[bank][:, 0:256], AF.Silu, [PS(bank)], [("sa", b)])
                S_.tt("vector", hid[b][:], sa[b][:], psb[bank][:, 256:512], ALU.mult, [("sa", b), PS(bank)], [("hid", b)])

            def d_down(t):
                if t < 0:
                    return
                b = t % 2
                wb3 = t % 3
                hv = hid[b][:].rearrange("s (p j) -> s j p", j=2)
                for j in range(2):
                    S_.tr(psbf[4][:, j * 128:(j + 1) * 128], hv[:, j, :], identb[:], [("hid", b), "identb"], [PS(4)])
                S_.cp("scalar", hidT[b][:], psbf[4][:, 0:256].rearrange("p (j n) -> p j n", j=2), [PS(4)], [("hidT", b)])
                for hf in range(2):
                    bk = 5 + hf
                    for j in range(2):
                        S_.mm(psb[bk][:], hidT[b][:, j, :], Wd_[wb3][:, j, hf * 512:(hf + 1) * 512], j == 0, j == 1,
                              [("hidT", b), ("Wd", wb3)], [PS(bk)])
                    S_.cp("scalar" if hf == 0 else "vector", yt[b][:, hf * 512:(hf + 1) * 512], psb[bk][:], [PS(bk)], [("yt", b, hf)])
                S_.dma("scalar", YS[t * 128:(t + 1) * 128, :], yt[b][:], [("yt", b, 0), ("yt", b, 1)], [("YS", t)])

            d_load(0)
            d_load(1)
            d_trA(0)
            for t in range(NMT):
                d_trA(t + 1)
                d_au(t)
                d_down(t - 1)
                d_load(t + 2)
            d_down(NMT - 1)
            S_.memset("vector", ssf[:], 0.0, ["ssf"])
            for i in range(NT):
                b = i % 5
                for q, yy in ((0, y1), (1, y2)):
                    S_.op("gpsimd", (lambda yy, q, i: lambda e: e.indirect_dma_start(
                        out=yy[i % 5][:], out_offset=None, in_=YS,
                        in_offset=bass.IndirectOffsetOnAxis(ap=posi[:, q, i:i + 1], axis=0)))(yy, q, i),
                        YSK + [("posi", q)], [("y%d" % q, b)], dma=True)
                S_.dma("sync", xf[b][:], X1[i * 128:(i + 1) * 128, :], [("X1", i)], [("xf", b)])
                S_.ts("vector", y1[b][:], y1[b][:], W1[:, i:i + 1], None, ALU.mult, None, [("y0", b), ("W1", i)], [("y0", b)])
                S_.stt(y1[b][:], y2[b][:], W2[:, i:i + 1], y1[b][:], ALU.mult, ALU.add, [("y1", b), ("y0", b), ("W2", i)], [("y0", b)])
                S_.tt("gpsimd", y1[b][:], y1[b][:], Gf[:], ALU.mult, [("y0", b)] + full(Gf), [("y0", b)])
                S_.tt("vector", xf[b][:], xf[b][:], y1[b][:], ALU.add, [("xf", b), ("y0", b)], [("xf", b)])
                S_.act(junk3[:], xf[b][:], AF.Square, [("xf", b), "ssf"], ["junk3", ("ssf", i)], accum=ssf[:, i:i + 1])
                S_.ts("vector", rsf[:, i:i + 1], ssf[:, i:i + 1], 1.0 / D, EPS, ALU.mult, ALU.add, [("ssf", i)], [("rsf", i)])
                S_.act(rsf[:, i:i + 1], rsf[:, i:i + 1], AF.Ln, [("rsf", i)], [("rsf", i)])
                S_.act(rsf[:, i:i + 1], rsf[:, i:i + 1], AF.Exp, [("rsf", i)], [("rsf", i)], scale=-0.5)
                S_.stt(y2[b][:], xf[b][:], rsf[:, i:i + 1], Gfin[:], ALU.mult, ALU.mult, [("xf", b), ("rsf", i), "Gfin", ("y1", b)], [("y1", b)])
                S_.dma("scalar", out[i * 128:(i + 1) * 128, :], y2[b][:], [("y1", b)], [("out", i)])
        S_.emit()
    return nc


def _t5_bucket_np(u):
    u = np.asarray(u)
    nf = np.maximum(u, 1).astype(np.float32)
    large = 16 + (np.log(nf / np.float32(16)) / np.float32(math.log(128 / 16)) * np.float32(16)).astype(np.int32)
    large = np.minimum(large, 31)
    return np.where(u < 16, u, large)


def make_in_maps(inputs, S, cores):
    f = lambda a: np.ascontiguousarray(np.asarray(a, dtype=np.float32))
    x = f(inputs["x"])
    c = f(inputs["c"])
    buck = _t5_bucket_np(np.arange(128))
    selb = np.zeros((32, 128), np.float32)
    selb[buck, np.arange(128)] = 1.0
    wr = np.concatenate([f(inputs["w_router_group"])[0]] + [f(inputs["w_router_expert"])[0, g] for g in range(4)], axis=1)
    br = np.concatenate([f(inputs["b_router_group"])[0].reshape(1, 4), f(inputs["b_router_expert"])[0].reshape(1, 32)], axis=1)
    shared = {
        "w_ada": f(inputs["w_ada"])[0], "b_ada": f(inputs["b_ada"])[0].reshape(1, -1),
        "g_mix": f(inputs["g_norm_mix"])[0].reshape(1, -1), "g_ffn": f(inputs["g_norm_ffn"])[0].reshape(1, -1),
        "g_fin": f(inputs["g_final"]).reshape(1, -1), "w_in": f(inputs["w_in"])[0],
        "sinks": f(inputs["sinks"])[0].reshape(1, 8), "b_forget": f(inputs["b_forget"])[0].reshape(1, 8),
        "rel_tab": f(inputs["rel_bias_table"]), "selb": selb,
        "wp_a": f(inputs["w_proj_swa"])[0], "wp_b": f(inputs["w_proj_fox"])[0], "w_out": f(inputs["w_out"])[0],
        "wr": f(wr), "br": f(br),
        "wg": f(inputs["w_gate_exp"])[0].reshape(4096, 2048), "wu": f(inputs["w_up_exp"])[0].reshape(4096, 2048),
        "wd": f(inputs["w_down_exp"])[0].reshape(4096, 2048),
    }
    maps = []
    for b in cores:
        m = dict(shared)
        m["x"] = np.ascontiguousarray(x[b, :S])
        m["ccol"] = np.ascontiguousarray(c[b].reshape(8, 128).T)
        maps.append(m)
    return maps


_NC_CACHE = {}


def kernel(**inputs):
    S = 4096
    if S not in _NC_CACHE:
        _NC_CACHE[S] = build_nc(S)
    nc = _NC_CACHE[S]
    in_maps = make_in_maps(inputs, S, list(range(8)))
    res = run_bass_kernel_spmd(nc, in_maps, core_ids=list(range(8)))
    return np.stack([np.asarray(r["out"], dtype=np.float32) for r in res.results], axis=0)
```

```python
import contextlib
import math
import numpy as np
import concourse.bass as bass
import concourse.mybir as mybir
from concourse.bass_utils import run_bass_kernel_spmd

F32 = mybir.dt.float32
BF16 = mybir.dt.bfloat16
I32 = mybir.dt.int32
AF = mybir.ActivationFunctionType
ALU = mybir.AluOpType
AX = mybir.AxisListType

D = 1024
EPS = 1e-6
NEG = -1e30


class Op:
    __slots__ = ("eng", "fn", "deps", "flag", "semval", "dma", "sem")

    def __init__(self, eng, fn, dma):
        self.eng = eng
        self.fn = fn
        self.deps = []
        self.flag = False
        self.semval = 0
        self.dma = dma
        self.sem = None


class Sched:
    ENGS = ["sync", "scalar", "vector", "gpsimd", "tensor"]

    def __init__(self, nc, n_dma_sems=48):
        self.nc = nc
        self.streams = {e: [] for e in self.ENGS}
        self.res = {}
        self.n_dma_sems = n_dma_sems
        self.dma_count = 0
        self.dma_count_sw = 0
        self.dma_last = [None] * n_dma_sems
        self.dma_uses = [0] * n_dma_sems
        self.nbar = 0

    def op(self, eng, fn, r=(), w=(), dma=False, extra=()):
        o = Op(eng, fn, dma)
        deps = set(extra)
        for k in r:
            st = self.res.get(k)
            if st is None:
                st = self.res[k] = [None, []]
            if st[0] is not None:
                deps.add(st[0])
        for k in w:
            st = self.res.get(k)
            if st is None:
                st = self.res[k] = [None, []]
            if st[0] is not None:
                deps.add(st[0])
            for rd in st[1]:
                deps.add(rd)
        if dma:
            half = self.n_dma_sems // 2
            if eng == "gpsimd":
                i = half + self.dma_count_sw % half
                self.dma_count_sw += 1
            else:
                i = self.dma_count % half
                self.dma_count += 1
            prev = self.dma_last[i]
            if prev is not None:
                deps.add(prev)
            self.dma_last[i] = o
            self.dma_uses[i] += 1
            o.sem = i
            o.semval = 16 * self.dma_uses[i]
        deps.discard(o)
        for d in deps:
            if d.dma:
                o.deps.append(d)
            elif d.eng == eng:
                if eng == "tensor":
                    continue
                o.deps.append(d)
                d.flag = True
            else:
                o.deps.append(d)
                d.flag = True
        for k in r:
            self.res[k][1].append(o)
        for k in w:
            st = self.res[k]
            st[0] = o
            st[1] = []
        self.streams[eng].append(o)
        return o

    def mm(self, out, lhsT, rhs, start, stop, r, w):
        return self.op("tensor", lambda e: e.matmul(out, lhsT=lhsT, rhs=rhs, start=start, stop=stop), r, w)

    def tr(self, out, in_, ident, r, w):
        return self.op("tensor", lambda e: e.transpose(out=out, in_=in_, identity=ident), r, w)

    def act(self, out, in_, func, r, w, bias=None, scale=None, accum=None):
        kw = {}
        if bias is not None:
            kw["bias"] = bias
        if scale is not None:
            kw["scale"] = scale
        if accum is not None:
            kw["accum_out"] = accum
        return self.op("scalar", lambda e: e.activation(out=out, in_=in_, func=func, **kw), r, w)

    def dma(self, eng, out, in_, r, w):
        return self.op(eng, lambda e: e.dma_start(out=out, in_=in_), r, w, dma=True)

    def tt(self, eng, out, in0, in1, op, r, w):
        return self.op(eng, lambda e: e.tensor_tensor(out=out, in0=in0, in1=in1, op=op), r, w)

    def ts(self, eng, out, in0, s1, s2, op0, op1, r, w):
        if op1 is None:
            return self.op(eng, lambda e: e.tensor_scalar(out=out, in0=in0, scalar1=s1, scalar2=None, op0=op0), r, w)
        return self.op(eng, lambda e: e.tensor_scalar(out=out, in0=in0, scalar1=s1, scalar2=s2, op0=op0, op1=op1), r, w)

    def stt(self, out, in0, scalar, in1, op0, op1, r, w):
        return self.op("vector", lambda e: e.scalar_tensor_tensor(out=out, in0=in0, scalar=scalar, in1=in1, op0=op0, op1=op1), r, w)

    def cp(self, eng, out, in_, r, w):
        if eng == "scalar":
            return self.op("scalar", lambda e: e.activation(out=out, in_=in_, func=AF.Copy), r, w)
        return self.op(eng, lambda e: e.tensor_copy(out=out, in_=in_), r, w)

    def memset(self, eng, ap, val, w):
        return self.op(eng, lambda e: e.memset(ap, val), (), w)

    def barrier(self, scratch):
        n = self.nbar
        self.nbar += 1
        outstanding = [o for o in self.dma_last if o is not None]
        pe_last = [self.streams["tensor"][-1]] if self.streams["tensor"] else []
        self.op("scalar", lambda eng: eng.activation(out=scratch[:, 0:1], in_=scratch[:, 15:16], func=AF.Copy),
                ["barscr"], [("bar", n, "scalar")])
        self.op("vector", lambda eng: eng.memset(scratch[:, 1:2], 0.0), ["barscr"], [("bar", n, "vector")])
        self.op("gpsimd", lambda eng: eng.memset(scratch[:, 2:3], 0.0), ["barscr"], [("bar", n, "gpsimd")], extra=outstanding)
        self.op("sync", lambda eng: eng.dma_start(out=scratch[:, 8:10], in_=scratch[:, 12:14]),
                ["barscr"], [("bar", n, "sync")], dma=True, extra=outstanding)
        allk = [("bar", n, e) for e in ["scalar", "vector", "gpsimd", "sync"]]
        self.op("scalar", lambda eng: eng.activation(out=scratch[:, 4:5], in_=scratch[:, 15:16], func=AF.Copy),
                allk, [("bar2", n, "scalar")], extra=pe_last)
        self.op("vector", lambda eng: eng.memset(scratch[:, 5:6], 0.0), allk, [("bar2", n, "vector")], extra=pe_last)
        self.op("gpsimd", lambda eng: eng.memset(scratch[:, 6:7], 0.0), allk, [("bar2", n, "gpsimd")], extra=pe_last)
        self.op("sync", lambda eng: eng.dma_start(out=scratch[:, 10:12], in_=scratch[:, 12:14]),
                allk, [("bar2", n, "sync")], dma=True, extra=pe_last)

    def reg(self, e, val):
        cache = self.__dict__.setdefault("_regs", {})
        if val not in cache:
            cache[val] = e.to_reg(val)
        return cache[val]

    def recip(self, out, in_, r, w):
        return self.op("vector", lambda e: e.reciprocal(out=out, in_=in_), r, w)

    def rmax(self, out, in_, r, w):
        return self.op("vector", lambda e: e.reduce_max(out=out, in_=in_, axis=AX.X), r, w)

    def rsum(self, out, in_, r, w):
        return self.op("vector", lambda e: e.reduce_sum(out=out, in_=in_, axis=AX.X), r, w)

    def max8(self, out, in_, r, w):
        return self.op("vector", lambda e: e.max(out=out, in_=in_), r, w)

    def emit(self):
        nc = self.nc
        for e in self.ENGS:
            c = 0
            for o in self.streams[e]:
                if o.dma:
                    continue
                if o.flag:
                    c += 1
                    o.semval = c
        with contextlib.ExitStack() as es:
            esem = {e: es.enter_context(nc.semaphore("c_" + e)) for e in self.ENGS}
            dsem = [es.enter_context(nc.semaphore("d_%d" % i)) for i in range(self.n_dma_sems)]
            block = es.enter_context(nc.Block())

            def run_stream(ename, eng):
                waited = {}
                for o in self.streams[ename]:
                    for d in o.deps:
                        if d.dma:
                            key = ("d", d.sem)
                            sem = dsem[d.sem]
                        else:
                            key = ("e", d.eng)
                            sem = esem[d.eng]
                        if waited.get(key, 0) >= d.semval:
                            continue
                        waited[key] = d.semval
                        eng.wait_ge(sem, d.semval)
                    ins = o.fn(eng)
                    if o.dma:
                        ins.then_inc(dsem[o.sem], 16)
                    elif o.flag:
                        ins.then_inc(esem[ename], 1)
                for o in self.streams[ename]:
                    if o.dma and self.dma_last[o.sem] is o:
                        if waited.get(("d", o.sem), 0) < o.semval:
                            eng.wait_ge(dsem[o.sem], o.semval)

            @block.sync
            def _(eng):
                run_stream("sync", eng)

            @block.scalar
            def _(eng):
                run_stream("scalar", eng)

            @block.vector
            def _(eng):
                run_stream("vector", eng)

            @block.gpsimd
            def _(eng):
                run_stream("gpsimd", eng)

            @block.tensor
            def _(eng):
                run_stream("tensor", eng)


def bcast_rows(ap, nparts):
    pat = [list(p) for p in ap.ap]
    return bass.AP(ap.tensor, ap.offset, [[0, nparts]] + pat[1:])


def build_nc(S, debug=False):
    NT = S // 128
    NCH = S // 512
    NMT = 2 * NT + 32
    nc = bass.Bass("TRN2", target_bir_lowering=False)

    def din(name, shape, dt=F32):
        return nc.dram_tensor(name, shape, dt, kind="ExternalInput").ap()

    x = din("x", [S, D])
    ccol = din("ccol", [128, 8])
    w_ada = din("w_ada", [D, 6 * D])
    b_ada = din("b_ada", [1, 6 * D])
    g_mix = din("g_mix", [1, D])
    g_ffn = din("g_ffn", [1, D])
    g_fin = din("g_fin", [1, D])
    w_in = din("w_in", [D, 4360])
    sinks = din("sinks", [1, 8])
    b_forget = din("b_forget", [1, 8])
    rel_tab = din("rel_tab", [32, 8])
    selb = din("selb", [32, 128])
    wp_a = din("wp_a", [512, D])
    wp_b = din("wp_b", [512, D])
    w_out = din("w_out", [D, D])
    wr = din("wr", [D, 36])
    br = din("br", [1, 36])
    wg = din("wg", [4096, 2048])
    wu = din("wu", [4096, 2048])
    wd = din("wd", [4096, 2048])
    out = nc.dram_tensor("out", [S, D], F32, kind="ExternalOutput").ap()

    def dscr(name, shape, dt):
        return nc.dram_tensor(name, shape, dt, kind="ExternalOutput" if debug else "Internal").ap()

    QB = dscr("QB", [8, 70, S], BF16)
    KB = dscr("KB", [8, 70, S], BF16)
    VB = dscr("VB", [8, 128, NT * 65], BF16)
    OaT = dscr("OaT", [512, S], BF16)
    ObT = dscr("ObT", [512, S], BF16)
    GT = dscr("GT", [2048, S], BF16)
    X1 = dscr("X1", [S, D], F32)
    H2P = dscr("H2P", [S, D], BF16)
    H2S = dscr("H2S", [NMT * 128, D], BF16)
    YS = dscr("YS", [NMT * 128, D], F32)
    L2 = dscr("L2", [8, 384], F32)
    MODS = dscr("MODS", [4, 128, D], F32)
    WXB = [nc.dram_tensor(nm, [4096, 2048], BF16, kind="Internal").ap() for nm in ("WGB", "WUB", "WDB")]

    S_ = Sched(nc)
    top = contextlib.ExitStack()
    with top:
        def sbt(stack, name, shape, dt):
            return stack.enter_context(nc.sbuf_tensor(name, shape, dt))

        identb = sbt(top, "identb", [128, 128], BF16)
        identf = sbt(top, "identf", [128, 128], F32)
        onesf = sbt(top, "onesf", [128, 128], F32)
        onesb = sbt(top, "onesb", [128, 128], BF16)
        barscr = sbt(top, "barscr", [128, 16], F32)
        M1 = sbt(top, "M1", [128, NT, 32], F32)
        M2 = sbt(top, "M2", [128, NT, 32], F32)
        W1 = sbt(top, "W1", [128, NT], F32)
        W2 = sbt(top, "W2", [128, NT], F32)
        R1 = sbt(top, "R1", [128, NT], F32)
        R2 = sbt(top, "R2", [128, NT], F32)
        Macc = sbt(top, "Macc", [128, 32], BF16)
        psb = [top.enter_context(nc.psum_tensor("psb%d" % i, [128, 512], F32)) for i in range(8)]
        NCB = 2
        cst = [sbt(top, "cst%d" % i, [128, 2048], BF16) for i in range(NCB)]

        def conv_gen():
            srcs = (wg, wu, wd)
            prev = None
            for jn in range(96):
                e_, m_ = jn // 3, jn % 3
                S_.dma("gpsimd", cst[jn % NCB][:], srcs[m_][e_ * 128:(e_ + 1) * 128, :], [], [("cst", jn % NCB)])
                if prev is not None:
                    pe_, pm_, pj = prev
                    S_.dma("sync", WXB[pm_][pe_ * 128:(pe_ + 1) * 128, :], cst[pj % NCB][:], [("cst", pj % NCB)], [("WB", pj)])
                prev = (e_, m_, jn)
                yield
            pe_, pm_, pj = prev
            S_.dma("sync", WXB[pm_][pe_ * 128:(pe_ + 1) * 128, :], cst[pj % NCB][:], [("cst", pj % NCB)], [("WB", pj)])
            yield

        conv = conv_gen()

        def PS(i):
            return ("ps", i)

        S_.memset("vector", barscr[:], 0.0, ["barscr"])
        S_.memset("gpsimd", identb[:], 1.0, ["identb"])
        S_.op("gpsimd", lambda e: e.affine_select(out=identb[:], in_=identb[:], pattern=[[-1, 128]],
                                                  compare_op=ALU.is_equal, fill=0.0, base=0, channel_multiplier=1),
              ["identb"], ["identb"])
        S_.memset("gpsimd", identf[:], 1.0, ["identf"])
        S_.op("gpsimd", lambda e: e.affine_select(out=identf[:], in_=identf[:], pattern=[[-1, 128]],
                                                  compare_op=ALU.is_equal, fill=0.0, base=0, channel_multiplier=1),
              ["identf"], ["identf"])
        S_.memset("vector", onesf[:], 1.0, ["onesf"])
        S_.memset("vector", onesb[:], 1.0, ["onesb"])
        S_.memset("vector", Macc[:], 0.0, ["Macc"])

        pab = contextlib.ExitStack()
        A1 = sbt(pab, "A1", [128, D], F32)
        B1 = sbt(pab, "B1", [128, D], F32)
        biasT = sbt(pab, "biasT", [128, 8, 256], F32)
        Win = sbt(pab, "Win", [128, 8, 4360], BF16)
        Wka2 = sbt(pab, "Wka2", [128, 8, 128], BF16)
        w_in_v = w_in.rearrange("(k p) n -> p k n", p=128)
        for (c0, c1) in ((0, 2048), (2048, 4096), (4096, 4360)):
            S_.dma("gpsimd", Win[:, :, c0:c1], w_in_v[:, :, c0:c1], [], [("Win", c0)])
        WIN = [("Win", 0), ("Win", 2048), ("Win", 4096)]
        WKA2 = ["Wka2a", "Wka2b"]
        pro = contextlib.ExitStack()
        with pro:
            cact = sbt(pro, "cact", [128, 8], F32)
            A2 = sbt(pro, "A2", [128, D], F32)
            B2 = sbt(pro, "B2", [128, D], F32)
            Gm = sbt(pro, "Gm", [128, D], F32)
            Gf = sbt(pro, "Gf", [128, D], F32)
            CB = sbt(pro, "CB", [128, 8, 128], F32)
            wa = [sbt(pro, "wa%d" % i, [128, 8, 512], F32) for i in range(2)]
            badaB = sbt(pro, "badaB", [128, 6 * D], F32)
            gmixB = sbt(pro, "gmixB", [128, D], F32)
            gffnB = sbt(pro, "gffnB", [128, D], F32)
            S_.dma("sync", cact[:], ccol, [], ["cact"])
            S_.dma("sync", badaB[:], bcast_rows(b_ada, 128), [], ["badaB"])
            S_.dma("sync", gmixB[:], bcast_rows(g_mix, 128), [], ["gmixB"])
            S_.dma("sync", gffnB[:], bcast_rows(g_ffn, 128), [], ["gffnB"])
            S_.act(cact[:], cact[:], AF.Silu, ["cact"], ["cact"])
            for k in range(8):
                S_.cp("vector", CB[:, k, :], cact[:, k:k + 1].to_broadcast([128, 128]), ["cact"], [("CB", k)])
            dests = [B1, A1, Gm, B2, A2, Gf]
            w_ada_v = w_ada.rearrange("(k p) n -> p k n", p=128)
            for cc in range(12):
                wb = wa[cc % 2]
                S_.dma("sync", wb[:], w_ada_v[:, :, cc * 512:(cc + 1) * 512], [], [("wa", cc % 2)])
                pb = psb[cc % 2]
                for k in range(8):
                    S_.mm(pb[:], CB[:, k, :], wb[:, k, :], k == 0, k == 7,
                          [("CB", k), ("wa", cc % 2)], [PS(cc % 2)])
                dst = dests[cc // 2]
                S_.tt("vector", dst[:, (cc % 2) * 512:(cc % 2 + 1) * 512], pb[:], badaB[:, cc * 512:(cc + 1) * 512],
                      ALU.add, [PS(cc % 2), "badaB"], [(dst.name, cc % 2)])
            for (At, gB) in ((A1, gmixB), (A2, gffnB)):
                for hh in range(2):
                    sl = slice(hh * 512, (hh + 1) * 512)
                    S_.stt(At[:, sl], At[:, sl], 1.0, gB[:, sl], ALU.add, ALU.mult,
                           [(At.name, hh), gB.name], [(At.name, hh)])
            for q, tl in enumerate((A2, B2, Gm, Gf)):
                S_.dma("sync", MODS[q], tl[:], [(tl.name, 0), (tl.name, 1)], [("MODS", q)])
            bt = contextlib.ExitStack()
            with bt:
                relsb = sbt(bt, "relsb", [32, 8], F32)
                selsb = sbt(bt, "selsb", [32, 128], F32)
                tvec = sbt(bt, "tvec", [128, 8], F32)
                line = sbt(bt, "line", [8, 384], F32)
                antiJ = sbt(bt, "antiJ", [128, 2, 256], F32)
                brT = sbt(bt, "brT", [128, 2, 128], F32)
                S_.dma("sync", relsb[:], rel_tab, [], ["relsb"])
                S_.dma("sync", selsb[:], selb, [], ["selsb"])
                S_.mm(psb[2][:, 0:8], selsb[:], relsb[:], True, True, ["relsb", "selsb"], [PS(2)])
                S_.cp("vector", tvec[:], psb[2][:, 0:8], [PS(2)], ["tvec"])
                S_.op("tensor", lambda e: e.transpose(out=psb[3][0:8, 0:128], in_=tvec[:], identity=identf[:]),
                      ["tvec", "identf"], [PS(3)])
                S_.memset("vector", line[:], NEG, ["line"])
                S_.cp("vector", line[:, 127:255], psb[3][0:8, 0:128], [PS(3), "line"], ["line"])
                S_.dma("sync", L2, line[:], ["line"], ["L2"])
                S_.memset("gpsimd", antiJ[:], 1.0, ["antiJ"])
                for hf in range(2):
                    S_.op("gpsimd", (lambda hf: lambda e: e.affine_select(
                        out=antiJ[:, hf, :], in_=antiJ[:, hf, :], pattern=[[1, 256]],
                        compare_op=ALU.is_equal, fill=0.0, base=hf * 128 - 255, channel_multiplier=1))(hf),
                        ["antiJ"], ["antiJ"])
                for h in range(8):
                    for hf in range(2):
                        src = bass.AP(L2.tensor, h * 384 + hf * 128, [[1, 128], [1, 128]])
                        S_.dma("sync", brT[:, hf, :], src, ["L2"], [("brT", hf)])
                    for hf in range(2):
                        S_.mm(psb[4][:, 0:256], brT[:, hf, :], antiJ[:, hf, :], hf == 0, hf == 1,
                              [("brT", hf), "antiJ"], [PS(4)])
                    S_.cp("vector", biasT[:, h, :], psb[4][:, 0:256], [PS(4)], [("biasT", h)])

        S_.barrier(barscr)

        def full(t):
            return [(t.name, 0), (t.name, 1)]

        pa = contextlib.ExitStack()
        with pa:
            xt = [sbt(pa, "xt%d" % i, [128, D], F32) for i in range(2)]
            tmpf = [sbt(pa, "tmpf%d" % i, [128, D], F32) for i in range(1)]
            hb = [sbt(pa, "hb%d" % i, [128, D], BF16) for i in range(2)]
            hT = [sbt(pa, "hT%d" % i, [128, 8, 512], BF16) for i in range(1)]
            ssA = sbt(pa, "ssA", [128, NT], F32)
            rsA = sbt(pa, "rsA", [128, NT], F32)
            junk = sbt(pa, "junk", [128, D], BF16)
            Qtm = [sbt(pa, "Qtm%d" % i, [128, 8, 70], BF16) for i in range(2)]
            Ktm = [sbt(pa, "Ktm%d" % i, [128, 8, 70], BF16) for i in range(2)]
            QBst = [sbt(pa, "QBst%d" % i, [70, 8, 512], BF16) for i in range(1)]
            KBst = [sbt(pa, "KBst%d" % i, [70, 8, 512], BF16) for i in range(1)]
            Vst = [sbt(pa, "Vst%d" % i, [128, 8, 4, 65], BF16) for i in range(2)]
            Va = sbt(pa, "Va", [128, 12, 128], BF16)
            QTa = [sbt(pa, "QTa%d" % i, [128, 4, 512], BF16) for i in range(2)]
            KTa = sbt(pa, "KTa", [128, 2, 1536], BF16)
            Gst = [sbt(pa, "Gst%d" % i, [128, 4, 512], BF16) for i in range(2)]
            bfB = sbt(pa, "bfB", [128, 8], F32)
            sinkB = sbt(pa, "sinkB", [128, 8], F32)
            carryB = sbt(pa, "carryB", [128, 8], F32)
            tri = sbt(pa, "tri", [128, 128], F32)
            fz = [sbt(pa, "fz%d" % i, [128, 8], F32) for i in range(2)]
            cumt = [sbt(pa, "cumt%d" % i, [128, 8], F32) for i in range(2)]
            r1t = [sbt(pa, "r1t%d" % i, [128, 8], F32) for i in range(2)]
            ssb = [sbt(pa, "ssb%d" % i, [128, 256], F32) for i in range(5)]
            pbf = [sbt(pa, "pbf%d" % i, [128, 256], BF16) for i in range(5)]
            pTs = [sbt(pa, "pTs%d" % i, [128, 2, 128], BF16) for i in range(5)]
            swst = sbt(pa, "swst", [128, NT * 8, 6], F32)
            rinvA = [sbt(pa, "rinvA%d" % i, [128, 8], F32) for i in range(2)]
            Oatm = [sbt(pa, "Oatm%d" % i, [128, 512], BF16) for i in range(2)]
            OaTst = [sbt(pa, "OaTst%d" % i, [128, 4, 512], BF16) for i in range(1)]

            S_.cp("vector", Wka2[:, :, 0:64], Win[:, :, 576:640], WIN, ["Wka2a"])
            S_.cp("vector", Wka2[:, :, 64:128], Win[:, :, 512:576], WIN, ["Wka2b"])
            S_.dma("sync", bfB[:], bcast_rows(b_forget, 128), [], ["bfB"])
            S_.dma("sync", sinkB[:], bcast_rows(sinks, 128), [], ["sinkB"])
            S_.memset("vector", carryB[:], 0.0, ["carryB"])
            S_.memset("vector", ssA[:], 0.0, ["ssA"])
            S_.memset("vector", swst[:], 0.0, ["swst"])
            S_.memset("gpsimd", tri[:], 1.0, ["tri"])
            S_.op("gpsimd", lambda e: e.affine_select(out=tri[:], in_=tri[:], pattern=[[1, 128]],
                                                      compare_op=ALU.is_ge, fill=0.0, base=0, channel_multiplier=-1),
                  ["tri"], ["tri"])
            for i in range(2):
                S_.memset("vector", Qtm[i][:], 1.0, [("Qtm", i)])
                S_.memset("gpsimd", Ktm[i][:], 1.0, [("Ktm", i)])
                S_.memset("gpsimd", Vst[i][:], 1.0, [("Vst", i)])

            scale_q = 0.125
            psbf = [p[:].bitcast(BF16) for p in psb]

            def emit_H(i):
                if i >= NT:
                    return
                xb = xt[i % 2]
                S_.dma("sync", xb[:], x[i * 128:(i + 1) * 128, :], [], [("xt", i % 2)])
                if i < 32:
                    next(conv, None)
                S_.act(junk[:], xb[:], AF.Square, [("xt", i % 2), "ssA"], ["junk", ("ss", i)], accum=ssA[:, i:i + 1])
                S_.ts("vector", rsA[:, i:i + 1], ssA[:, i:i + 1], 1.0 / D, EPS, ALU.mult, ALU.add, [("ss", i)], [("rs", i)])
                S_.act(rsA[:, i:i + 1], rsA[:, i:i + 1], AF.Ln, [("rs", i)], [("rs", i)])
                S_.act(rsA[:, i:i + 1], rsA[:, i:i + 1], AF.Exp, [("rs", i)], [("rs", i)], scale=-0.5)
                tf = tmpf[0]
                S_.stt(tf[:], xb[:], rsA[:, i:i + 1], A1[:], ALU.mult, ALU.mult,
                       [("xt", i % 2), ("rs", i)] + full(A1), [("tmpf", 0)])
                S_.tt("gpsimd", hb[i % 2][:], tf[:], B1[:], ALU.add, [("tmpf", 0)] + full(B1), [("hb", i % 2)])

            def emit_T(c, t):
                i = 4 * c + t
                h_ = hb[i % 2]
                for k in range(8):
                    S_.tr(psbf[0][:, k * 128:(k + 1) * 128], h_[:, k * 128:(k + 1) * 128], identb[:],
                          [("hb", i % 2), "identb"], [PS(0)])
                S_.cp("scalar" if t % 2 == 0 else "vector", hT[0][:, :, t * 128:(t + 1) * 128],
                      psbf[0][:].rearrange("p (k n) -> p k n", k=8), [PS(0)], [("hT", 0, t)])

            def emit_P(c, t):
                i = 4 * c + t
                hTc = hT[0]
                rhT = [("hT", 0, t)]

                def tokproj(ps_ap, key, c0, c1):
                    for k in range(8):
                        S_.mm(ps_ap, hTc[:, k, t * 128:(t + 1) * 128], Win[:, k, c0:c1],
                              k == 0, k == 7, rhT + WIN, [key])
                q_ = Qtm[i % 2]
                k_ = Ktm[i % 2]
                f_ = fz[i % 2]
                tokproj(psb[3][:, 0:8], PS(3), 2304, 2312)
                S_.tt("vector", f_[:], psb[3][:, 0:8], bfB[:], ALU.add, [PS(3), "bfB"], [("fz", i % 2)])
                S_.act(f_[:], f_[:], AF.Exp, [("fz", i % 2)], [("fz", i % 2)], scale=-1.0)
                S_.act(f_[:], f_[:], AF.Ln, [("fz", i % 2)], [("fz", i % 2)], bias=1.0)
                tokproj(psb[1][:], PS(1), 768, 1280)
                S_.act(q_[:, :, 0:64], psb[1][:].rearrange("p (h d) -> p h d", h=8), AF.Copy,
                       [PS(1)], [("Qtm", i % 2)], scale=scale_q)
                tokproj(psb[2][:], PS(2), 1280, 1792)
                S_.cp("vector", k_[:, :, 0:64], psb[2][:].rearrange("p (h d) -> p h d", h=8), [PS(2)], [("Ktm", i % 2)])
                tokproj(psb[3][:], PS(3), 1792, 2304)
                vs = Vst[c % 2]
                S_.cp("scalar", vs[:, :, t, 0:64], psb[3][:].rearrange("p (h d) -> p h d", h=8), [PS(3)], [("Vst", c % 2)])
                tokproj(psb[1][:, 0:128], PS(1), 640, 768)
                S_.cp("vector", Va[:, i % 12, :], psb[1][:, 0:128], [PS(1)], [("Va", i % 12)])
                S_.mm(psb[2][:, 0:8], tri[:], f_[:], True, True, ["tri", ("fz", i % 2)], [PS(2)])
                S_.mm(psb[2][:, 8:16], onesf[:], f_[:], True, True, ["onesf", ("fz", i % 2)], [PS(2)])
                cm = cumt[i % 2]
                S_.tt("vector", cm[:], carryB[:], psb[2][:, 0:8], ALU.subtract, ["carryB", PS(2)], [("cumt", i % 2)])
                S_.tt("vector", carryB[:], carryB[:], psb[2][:, 8:16], ALU.subtract, ["carryB", PS(2)], ["carryB"])
                r1 = r1t[i % 2]
                S_.cp("vector", q_[:, :, 64], cm[:], [("cumt", i % 2)], [("Qtm", i % 2)])
                S_.tt("vector", r1[:], cm[:], q_[:, :, 64], ALU.subtract, [("cumt", i % 2), ("Qtm", i % 2)], [("r1t", i % 2)])
                S_.cp("vector", q_[:, :, 65], r1[:], [("r1t", i % 2)], [("Qtm", i % 2)])
                S_.tt("vector", r1[:], r1[:], q_[:, :, 65], ALU.subtract, [("r1t", i % 2), ("Qtm", i % 2)], [("r1t", i % 2)])
                S_.cp("vector", q_[:, :, 66], r1[:], [("r1t", i % 2)], [("Qtm", i % 2)])
                S_.ts("vector", k_[:, :, 67:70], q_[:, :, 64:67], -1.0, None, ALU.mult, None,
                      [("Qtm", i % 2)], [("Ktm", i % 2)])

            def emit_C2(c, t):
                i = 4 * c + t
                q_ = Qtm[i % 2]
                k_ = Ktm[i % 2]
                for (src, dstst, key, bank, ceng) in ((q_, QBst[0], "QBst", 4, "scalar"), (k_, KBst[0], "KBst", 6, "vector")):
                    for h in range(8):
                        S_.tr(psbf[bank][0:70, h * 128:(h + 1) * 128], src[:, h, :], identb[:],
                              [("Qtm" if key == "QBst" else "Ktm", i % 2), "identb"], [PS(bank)])
                    S_.cp(ceng, dstst[:, :, t * 128:(t + 1) * 128],
                          psbf[bank][0:70, :].rearrange("p (h n) -> p h n", h=8), [PS(bank)], [(key, 0)])

            def emit_chunk_stores(c):
                for h in range(8):
                    pass
                S_.dma("gpsimd", QB[:, :, c * 512:(c + 1) * 512].rearrange("h r n -> r h n"), QBst[0][:],
                       [("QBst", 0)], [("QB", c)])
                S_.dma("gpsimd", KB[:, :, c * 512:(c + 1) * 512].rearrange("h r n -> r h n"), KBst[0][:],
                       [("KBst", 0)], [("KB", c)])
                S_.dma("gpsimd", VB[:, :, c * 260:(c + 1) * 260].rearrange("h p n -> p h n"),
                       Vst[c % 2][:].rearrange("p h t d -> p h (t d)"), [("Vst", c % 2)], [("VB", c)])

            def emit_featgroups(c, swa_gen):
                hTc = hT[0]
                rh = [("hT", 0, t) for t in range(4)]
                groups = []
                for j in range(4):
                    groups.append(("qa", j))
                groups.append(("ka", 0))
                groups.append(("ka", 1))
                for m in range(16):
                    groups.append(("g", m))
                for gi, (kind, j) in enumerate(groups):
                    bank = 1 + gi % 3
                    if kind == "qa":
                        lw = lambda k: Win[:, k, j * 128:(j + 1) * 128]
                        rw = WIN
                    elif kind == "ka":
                        lw = (lambda k: Win[:, k, 512:640]) if j == 0 else (lambda k: Wka2[:, k, :])
                        rw = WIN if j == 0 else WKA2
                    else:
                        lw = lambda k: Win[:, k, 2312 + j * 128:2312 + (j + 1) * 128]
                        rw = WIN
                    for k in range(8):
                        S_.mm(psb[bank][:], lw(k), hTc[:, k, :], k == 0, k == 7, rh + rw, [PS(bank)])
                    if kind == "qa":
                        S_.act(QTa[c % 2][:, j, :], psb[bank][:], AF.Copy, [PS(bank)], [("QTa", c % 2, j)], scale=scale_q)
                    elif kind == "ka":
                        S_.cp("vector", KTa[:, j, (c % 3) * 512:(c % 3 + 1) * 512], psb[bank][:], [PS(bank)], [("KTa", j, c % 3)])
                    else:
                        gb = Gst[(j // 4) % 2]
                        S_.act(gb[:, j % 4, :], psb[bank][:], AF.Tanh, [PS(bank)], [("Gst", (j // 4) % 2)], scale=0.5)
                        if j % 4 == 3:
                            S_.dma("gpsimd", GT[(j - 3) * 128:(j + 1) * 128, c * 512:(c + 1) * 512].rearrange("(m p) n -> p m n", p=128),
                                   gb[:], [("Gst", (j // 4) % 2)], [("GT", c, j // 4)])
                    if swa_gen is not None:
                        for _ in range(5):
                            next(swa_gen, None)

            def swa_chunk(c):
                units = [(t, hq) for t in range(4) for hq in range(8)]
                st1 = {}

                def stage1(u):
                    t, hq = units[u]
                    i = 4 * c + t
                    j, b = hq // 2, 64 * (hq % 2)
                    kv = hq // 4
                    var = 0 if (kv == 0) == (b == 0) else 1
                    nk = 256 if i > 0 else 128
                    k0 = (i - 1) * 128 if i > 0 else 0
                    sl = u % 5
                    tiles_ = ([i - 1] if i > 0 else []) + [i]
                    for bk, ti in enumerate(tiles_):
                        off = ((ti // 4) % 3) * 512 + (ti % 4) * 128
                        S_.mm(psb[5][:, bk * 128:(bk + 1) * 128], QTa[c % 2][b:b + 64, j, t * 128:(t + 1) * 128],
                              KTa[b:b + 64, var, off:off + 128], True, True,
                              [("QTa", c % 2, j), ("KTa", var, (ti // 4) % 3)], [PS(5)])
                    col = i * 8 + hq
                    S_.tt("vector", ssb[sl][:, 0:nk], psb[5][:, 0:nk], biasT[:, hq, 256 - nk:256], ALU.add,
                          [PS(5), ("biasT", hq)], [("ssb", sl)])
                    S_.rmax(swst[:, col, 0:1], ssb[sl][:, 0:nk], [("ssb", sl), "swst"], [("swst", col)])
                    S_.tt("vector", swst[:, col, 1:2], swst[:, col, 0:1], sinkB[:, hq:hq + 1], ALU.max,
                          [("swst", col), "sinkB"], [("swst", col)])
                    S_.ts("vector", swst[:, col, 2:3], swst[:, col, 1:2], -1.0, None, ALU.mult, None,
                          [("swst", col)], [("swst", col)])
                    S_.act(pbf[sl][:, 0:nk], ssb[sl][:, 0:nk], AF.Exp, [("ssb", sl), ("swst", col)],
                           [("pbf", sl), ("swst", col)], bias=swst[:, col, 2:3], accum=swst[:, col, 3:4])
                    S_.act(swst[:, col, 4:5], sinkB[:, hq:hq + 1], AF.Exp, ["sinkB", ("swst", col)], [("swst", col)],
                           bias=swst[:, col, 2:3])
                    st1[u] = (nk, sl)

                def stage2(u):
                    t, hq = units[u]
                    nk, sl = st1[u]
                    nb = nk // 128
                    i = 4 * c + t
                    col = i * 8 + hq
                    S_.tt("vector", swst[:, col, 5:6], swst[:, col, 3:4], swst[:, col, 4:5], ALU.add,
                          [("swst", col)], [("swst", col)])
                    S_.recip(rinvA[i % 2][:, hq:hq + 1], swst[:, col, 5:6], [("swst", col)], [("rinvA", i % 2, hq)])
                    for bk in range(nb):
                        S_.tr(psbf[6][:, bk * 128:(bk + 1) * 128], pbf[sl][:, bk * 128:(bk + 1) * 128], identb[:],
                              [("pbf", sl), "identb"], [PS(6)])
                    S_.cp("scalar" if u % 2 else "vector", pTs[sl][:, 0:nb, :],
                          psbf[6][:, 0:nb * 128].rearrange("p (b n) -> p b n", b=nb), [PS(6)], [("pTs", sl)])

                def stage3(u):
                    t, hq = units[u]
                    i = 4 * c + t
                    nk, sl = st1[u]
                    nb = nk // 128
                    kv = hq // 4
                    for bk in range(nb):
                        ktile = i - (nb - 1) + bk
                        S_.mm(psb[7][:, hq * 64:(hq + 1) * 64], pTs[sl][:, bk, :], Va[:, ktile % 12, kv * 64:(kv + 1) * 64],
                              bk == 0, bk == nb - 1, [("pTs", sl), ("Va", ktile % 12)], [("ps7", hq)])
                    if hq == 7:
                        oa = Oatm[i % 2]
                        S_.tt("vector", oa[:].rearrange("p (h d) -> p h d", h=8),
                              psb[7][:].rearrange("p (h d) -> p h d", h=8),
                              rinvA[i % 2][:].unsqueeze(2).to_broadcast([128, 8, 64]), ALU.mult,
                              [("ps7", h) for h in range(8)] + [("rinvA", i % 2, h) for h in range(8)], [("Oatm", i % 2)])
                        for j in range(4):
                            S_.tr(psbf[4][:, j * 128:(j + 1) * 128], oa[:, j * 128:(j + 1) * 128], identb[:],
                                  [("Oatm", i % 2), "identb"], [PS(4)])
                        S_.cp("scalar", OaTst[0][:, :, t * 128:(t + 1) * 128],
                              psbf[4][:, 0:512].rearrange("p (j n) -> p j n", j=4), [PS(4)], [("OaTst", 0)])
                        if t == 3:
                            S_.dma("gpsimd", OaT[:, c * 512:(c + 1) * 512].rearrange("(j p) n -> p j n", p=128),
                                   OaTst[0][:], [("OaTst", 0)], [("OaT", c)])

                n = len(units)
                for step in range(n + 4):
                    if step < n:
                        stage1(step)
                        yield
                    if 0 <= step - 2 < n:
                        stage2(step - 2)
                        yield
                    if 0 <= step - 4 < n:
                        stage3(step - 4)
                        yield

            prev_swa = None
            emit_H(0)
            for c in range(NCH):
                for t in range(4):
                    emit_T(c, t)
                    emit_H(4 * c + t + 1)
                    if t > 0:
                        emit_C2(c, t - 1)
                    emit_P(c, t)
                emit_C2(c, 3)
                emit_chunk_stores(c)
                emit_featgroups(c, prev_swa)
                if prev_swa is not None:
                    for _ in prev_swa:
                        pass
                prev_swa = swa_chunk(c)
            for _ in prev_swa:
                pass
        S_.barrier(barscr)
        pab.close()

        pbx = contextlib.ExitStack()
        with pbx:
            KBh = [sbt(pbx, "KBh%d" % i, [70, S], BF16) for i in range(2)]
            QBh = [sbt(pbx, "QBh%d" % i, [70, S], BF16) for i in range(2)]
            VBh = [sbt(pbx, "VBh%d" % i, [128, NT * 65], BF16) for i in range(2)]
            PTt = [sbt(pbx, "PTt%d" % i, [128, 512], BF16) for i in range(6)]
            cmask = sbt(pbx, "cmask", [128, 128], BF16)
            otf = [sbt(pbx, "otf%d" % i, [64, 512], F32) for i in range(2)]
            rinv = [sbt(pbx, "rinv%d" % i, [65, 512], F32) for i in range(2)]
            obst = [sbt(pbx, "obst%d" % i, [64, 512], BF16) for i in range(2)]
            S_.memset("gpsimd", cmask[:], -30000.0, ["cmask"])
            S_.op("gpsimd", lambda e: e.affine_select(out=cmask[:], in_=cmask[:], pattern=[[-1, 128]],
                                                      compare_op=ALU.is_gt, fill=0.0, base=0, channel_multiplier=1),
                  ["cmask"], ["cmask"])
            units = []
            for h in range(8):
                for c in range(NCH):
                    for kt in range(4 * c + 4):
                        units.append((h, c, kt))
            loaded = set()
            STB = [0, 1, 2, 3, 4]

            def load_head(h):
                if h in loaded or h >= 8:
                    return
                loaded.add(h)
                S_.dma("sync", KBh[h % 2][:], KB[h], [("KB", c) for c in range(NCH)], [("KBh", h % 2)])
                S_.dma("sync", QBh[h % 2][:], QB[h], [("QB", c) for c in range(NCH)], [("QBh", h % 2)])
                S_.dma("sync", VBh[h % 2][:], VB[h], [("VB", c) for c in range(NCH)], [("VBh", h % 2)])

            def st_mm(u):
                h, c, kt = units[u]
                j = kt - 4 * c
                q0 = 128 * j if j > 0 else 0
                bank = STB[u % 5]
                S_.mm(psb[bank][:, q0:512], KBh[h % 2][:, kt * 128:(kt + 1) * 128],
                      QBh[h % 2][:, c * 512 + q0:(c + 1) * 512], True, j < 0,
                      [("KBh", h % 2), ("QBh", h % 2)], [PS(bank)])
                if j >= 0:
                    S_.mm(psb[bank][:, q0:q0 + 128], identb[:], cmask[:], False, True, ["identb", "cmask"], [PS(bank)])

            def exp_pv(u):
                h, c, kt = units[u]
                j = kt - 4 * c
                q0 = 128 * j if j > 0 else 0
                bank = STB[u % 5]
                pt = PTt[u % 6]
                S_.act(pt[:, q0:512], psb[bank][:, q0:512], AF.Exp, [PS(bank)], [("PTt", u % 6)])
                ob = 5 + (h * NCH + c) % 2
                last = kt == 4 * c + 3
                S_.mm(psb[ob][0:65, q0:512], VBh[h % 2][:, kt * 65:(kt + 1) * 65], pt[:, q0:512], kt == 0, last,
                      [("VBh", h % 2), ("PTt", u % 6)], [PS(ob)])
                if last:
                    pp = (h * NCH + c) % 2

                    def normalize(h=h, c=c, pp=pp, ob=ob):
                        S_.act(rinv[pp][64:65, :], psb[ob][64:65, :], AF.Ln, [PS(ob)], [("rinv", pp)])
                        S_.act(rinv[pp][64:65, :], rinv[pp][64:65, :], AF.Exp, [("rinv", pp)], [("rinv", pp)], scale=-1.0)
                        S_.mm(psb[7][0:64, :], onesf[64:65, 0:64], rinv[pp][64:65, :], True, True,
                              ["onesf", ("rinv", pp)], [PS(7)])
                        S_.cp("vector", otf[pp][:], psb[ob][0:64, :], [PS(ob)], [("otf", pp)])
                        S_.tt("vector", obst[pp][:], otf[pp][:], psb[7][0:64, :], ALU.mult,
                              [("otf", pp), PS(7)], [("obst", pp)])
                        S_.dma("gpsimd", ObT[h * 64:(h + 1) * 64, c * 512:(c + 1) * 512], obst[pp][:],
                               [("obst", pp)], [("ObT", c)])
                    pendB.append([3, normalize])

            pendB = []

            def tickB():
                for p_ in [q_ for q_ in pendB if q_[0] <= 0]:
                    pendB.remove(p_)
                    p_[1]()
                for p_ in pendB:
                    p_[0] -= 1

            load_head(0)
            load_head(1)
            n = len(units)
            LOOK = 4
            for u in range(n + LOOK):
                if u < n:
                    st_mm(u)
                if u - LOOK >= 0:
                    tickB()
                    exp_pv(u - LOOK)
                    hh, cc, kk = units[u - LOOK]
                    if kk == 4 * cc + 3 and (hh * NCH + cc) % 2 == 0:
                        next(conv, None)
                    if cc == NCH - 1 and kk == 4 * cc + 3:
                        load_head(hh + 2)
            while pendB:
                tickB()
        S_.barrier(barscr)

        pc = contextlib.ExitStack()
        with pc:
            Wpa = sbt(pc, "Wpa", [128, 4, D], BF16)
            Wpb = sbt(pc, "Wpb", [128, 4, D], BF16)
            Wout = sbt(pc, "Wout", [128, 8, D], BF16)
            Wr = sbt(pc, "Wr", [128, 8, 36], F32)
            brB = sbt(pc, "brB", [128, 36], F32)
            oaC = [sbt(pc, "oaC%d" % i, [128, 4, 512], BF16) for i in range(2)]
            obC = [sbt(pc, "obC%d" % i, [128, 4, 512], BF16) for i in range(2)]
            gtC = [sbt(pc, "gtC%d" % i, [128, 16, 512], BF16) for i in range(2)]
            xc = [sbt(pc, "xc%d" % i, [128, D], F32) for i in range(2)]
            x1t = [sbt(pc, "x1t%d" % i, [128, D], F32) for i in range(2)]
            h2f = [sbt(pc, "h2f%d" % i, [128, D], F32) for i in range(4)]
            h2b = [sbt(pc, "h2b%d" % i, [128, D], BF16) for i in range(2)]
            h2T = [sbt(pc, "h2T%d" % i, [128, 8, 128], F32) for i in range(2)]
            mT = [sbt(pc, "mT%d" % i, [128, 8, 512], BF16) for i in range(2)]
            ga = [sbt(pc, "ga%d" % i, [128, 512], F32) for i in range(2)]
            gb_ = [sbt(pc, "gb%d" % i, [128, 512], F32) for i in range(2)]
            ss2 = sbt(pc, "ss2", [128, NT], F32)
            rs2 = sbt(pc, "rs2", [128, NT], F32)
            junk2 = sbt(pc, "junk2", [128, D], BF16)
            lg = [sbt(pc, "lg%d" % i, [128, 36], F32) for i in range(2)]
            rt = sbt(pc, "rt", [128, NT, 16], F32)
            msk = [sbt(pc, "msk%d" % i, [128, 32], F32) for i in range(2)]
            top8 = [sbt(pc, "top8%d" % i, [128, 8], F32) for i in range(2)]
            gex = [sbt(pc, "gex%d" % i, [128, 4], F32) for i in range(2)]
            Mb = [sbt(pc, "Mb%d" % i, [128, 32], BF16) for i in range(2)]
            lstrict = sbt(pc, "lstrict", [128, 128], BF16)
            rk = [sbt(pc, "rk%d" % i, [128, 32], F32) for i in range(2)]
            rj = [sbt(pc, "rj%d" % i, [128, 32], F32) for i in range(2)]

            A2 = sbt(pc, "A2c", [128, D], F32)
            B2 = sbt(pc, "B2c", [128, D], F32)
            Gm = sbt(pc, "Gmc", [128, D], F32)
            for q, tl in ((0, A2), (1, B2), (2, Gm)):
                S_.dma("sync", tl[:], MODS[q], [("MODS", q)], [(tl.name, 0), (tl.name, 1)])
            S_.dma("gpsimd", Wpa[:], wp_a.rearrange("(j p) n -> p j n", p=128), [], ["Wpa"])
            S_.dma("gpsimd", Wpb[:], wp_b.rearrange("(j p) n -> p j n", p=128), [], ["Wpb"])
            for k in range(8):
                S_.dma("sync", xc[k % 2][:], w_out[k * 128:(k + 1) * 128, :], [], [("xc", k % 2)])
                S_.stt(Wout[:, k, :], xc[k % 2][:], 0.5, Gm[:], ALU.mult, ALU.mult, [("xc", k % 2)] + full(Gm), [("Wout", k)])
            WOUT = [("Wout", k) for k in range(8)]
            S_.dma("sync", Wr[:], wr.rearrange("(k p) n -> p k n", p=128), [], ["Wr"])
            S_.dma("sync", brB[:], bcast_rows(br, 128), [], ["brB"])
            S_.memset("vector", ss2[:], 0.0, ["ss2"])
            S_.memset("vector", rt[:], 0.0, ["rt"])
            S_.memset("gpsimd", lstrict[:], 1.0, ["lstrict"])
            S_.op("gpsimd", lambda e: e.affine_select(out=lstrict[:], in_=lstrict[:], pattern=[[1, 128]],
                                                      compare_op=ALU.is_gt, fill=0.0, base=0, channel_multiplier=-1),
                  ["lstrict"], ["lstrict"])

            def load_chunk(c):
                if c >= NCH:
                    return
                S_.dma("sync", oaC[c % 2][:], OaT[:, c * 512:(c + 1) * 512].rearrange("(j p) n -> p j n", p=128),
                       [("OaT", c)], [("oaC", c % 2)])
                S_.dma("sync", obC[c % 2][:], ObT[:, c * 512:(c + 1) * 512].rearrange("(j p) n -> p j n", p=128),
                       [("ObT", c)], [("obC", c % 2)])
                S_.dma("sync", gtC[c % 2][:], GT[:, c * 512:(c + 1) * 512].rearrange("(m p) n -> p m n", p=128),
                       [("GT", c, q) for q in range(4)], [("gtC", c % 2)])

            def c_merge(c, m):
                pa_, pb_ = (0, 1) if m % 2 == 0 else (2, 3)
                for j in range(4):
                    S_.mm(psb[pa_][:], Wpa[:, j, m * 128:(m + 1) * 128], oaC[c % 2][:, j, :], j == 0, j == 3,
                          ["Wpa", ("oaC", c % 2)], [PS(pa_)])
                for j in range(4):
                    S_.mm(psb[pb_][:], Wpb[:, j, m * 128:(m + 1) * 128], obC[c % 2][:, j, :], j == 0, j == 3,
                          ["Wpb", ("obC", c % 2)], [PS(pb_)])
                S_.stt(ga[m % 2][:], gtC[c % 2][:, m, :], 1.0, psb[pa_][:], ALU.add, ALU.mult,
                       [PS(pa_), ("gtC", c % 2)], [("ga", m % 2)])
                S_.stt(gb_[m % 2][:], gtC[c % 2][:, 8 + m, :], 1.0, psb[pb_][:], ALU.add, ALU.mult,
                       [PS(pb_), ("gtC", c % 2)], [("gb", m % 2)])
                S_.tt("gpsimd", mT[c % 2][:, m, :], ga[m % 2][:], gb_[m % 2][:], ALU.add,
                      [("ga", m % 2), ("gb", m % 2)], [("mT", c % 2, m)])

            def c_W(c, t):
                i = 4 * c + t
                xb = xc[i % 2]
                S_.dma("sync", xb[:], x[i * 128:(i + 1) * 128, :], [], [("xc", i % 2)])
                next(conv, None)
                x1 = x1t[i % 2]
                for hf in range(2):
                    bank = 4 + hf
                    for m in range(8):
                        S_.mm(psb[bank][:], mT[c % 2][:, m, t * 128:(t + 1) * 128], Wout[:, m, hf * 512:(hf + 1) * 512],
                              m == 0, m == 7, [("mT", c % 2, m), ("Wout", m)], [PS(bank)])
                    S_.tt("vector", x1[:, hf * 512:(hf + 1) * 512], psb[bank][:], xb[:, hf * 512:(hf + 1) * 512],
                          ALU.add, [PS(bank), ("xc", i % 2)], [("x1t", i % 2, hf)])
                X1K = [("x1t", i % 2, 0), ("x1t", i % 2, 1)]
                S_.dma("gpsimd", X1[i * 128:(i + 1) * 128, :], x1[:], X1K, [("X1", i)])
                S_.act(junk2[:], x1[:], AF.Square, X1K + ["ss2"], ["junk2", ("ss2", i)], accum=ss2[:, i:i + 1])

            def c_W2(i):
                x1 = x1t[i % 2]
                X1K = [("x1t", i % 2, 0), ("x1t", i % 2, 1)]
                S_.ts("vector", rs2[:, i:i + 1], ss2[:, i:i + 1], 1.0 / D, EPS, ALU.mult, ALU.add, [("ss2", i)], [("rs2", i)])
                S_.act(rs2[:, i:i + 1], rs2[:, i:i + 1], AF.Ln, [("rs2", i)], [("rs2", i)])
                S_.act(rs2[:, i:i + 1], rs2[:, i:i + 1], AF.Exp, [("rs2", i)], [("rs2", i)], scale=-0.5)
                hf_ = h2f[i % 4]
                S_.stt(hf_[:], x1[:], rs2[:, i:i + 1], A2[:], ALU.mult, ALU.mult, X1K + [("rs2", i)] + full(A2), [("h2f", i % 4)])
                S_.tt("gpsimd", hf_[:], hf_[:], B2[:], ALU.add, [("h2f", i % 4)] + full(B2), [("h2f", i % 4)])
                S_.cp("gpsimd", h2b[i % 2][:].rearrange("t (k p) -> t k p", k=8),
                      hf_[:].rearrange("t (p k) -> t k p", k=8), [("h2f", i % 4)], [("h2b", i % 2)])
                S_.dma("gpsimd", H2P[i * 128:(i + 1) * 128, :], h2b[i % 2][:], [("h2b", i % 2)], [("H2P", i)])

            def c_R1(i):
                hf_ = h2f[i % 4]
                for rnd in range(2):
                    for kk in range(4):
                        k = rnd * 4 + kk
                        S_.tr(psb[6][:, kk * 128:(kk + 1) * 128], hf_[:, k * 128:(k + 1) * 128], identf[:],
                              [("h2f", i % 4), "identf"], [PS(6)])
                    S_.cp("scalar" if rnd == 0 else "vector", h2T[i % 2][:, rnd * 4:rnd * 4 + 4, :],
                          psb[6][:].rearrange("p (k n) -> p k n", k=4), [PS(6)], [("h2T", i % 2, rnd)])
                for k in range(8):
                    S_.mm(psb[7][:, 0:36], h2T[i % 2][:, k, :], Wr[:, k, :], k == 0, k == 7,
                          [("h2T", i % 2, k // 4), "Wr"], [PS(7)])
                L = lg[i % 2]
                LK = ("lg", i % 2)
                S_.tt("vector", L[:], psb[7][:, 0:36], brB[:], ALU.add, [PS(7), "brB"], [LK])
                RTK = ("rt", i)
                S_.rmax(rt[:, i, 0:1], L[:, 0:4], [LK, "rt"], [RTK])
                S_.ts("vector", rt[:, i, 1:2], rt[:, i, 0:1], -1.0, None, ALU.mult, None, [RTK], [RTK])
                S_.act(gex[i % 2][:], L[:, 0:4], AF.Exp, [LK, RTK], [("gex", i % 2), RTK], bias=rt[:, i, 1:2], accum=rt[:, i, 2:3])
                S_.recip(rt[:, i, 3:4], rt[:, i, 2:3], [RTK], [RTK])
                S_.ts("vector", gex[i % 2][:], L[:, 0:4], rt[:, i, 0:1], None, ALU.is_equal, None, [LK, RTK, ("gex", i % 2)], [("gex", i % 2)])
                S_.ts("vector", gex[i % 2][:], gex[i % 2][:], -1.0, 1e30, ALU.add, ALU.mult, [("gex", i % 2)], [("gex", i % 2)])
                mk = msk[i % 2]
                S_.tt("vector", mk[:].rearrange("p (g e) -> p g e", g=4), L[:, 4:36].rearrange("p (g e) -> p g e", g=4),
                      gex[i % 2][:].unsqueeze(2).to_broadcast([128, 4, 8]), ALU.add, [LK, ("gex", i % 2)], [("msk", i % 2)])
                S_.max8(top8[i % 2][:], mk[:], [("msk", i % 2)], [("top8", i % 2)])
                S_.ts("vector", M1[:, i, :], mk[:], top8[i % 2][:, 0:1], None, ALU.is_equal, None, [("msk", i % 2), ("top8", i % 2)], [("M1", i)])
                S_.ts("vector", M2[:, i, :], mk[:], top8[i % 2][:, 1:2], None, ALU.is_equal, None, [("msk", i % 2), ("top8", i % 2)], [("M2", i)])
                S_.tt("vector", rt[:, i, 4:5], top8[i % 2][:, 1:2], top8[i % 2][:, 0:1], ALU.subtract, [("top8", i % 2), RTK], [RTK])
                S_.act(rt[:, i, 5:6], rt[:, i, 4:5], AF.Exp, [RTK], [RTK])
                S_.ts("vector", rt[:, i, 6:7], rt[:, i, 5:6], 1.0, None, ALU.add, None, [RTK], [RTK])
                S_.recip(rt[:, i, 6:7], rt[:, i, 6:7], [RTK], [RTK])
                S_.tt("vector", W1[:, i:i + 1], rt[:, i, 6:7], rt[:, i, 3:4], ALU.mult, [RTK], [("W1", i)])
                S_.tt("vector", W2[:, i:i + 1], rt[:, i, 3:4], W1[:, i:i + 1], ALU.subtract, [RTK, ("W1", i)], [("W2", i)])
                mb = Mb[i % 2]
                S_.tt("vector", mb[:], M1[:, i, :], M2[:, i, :], ALU.add, [("M1", i), ("M2", i)], [("Mb", i % 2)])

            def c_R2(i):
                mb = Mb[i % 2]
                S_.mm(psb[7][:, 64:96], lstrict[:], mb[:], True, False, ["lstrict", ("Mb", i % 2)], [PS(7)])
                S_.mm(psb[7][:, 64:96], onesb[:], Macc[:], False, True, ["onesb", "Macc"], [PS(7)])
                S_.cp("vector", rk[i % 2][:], psb[7][:, 64:96], [PS(7)], [("rk", i % 2)])
                S_.tt("gpsimd", Macc[:], Macc[:], mb[:], ALU.add, ["Macc", ("Mb", i % 2)], ["Macc"])
                for (Mx, Rx, nm) in ((M1, R1, "R1"), (M2, R2, "R2")):
                    S_.tt("vector", rj[i % 2][:], Mx[:, i, :], rk[i % 2][:], ALU.mult,
                          [("M1" if nm == "R1" else "M2", i), ("rk", i % 2)], [("rj", i % 2)])
                    S_.rsum(Rx[:, i:i + 1], rj[i % 2][:], [("rj", i % 2)], [(nm, i)])

            pend = []

            def tick():
                due = [p_ for p_ in pend if p_[0] <= 0]
                for p_ in due:
                    pend.remove(p_)
                    p_[1](p_[2])
                for p_ in pend:
                    p_[0] -= 1

            load_chunk(0)
            for c in range(NCH):
                load_chunk(c + 1)
                for m in range(8):
                    c_merge(c, m)
                    tick()
                for t in range(4):
                    i = 4 * c + t
                    c_W(c, t)
                    tick()
                    pend.append([0, c_W2, i])
                    pend.append([2, c_R1, i])
                    pend.append([3, c_R2, i])
            while pend:
                tick()
        S_.barrier(barscr)

        pd = contextlib.ExitStack()
        with pd:
            cnt = sbt(pd, "cnt", [128, 32], F32)
            thr = sbt(pd, "thr", [128, 32], F32)
            cmp_ = sbt(pd, "cmp", [128, 32, 32], F32)
            ntl = sbt(pd, "ntl", [128, 32], F32)
            incl = sbt(pd, "incl", [128, 32], F32)
            base = sbt(pd, "base", [128, 32], F32)
            ones32 = sbt(pd, "ones32", [128, 32], F32)
            tio = sbt(pd, "tio", [128, NMT], F32)
            cmp2 = sbt(pd, "cmp2", [128, NMT, 32], F32)
            etf = sbt(pd, "etf", [128, NMT], F32)
            pio = sbt(pd, "pio", [128, 1], F32)
            widx = sbt(pd, "widx", [128, NMT], I32)
            posf = sbt(pd, "posf", [128, 2, NT], F32)
            posi = sbt(pd, "posi", [128, 2, NT], I32)
            mbig = sbt(pd, "mbig", [128, NT, 32], F32)
            h2g = [sbt(pd, "h2g%d" % i, [128, D], BF16) for i in range(4)]
            hs = [sbt(pd, "hs%d" % i, [128, D], BF16) for i in range(3)]
            hsT = [sbt(pd, "hsT%d" % i, [128, 8, 128], BF16) for i in range(2)]
            Wg_ = [sbt(pd, "Wg%d" % i, [128, 8, 256], BF16) for i in range(3)]
            Wu_ = [sbt(pd, "Wu%d" % i, [128, 8, 256], BF16) for i in range(3)]
            Wd_ = [sbt(pd, "Wd%d" % i, [128, 2, D], BF16) for i in range(3)]
            sa = [sbt(pd, "sa%d" % i, [128, 256], F32) for i in range(2)]
            hid = [sbt(pd, "hid%d" % i, [128, 256], BF16) for i in range(2)]
            hidT = [sbt(pd, "hidT%d" % i, [128, 2, 128], BF16) for i in range(2)]
            yt = [sbt(pd, "yt%d" % i, [128, D], F32) for i in range(2)]
            y1 = [sbt(pd, "y1_%d" % i, [128, D], F32) for i in range(3)]
            y2 = [sbt(pd, "y2_%d" % i, [128, D], F32) for i in range(3)]
            xf = [sbt(pd, "xf%d" % i, [128, D], F32) for i in range(3)]
            ssf = sbt(pd, "ssf", [128, NT], F32)
            rsf = sbt(pd, "rsf", [128, NT], F32)
            junk3 = sbt(pd, "junk3", [128, D], BF16)

            Gf = sbt(pd, "Gfd", [128, D], F32)
            Gfin = sbt(pd, "Gfin", [128, D], F32)
            S_.dma("sync", Gf[:], MODS[3], [("MODS", 3)], [("Gfd", 0), ("Gfd", 1)])
            S_.dma("sync", Gfin[:], bcast_rows(g_fin, 128), [], ["Gfin"])
            for _ in conv:
                pass
            WBK = [("WB", jn) for jn in range(96)]
            allM = [("M1", i) for i in range(NT)] + [("M2", i) for i in range(NT)]
            S_.mm(psb[0][:, 0:32], onesb[:], Macc[:], True, True, ["onesb", "Macc"], [PS(0)])
            S_.cp("vector", cnt[:], psb[0][:, 0:32], [PS(0)], ["cnt"])
            S_.op("gpsimd", lambda e: e.iota(thr[:], pattern=[[128, 32]], base=0, channel_multiplier=0,
                                             allow_small_or_imprecise_dtypes=True), [], ["thr"])
            S_.op("gpsimd", lambda e: e.iota(tio[:], pattern=[[1, NMT]], base=0, channel_multiplier=0,
                                             allow_small_or_imprecise_dtypes=True), [], ["tio"])
            S_.op("gpsimd", lambda e: e.iota(pio[:], pattern=[[0, 1]], base=0, channel_multiplier=1,
                                             allow_small_or_imprecise_dtypes=True), [], ["pio"])
            S_.memset("vector", ones32[:], 1.0, ["ones32"])
            S_.tt("vector", cmp_[:], cnt[:].unsqueeze(2).to_broadcast([128, 32, 32]),
                  thr[:].unsqueeze(1).to_broadcast([128, 32, 32]), ALU.is_gt, ["cnt", "thr"], ["cmp"])
            S_.rsum(ntl[:], cmp_[:], ["cmp"], ["ntl"])
            S_.op("vector", lambda e: e.tensor_tensor_scan(out=incl[:], data0=ones32[:], data1=ntl[:], initial=0.0,
                                                           op0=ALU.mult, op1=ALU.add), ["ones32", "ntl"], ["incl"])
            S_.tt("vector", base[:], incl[:], ntl[:], ALU.subtract, ["incl", "ntl"], ["base"])
            S_.ts("vector", base[:], base[:], 128.0, None, ALU.mult, None, ["base"], ["base"])
            S_.tt("vector", cmp2[:], incl[:].unsqueeze(1).to_broadcast([128, NMT, 32]),
                  tio[:].unsqueeze(2).to_broadcast([128, NMT, 32]), ALU.is_le, ["incl", "tio"], ["cmp2"])
            S_.rsum(etf[:], cmp2[:], ["cmp2"], ["etf"])
            S_.ts("vector", etf[:], etf[:], 128.0, None, ALU.mult, None, ["etf"], ["etf"])
            S_.ts("vector", etf[:], etf[:], pio[:, 0:1], None, ALU.add, None, ["etf", "pio"], ["etf"])
            S_.cp("vector", widx[:], etf[:], ["etf"], ["widx"])
            for q, (Mx, Rx, nm) in enumerate(((M1, R1, "R1"), (M2, R2, "R2"))):
                S_.tt("vector", mbig[:], Mx[:], base[:].unsqueeze(1).to_broadcast([128, NT, 32]), ALU.mult,
                      [("M1" if q == 0 else "M2", i) for i in range(NT)] + ["base"], ["mbig"])
                S_.rsum(posf[:, q, :], mbig[:], ["mbig"], [("posf", q)])
                S_.tt("vector", posf[:, q, :], posf[:, q, :], Rx[:], ALU.add, [("posf", q)] + [(nm, i) for i in range(NT)], [("posf", q)])
                S_.cp("vector", posi[:, q, :], posf[:, q, :], [("posf", q)], [("posi", q)])
            for i in range(NT):
                S_.dma("sync", h2g[i % 4][:], H2P[i * 128:(i + 1) * 128, :], [("H2P", i)], [("h2g", i % 4)])
                for q in range(2):
                    S_.op("gpsimd", (lambda i, q: lambda e: e.indirect_dma_start(
                        out=H2S, out_offset=bass.IndirectOffsetOnAxis(ap=posi[:, q, i:i + 1], axis=0),
                        in_=h2g[i % 4][:], in_offset=None))(i, q),
                        [("h2g", i % 4), ("posi", q)], [("H2S", i, q)], dma=True)
            H2SK = [("H2S", i, q) for i in range(NT) for q in range(2)]
            YSK = [("YS", t) for t in range(NMT)]
            def d_load(t):
                if t >= NMT:
                    return
                S_.dma("sync", hs[t % 3][:], H2S[t * 128:(t + 1) * 128, :], H2SK, [("hs", t % 3)])
                wb3 = t % 3
                for (Wt, src, nm) in ((Wg_, WXB[0], "Wg"), (Wu_, WXB[1], "Wu"), (Wd_, WXB[2], "Wd")):
                    dst = Wt[wb3][:].rearrange("p a f -> p (a f)")
                    S_.op("gpsimd", (lambda dst, src, t: lambda e: e.indirect_dma_start(
                        out=dst, out_offset=None, in_=src,
                        in_offset=bass.IndirectOffsetOnAxis(ap=widx[:, t:t + 1], axis=0),
                        bounds_check=S_.reg(e, 4095), oob_is_err=False))(dst, src, t),
                        ["widx"] + (WBK if t == 0 else []), [(nm, wb3)], dma=True)

            def d_trA(t):
                if t >= NMT:
                    return
                b = t % 2
                for k in range(8):
                    S_.tr(psbf[b][:, k * 128:(k + 1) * 128], hs[t % 3][:, k * 128:(k + 1) * 128], identb[:],
                          [("hs", t % 3), "identb"], [PS(b)])
                S_.cp("vector", hsT[b][:], psbf[b][:].rearrange("p (k n) -> p k n", k=8), [PS(b)], [("hsT", b)])

            def d_au(t):
                b = t % 2
                wb3 = t % 3
                bank = 2 + b
                for k in range(8):
                    S_.mm(psb[bank][:, 0:256], hsT[b][:, k, :], Wg_[wb3][:, k, :], k == 0, k == 7,
                          [("hsT", b), ("Wg", wb3)], [PS(bank)])
                for k in range(8):
                    S_.mm(psb[bank][:, 256:512], hsT[b][:, k, :], Wu_[wb3][:, k, :], k == 0, k == 7,
                          [("hsT", b), ("Wu", wb3)], [PS(bank)])
                S_.act(sa[b][:], psb[bank][:, 0:256], AF.Silu, [PS(bank)], [("sa", b)])
                S_.tt("vector", hid[b][:], sa[b][:], psb[bank][:, 256:512], ALU.mult, [("sa", b), PS(bank)], [("hid", b)])

            def d_down(t):
                if t < 0:
                    return
                b = t % 2
                wb3 = t % 3
                hv = hid[b][:].rearrange("s (p j) -> s j p", j=2)
                for j in range(2):
                    S_.tr(psbf[4][:, j * 128:(j + 1) * 128], hv[:, j, :], identb[:], [("hid", b), "identb"], [PS(4)])
                S_.cp("scalar", hidT[b][:], psbf[4][:, 0:256].rearrange("p (j n) -> p j n", j=2), [PS(4)], [("hidT", b)])
                for hf in range(2):
                    bk = 5 + hf
                    for j in range(2):
                        S_.mm(psb[bk][:], hidT[b][:, j, :], Wd_[wb3][:, j, hf * 512:(hf + 1) * 512], j == 0, j == 1,
                              [("hidT", b), ("Wd", wb3)], [PS(bk)])
                    S_.cp("scalar" if hf == 0 else "vector", yt[b][:, hf * 512:(hf + 1) * 512], psb[bk][:], [PS(bk)], [("yt", b, hf)])
                S_.dma("scalar", YS[t * 128:(t + 1) * 128, :], yt[b][:], [("yt", b, 0), ("yt", b, 1)], [("YS", t)])

            d_load(0)
            d_load(1)
            d_trA(0)
            for t in range(NMT):
                d_trA(t + 1)
                d_au(t)
                d_down(t - 1)
                d_load(t + 2)
            d_down(NMT - 1)
            S_.memset("vector", ssf[:], 0.0, ["ssf"])
            for i in range(NT):
                b = i % 3
                for q, yy in ((0, y1), (1, y2)):
                    S_.op("gpsimd", (lambda yy, q, i: lambda e: e.indirect_dma_start(
                        out=yy[i % 3][:], out_offset=None, in_=YS,
                        in_offset=bass.IndirectOffsetOnAxis(ap=posi[:, q, i:i + 1], axis=0)))(yy, q, i),
                        YSK + [("posi", q)], [("y%d" % q, b)], dma=True)
                S_.dma("sync", xf[b][:], X1[i * 128:(i + 1) * 128, :], [("X1", i)], [("xf", b)])
                S_.ts("vector", y1[b][:], y1[b][:], W1[:, i:i + 1], None, ALU.mult, None, [("y0", b), ("W1", i)], [("y0", b)])
                S_.stt(y1[b][:], y2[b][:], W2[:, i:i + 1], y1[b][:], ALU.mult, ALU.add, [("y1", b), ("y0", b), ("W2", i)], [("y0", b)])
                S_.tt("gpsimd", y1[b][:], y1[b][:], Gf[:], ALU.mult, [("y0", b)] + full(Gf), [("y0", b)])
                S_.tt("vector", xf[b][:], xf[b][:], y1[b][:], ALU.add, [("xf", b), ("y0", b)], [("xf", b)])
                S_.act(junk3[:], xf[b][:], AF.Square, [("xf", b), "ssf"], ["junk3", ("ssf", i)], accum=ssf[:, i:i + 1])
                S_.ts("vector", rsf[:, i:i + 1], ssf[:, i:i + 1], 1.0 / D, EPS, ALU.mult, ALU.add, [("ssf", i)], [("rsf", i)])
                S_.act(rsf[:, i:i + 1], rsf[:, i:i + 1], AF.Ln, [("rsf", i)], [("rsf", i)])
                S_.act(rsf[:, i:i + 1], rsf[:, i:i + 1], AF.Exp, [("rsf", i)], [("rsf", i)], scale=-0.5)
                S_.stt(y2[b][:], xf[b][:], rsf[:, i:i + 1], Gfin[:], ALU.mult, ALU.mult, [("xf", b), ("rsf", i), "Gfin", ("y1", b)], [("y1", b)])
                S_.dma("scalar", out[i * 128:(i + 1) * 128, :], y2[b][:], [("y1", b)], [("out", i)])
        S_.emit()
    return nc


def _t5_bucket_np(u):
    u = np.asarray(u)
    nf = np.maximum(u, 1).astype(np.float32)
    large = 16 + (np.log(nf / np.float32(16)) / np.float32(math.log(128 / 16)) * np.float32(16)).astype(np.int32)
    large = np.minimum(large, 31)
    return np.where(u < 16, u, large)


def make_in_maps(inputs, S, cores):
    f = lambda a: np.ascontiguousarray(np.asarray(a, dtype=np.float32))
    x = f(inputs["x"])
    c = f(inputs["c"])
    buck = _t5_bucket_np(np.arange(128))
    selb = np.zeros((32, 128), np.float32)
    selb[buck, np.arange(128)] = 1.0
    wr = np.concatenate([f(inputs["w_router_group"])[0]] + [f(inputs["w_router_expert"])[0, g] for g in range(4)], axis=1)
    br = np.concatenate([f(inputs["b_router_group"])[0].reshape(1, 4), f(inputs["b_router_expert"])[0].reshape(1, 32)], axis=1)
    shared = {
        "w_ada": f(inputs["w_ada"])[0], "b_ada": f(inputs["b_ada"])[0].reshape(1, -1),
        "g_mix": f(inputs["g_norm_mix"])[0].reshape(1, -1), "g_ffn": f(inputs["g_norm_ffn"])[0].reshape(1, -1),
        "g_fin": f(inputs["g_final"]).reshape(1, -1), "w_in": f(inputs["w_in"])[0],
        "sinks": f(inputs["sinks"])[0].reshape(1, 8), "b_forget": f(inputs["b_forget"])[0].reshape(1, 8),
        "rel_tab": f(inputs["rel_bias_table"]), "selb": selb,
        "wp_a": f(inputs["w_proj_swa"])[0], "wp_b": f(inputs["w_proj_fox"])[0], "w_out": f(inputs["w_out"])[0],
        "wr": f(wr), "br": f(br),
        "wg": f(inputs["w_gate_exp"])[0].reshape(4096, 2048), "wu": f(inputs["w_up_exp"])[0].reshape(4096, 2048),
        "wd": f(inputs["w_down_exp"])[0].reshape(4096, 2048),
    }
    maps = []
    for b in cores:
        m = dict(shared)
        m["x"] = np.ascontiguousarray(x[b, :S])
        m["ccol"] = np.ascontiguousarray(c[b].reshape(8, 128).T)
        maps.append(m)
    return maps


_NC_CACHE = {}


def kernel(**inputs):
    S = 4096
    if S not in _NC_CACHE:
        _NC_CACHE[S] = build_nc(S)
    nc = _NC_CACHE[S]
    in_maps = make_in_maps(inputs, S, list(range(8)))
    res = run_bass_kernel_spmd(nc, in_maps, core_ids=list(range(8)))
    return np.stack([np.asarray(r["out"], dtype=np.float32) for r in res.results], axis=0)
```

```python
import contextlib
import math
import numpy as np
import concourse.bass as bass
import concourse.mybir as mybir
from concourse.bass_utils import run_bass_kernel_spmd

F32 = mybir.dt.float32
BF16 = mybir.dt.bfloat16
I32 = mybir.dt.int32
AF = mybir.ActivationFunctionType
ALU = mybir.AluOpType
AX = mybir.AxisListType

D = 1024
EPS = 1e-6
NEG = -1e30


class Op:
    __slots__ = ("eng", "fn", "deps", "flag", "semval", "dma", "sem")

    def __init__(self, eng, fn, dma):
        self.eng = eng
        self.fn = fn
        self.deps = []
        self.flag = False
        self.semval = 0
        self.dma = dma
        self.sem = None


class Sched:
    ENGS = ["sync", "scalar", "vector", "gpsimd", "tensor"]

    def __init__(self, nc, n_dma_sems=48):
        self.nc = nc
        self.streams = {e: [] for e in self.ENGS}
        self.res = {}
        self.n_dma_sems = n_dma_sems
        self.dma_count = 0
        self.dma_count_sw = 0
        self.dma_last = [None] * n_dma_sems
        self.dma_uses = [0] * n_dma_sems
        self.nbar = 0

    def op(self, eng, fn, r=(), w=(), dma=False, extra=()):
        o = Op(eng, fn, dma)
        deps = set(extra)
        for k in r:
            st = self.res.get(k)
            if st is None:
                st = self.res[k] = [None, []]
            if st[0] is not None:
                deps.add(st[0])
        for k in w:
            st = self.res.get(k)
            if st is None:
                st = self.res[k] = [None, []]
            if st[0] is not None:
                deps.add(st[0])
            for rd in st[1]:
                deps.add(rd)
        if dma:
            half = self.n_dma_sems // 2
            if eng == "gpsimd":
                i = half + self.dma_count_sw % half
                self.dma_count_sw += 1
            else:
                i = self.dma_count % half
                self.dma_count += 1
            prev = self.dma_last[i]
            if prev is not None:
                deps.add(prev)
            self.dma_last[i] = o
            self.dma_uses[i] += 1
            o.sem = i
            o.semval = 16 * self.dma_uses[i]
        deps.discard(o)
        for d in deps:
            if d.dma:
                o.deps.append(d)
            elif d.eng == eng:
                if eng == "tensor":
                    continue
                o.deps.append(d)
                d.flag = True
            else:
                o.deps.append(d)
                d.flag = True
        for k in r:
            self.res[k][1].append(o)
        for k in w:
            st = self.res[k]
            st[0] = o
            st[1] = []
        self.streams[eng].append(o)
        return o

    def mm(self, out, lhsT, rhs, start, stop, r, w):
        return self.op("tensor", lambda e: e.matmul(out, lhsT=lhsT, rhs=rhs, start=start, stop=stop), r, w)

    def tr(self, out, in_, ident, r, w):
        return self.op("tensor", lambda e: e.transpose(out=out, in_=in_, identity=ident), r, w)

    def act(self, out, in_, func, r, w, bias=None, scale=None, accum=None):
        kw = {}
        if bias is not None:
            kw["bias"] = bias
        if scale is not None:
            kw["scale"] = scale
        if accum is not None:
            kw["accum_out"] = accum
        return self.op("scalar", lambda e: e.activation(out=out, in_=in_, func=func, **kw), r, w)

    def dma(self, eng, out, in_, r, w):
        return self.op(eng, lambda e: e.dma_start(out=out, in_=in_), r, w, dma=True)

    def tt(self, eng, out, in0, in1, op, r, w):
        return self.op(eng, lambda e: e.tensor_tensor(out=out, in0=in0, in1=in1, op=op), r, w)

    def ts(self, eng, out, in0, s1, s2, op0, op1, r, w):
        if op1 is None:
            return self.op(eng, lambda e: e.tensor_scalar(out=out, in0=in0, scalar1=s1, scalar2=None, op0=op0), r, w)
        return self.op(eng, lambda e: e.tensor_scalar(out=out, in0=in0, scalar1=s1, scalar2=s2, op0=op0, op1=op1), r, w)

    def stt(self, out, in0, scalar, in1, op0, op1, r, w):
        return self.op("vector", lambda e: e.scalar_tensor_tensor(out=out, in0=in0, scalar=scalar, in1=in1, op0=op0, op1=op1), r, w)

    def cp(self, eng, out, in_, r, w):
        if eng == "scalar":
            return self.op("scalar", lambda e: e.activation(out=out, in_=in_, func=AF.Copy), r, w)
        return self.op(eng, lambda e: e.tensor_copy(out=out, in_=in_), r, w)

    def memset(self, eng, ap, val, w):
        return self.op(eng, lambda e: e.memset(ap, val), (), w)

    def barrier(self, scratch):
        n = self.nbar
        self.nbar += 1
        outstanding = [o for o in self.dma_last if o is not None]
        pe_last = [self.streams["tensor"][-1]] if self.streams["tensor"] else []
        self.op("scalar", lambda eng: eng.activation(out=scratch[:, 0:1], in_=scratch[:, 15:16], func=AF.Copy),
                ["barscr"], [("bar", n, "scalar")])
        self.op("vector", lambda eng: eng.memset(scratch[:, 1:2], 0.0), ["barscr"], [("bar", n, "vector")])
        self.op("gpsimd", lambda eng: eng.memset(scratch[:, 2:3], 0.0), ["barscr"], [("bar", n, "gpsimd")], extra=outstanding)
        self.op("sync", lambda eng: eng.dma_start(out=scratch[:, 8:10], in_=scratch[:, 12:14]),
                ["barscr"], [("bar", n, "sync")], dma=True, extra=outstanding)
        allk = [("bar", n, e) for e in ["scalar", "vector", "gpsimd", "sync"]]
        self.op("scalar", lambda eng: eng.activation(out=scratch[:, 4:5], in_=scratch[:, 15:16], func=AF.Copy),
                allk, [("bar2", n, "scalar")], extra=pe_last)
        self.op("vector", lambda eng: eng.memset(scratch[:, 5:6], 0.0), allk, [("bar2", n, "vector")], extra=pe_last)
        self.op("gpsimd", lambda eng: eng.memset(scratch[:, 6:7], 0.0), allk, [("bar2", n, "gpsimd")], extra=pe_last)
        self.op("sync", lambda eng: eng.dma_start(out=scratch[:, 10:12], in_=scratch[:, 12:14]),
                allk, [("bar2", n, "sync")], dma=True, extra=pe_last)

    def reg(self, e, val):
        cache = self.__dict__.setdefault("_regs", {})
        if val not in cache:
            cache[val] = e.to_reg(val)
        return cache[val]

    def recip(self, out, in_, r, w):
        return self.op("vector", lambda e: e.reciprocal(out=out, in_=in_), r, w)

    def rmax(self, out, in_, r, w):
        return self.op("vector", lambda e: e.reduce_max(out=out, in_=in_, axis=AX.X), r, w)

    def rsum(self, out, in_, r, w):
        return self.op("vector", lambda e: e.reduce_sum(out=out, in_=in_, axis=AX.X), r, w)

    def max8(self, out, in_, r, w):
        return self.op("vector", lambda e: e.max(out=out, in_=in_), r, w)

    def emit(self):
        nc = self.nc
        for e in self.ENGS:
            c = 0
            for o in self.streams[e]:
                if o.dma:
                    continue
                if o.flag:
                    c += 1
                    o.semval = c
        with contextlib.ExitStack() as es:
            esem = {e: es.enter_context(nc.semaphore("c_" + e)) for e in self.ENGS}
            dsem = [es.enter_context(nc.semaphore("d_%d" % i)) for i in range(self.n_dma_sems)]
            block = es.enter_context(nc.Block())

            def run_stream(ename, eng):
                waited = {}
                for o in self.streams[ename]:
                    for d in o.deps:
                        if d.dma:
                            key = ("d", d.sem)
                            sem = dsem[d.sem]
                        else:
                            key = ("e", d.eng)
                            sem = esem[d.eng]
                        if waited.get(key, 0) >= d.semval:
                            continue
                        waited[key] = d.semval
                        eng.wait_ge(sem, d.semval)
                    ins = o.fn(eng)
                    if o.dma:
                        ins.then_inc(dsem[o.sem], 16)
                    elif o.flag:
                        ins.then_inc(esem[ename], 1)
                for o in self.streams[ename]:
                    if o.dma and self.dma_last[o.sem] is o:
                        if waited.get(("d", o.sem), 0) < o.semval:
                            eng.wait_ge(dsem[o.sem], o.semval)

            @block.sync
            def _(eng):
                run_stream("sync", eng)

            @block.scalar
            def _(eng):
                run_stream("scalar", eng)

            @block.vector
            def _(eng):
                run_stream("vector", eng)

            @block.gpsimd
            def _(eng):
                run_stream("gpsimd", eng)

            @block.tensor
            def _(eng):
                run_stream("tensor", eng)


def bcast_rows(ap, nparts):
    pat = [list(p) for p in ap.ap]
    return bass.AP(ap.tensor, ap.offset, [[0, nparts]] + pat[1:])


def build_nc(S, debug=False):
    NT = S // 128
    NCH = S // 512
    NMT = 2 * NT + 32
    nc = bass.Bass("TRN2", target_bir_lowering=False)

    def din(name, shape, dt=F32):
        return nc.dram_tensor(name, shape, dt, kind="ExternalInput").ap()

    x = din("x", [S, D])
    ccol = din("ccol", [128, 8])
    w_ada = din("w_ada", [D, 6 * D])
    b_ada = din("b_ada", [1, 6 * D])
    g_mix = din("g_mix", [1, D])
    g_ffn = din("g_ffn", [1, D])
    g_fin = din("g_fin", [1, D])
    w_in = din("w_in", [D, 4360])
    sinks = din("sinks", [1, 8])
    b_forget = din("b_forget", [1, 8])
    rel_tab = din("rel_tab", [32, 8])
    selb = din("selb", [32, 128])
    wp_a = din("wp_a", [512, D])
    wp_b = din("wp_b", [512, D])
    w_out = din("w_out", [D, D])
    wr = din("wr", [D, 36])
    br = din("br", [1, 36])
    wg = din("wg", [4096, 2048])
    wu = din("wu", [4096, 2048])
    wd = din("wd", [4096, 2048])
    out = nc.dram_tensor("out", [S, D], F32, kind="ExternalOutput").ap()

    def dscr(name, shape, dt):
        return nc.dram_tensor(name, shape, dt, kind="ExternalOutput" if debug else "Internal").ap()

    QB = dscr("QB", [8, 70, S], BF16)
    KB = dscr("KB", [8, 70, S], BF16)
    VB = dscr("VB", [8, 128, NT * 65], BF16)
    OaT = dscr("OaT", [512, S], BF16)
    ObT = dscr("ObT", [512, S], BF16)
    GT = dscr("GT", [2048, S], BF16)
    X1 = dscr("X1", [S, D], F32)
    H2P = dscr("H2P", [S, D], BF16)
    H2S = dscr("H2S", [NMT * 128, D], BF16)
    YS = dscr("YS", [NMT * 128, D], F32)
    L2 = dscr("L2", [8, 384], F32)
    MODS = dscr("MODS", [4, 128, D], F32)
    WXB = [nc.dram_tensor(nm, [4096, 2048], BF16, kind="Internal").ap() for nm in ("WGB", "WUB", "WDB")]

    S_ = Sched(nc)
    top = contextlib.ExitStack()
    with top:
        def sbt(stack, name, shape, dt):
            return stack.enter_context(nc.sbuf_tensor(name, shape, dt))

        identb = sbt(top, "identb", [128, 128], BF16)
        identf = sbt(top, "identf", [128, 128], F32)
        onesf = sbt(top, "onesf", [128, 128], F32)
        onesb = sbt(top, "onesb", [128, 128], BF16)
        barscr = sbt(top, "barscr", [128, 16], F32)
        M1 = sbt(top, "M1", [128, NT, 32], F32)
        M2 = sbt(top, "M2", [128, NT, 32], F32)
        W1 = sbt(top, "W1", [128, NT], F32)
        W2 = sbt(top, "W2", [128, NT], F32)
        R1 = sbt(top, "R1", [128, NT], F32)
        R2 = sbt(top, "R2", [128, NT], F32)
        Macc = sbt(top, "Macc", [128, 32], BF16)
        psb = [top.enter_context(nc.psum_tensor("psb%d" % i, [128, 512], F32)) for i in range(8)]
        NCB = 2
        cst = [sbt(top, "cst%d" % i, [128, 2048], BF16) for i in range(NCB)]

        def conv_gen():
            srcs = (wg, wu, wd)
            prev = None
            for jn in range(96):
                e_, m_ = jn // 3, jn % 3
                S_.dma("gpsimd", cst[jn % NCB][:], srcs[m_][e_ * 128:(e_ + 1) * 128, :], [], [("cst", jn % NCB)])
                if prev is not None:
                    pe_, pm_, pj = prev
                    S_.dma("sync", WXB[pm_][pe_ * 128:(pe_ + 1) * 128, :], cst[pj % NCB][:], [("cst", pj % NCB)], [("WB", pj)])
                prev = (e_, m_, jn)
                yield
            pe_, pm_, pj = prev
            S_.dma("sync", WXB[pm_][pe_ * 128:(pe_ + 1) * 128, :], cst[pj % NCB][:], [("cst", pj % NCB)], [("WB", pj)])
            yield

        conv = conv_gen()

        def PS(i):
            return ("ps", i)

        S_.memset("vector", barscr[:], 0.0, ["barscr"])
        S_.memset("gpsimd", identb[:], 1.0, ["identb"])
        S_.op("gpsimd", lambda e: e.affine_select(out=identb[:], in_=identb[:], pattern=[[-1, 128]],
                                                  compare_op=ALU.is_equal, fill=0.0, base=0, channel_multiplier=1),
              ["identb"], ["identb"])
        S_.memset("gpsimd", identf[:], 1.0, ["identf"])
        S_.op("gpsimd", lambda e: e.affine_select(out=identf[:], in_=identf[:], pattern=[[-1, 128]],
                                                  compare_op=ALU.is_equal, fill=0.0, base=0, channel_multiplier=1),
              ["identf"], ["identf"])
        S_.memset("vector", onesf[:], 1.0, ["onesf"])
        S_.memset("vector", onesb[:], 1.0, ["onesb"])
        S_.memset("vector", Macc[:], 0.0, ["Macc"])

        pab = contextlib.ExitStack()
        A1 = sbt(pab, "A1", [128, D], F32)
        B1 = sbt(pab, "B1", [128, D], F32)
        biasT = sbt(pab, "biasT", [128, 8, 256], F32)
        Win = sbt(pab, "Win", [128, 8, 4360], BF16)
        Wka2 = sbt(pab, "Wka2", [128, 8, 128], BF16)
        w_in_v = w_in.rearrange("(k p) n -> p k n", p=128)
        for (c0, c1) in ((0, 2048), (2048, 4096), (4096, 4360)):
            S_.dma("gpsimd", Win[:, :, c0:c1], w_in_v[:, :, c0:c1], [], [("Win", c0)])
        WIN = [("Win", 0), ("Win", 2048), ("Win", 4096)]
        WKA2 = ["Wka2a", "Wka2b"]
        pro = contextlib.ExitStack()
        with pro:
            cact = sbt(pro, "cact", [128, 8], F32)
            A2 = sbt(pro, "A2", [128, D], F32)
            B2 = sbt(pro, "B2", [128, D], F32)
            Gm = sbt(pro, "Gm", [128, D], F32)
            Gf = sbt(pro, "Gf", [128, D], F32)
            CB = sbt(pro, "CB", [128, 8, 128], F32)
            wa = [sbt(pro, "wa%d" % i, [128, 8, 512], F32) for i in range(2)]
            badaB = sbt(pro, "badaB", [128, 6 * D], F32)
            gmixB = sbt(pro, "gmixB", [128, D], F32)
            gffnB = sbt(pro, "gffnB", [128, D], F32)
            S_.dma("sync", cact[:], ccol, [], ["cact"])
            S_.dma("sync", badaB[:], bcast_rows(b_ada, 128), [], ["badaB"])
            S_.dma("sync", gmixB[:], bcast_rows(g_mix, 128), [], ["gmixB"])
            S_.dma("sync", gffnB[:], bcast_rows(g_ffn, 128), [], ["gffnB"])
            S_.act(cact[:], cact[:], AF.Silu, ["cact"], ["cact"])
            for k in range(8):
                S_.cp("vector", CB[:, k, :], cact[:, k:k + 1].to_broadcast([128, 128]), ["cact"], [("CB", k)])
            dests = [B1, A1, Gm, B2, A2, Gf]
            w_ada_v = w_ada.rearrange("(k p) n -> p k n", p=128)
            for cc in range(12):
                wb = wa[cc % 2]
                S_.dma("sync", wb[:], w_ada_v[:, :, cc * 512:(cc + 1) * 512], [], [("wa", cc % 2)])
                pb = psb[cc % 2]
                for k in range(8):
                    S_.mm(pb[:], CB[:, k, :], wb[:, k, :], k == 0, k == 7,
                          [("CB", k), ("wa", cc % 2)], [PS(cc % 2)])
                dst = dests[cc // 2]
                S_.tt("vector", dst[:, (cc % 2) * 512:(cc % 2 + 1) * 512], pb[:], badaB[:, cc * 512:(cc + 1) * 512],
                      ALU.add, [PS(cc % 2), "badaB"], [(dst.name, cc % 2)])
            for (At, gB) in ((A1, gmixB), (A2, gffnB)):
                for hh in range(2):
                    sl = slice(hh * 512, (hh + 1) * 512)
                    S_.stt(At[:, sl], At[:, sl], 1.0, gB[:, sl], ALU.add, ALU.mult,
                           [(At.name, hh), gB.name], [(At.name, hh)])
            for q, tl in enumerate((A2, B2, Gm, Gf)):
                S_.dma("sync", MODS[q], tl[:], [(tl.name, 0), (tl.name, 1)], [("MODS", q)])
            bt = contextlib.ExitStack()
            with bt:
                relsb = sbt(bt, "relsb", [32, 8], F32)
                selsb = sbt(bt, "selsb", [32, 128], F32)
                tvec = sbt(bt, "tvec", [128, 8], F32)
                line = sbt(bt, "line", [8, 384], F32)
                antiJ = sbt(bt, "antiJ", [128, 2, 256], F32)
                brT = sbt(bt, "brT", [128, 2, 128], F32)
                S_.dma("sync", relsb[:], rel_tab, [], ["relsb"])
                S_.dma("sync", selsb[:], selb, [], ["selsb"])
                S_.mm(psb[2][:, 0:8], selsb[:], relsb[:], True, True, ["relsb", "selsb"], [PS(2)])
                S_.cp("vector", tvec[:], psb[2][:, 0:8], [PS(2)], ["tvec"])
                S_.op("tensor", lambda e: e.transpose(out=psb[3][0:8, 0:128], in_=tvec[:], identity=identf[:]),
                      ["tvec", "identf"], [PS(3)])
                S_.memset("vector", line[:], NEG, ["line"])
                S_.cp("vector", line[:, 127:255], psb[3][0:8, 0:128], [PS(3), "line"], ["line"])
                S_.dma("sync", L2, line[:], ["line"], ["L2"])
                S_.memset("gpsimd", antiJ[:], 1.0, ["antiJ"])
                for hf in range(2):
                    S_.op("gpsimd", (lambda hf: lambda e: e.affine_select(
                        out=antiJ[:, hf, :], in_=antiJ[:, hf, :], pattern=[[1, 256]],
                        compare_op=ALU.is_equal, fill=0.0, base=hf * 128 - 255, channel_multiplier=1))(hf),
                        ["antiJ"], ["antiJ"])
                for h in range(8):
                    for hf in range(2):
                        src = bass.AP(L2.tensor, h * 384 + hf * 128, [[1, 128], [1, 128]])
                        S_.dma("sync", brT[:, hf, :], src, ["L2"], [("brT", hf)])
                    for hf in range(2):
                        S_.mm(psb[4][:, 0:256], brT[:, hf, :], antiJ[:, hf, :], hf == 0, hf == 1,
                              [("brT", hf), "antiJ"], [PS(4)])
                    S_.cp("vector", biasT[:, h, :], psb[4][:, 0:256], [PS(4)], [("biasT", h)])

        S_.barrier(barscr)

        def full(t):
            return [(t.name, 0), (t.name, 1)]

        pa = contextlib.ExitStack()
        with pa:
            xt = [sbt(pa, "xt%d" % i, [128, D], F32) for i in range(2)]
            tmpf = [sbt(pa, "tmpf%d" % i, [128, D], F32) for i in range(1)]
            hb = [sbt(pa, "hb%d" % i, [128, D], BF16) for i in range(2)]
            hT = [sbt(pa, "hT%d" % i, [128, 8, 512], BF16) for i in range(1)]
            ssA = sbt(pa, "ssA", [128, NT], F32)
            rsA = sbt(pa, "rsA", [128, NT], F32)
            junk = sbt(pa, "junk", [128, D], BF16)
            Qtm = [sbt(pa, "Qtm%d" % i, [128, 8, 70], BF16) for i in range(2)]
            Ktm = [sbt(pa, "Ktm%d" % i, [128, 8, 70], BF16) for i in range(2)]
            QBst = [sbt(pa, "QBst%d" % i, [70, 8, 512], BF16) for i in range(1)]
            KBst = [sbt(pa, "KBst%d" % i, [70, 8, 512], BF16) for i in range(1)]
            Vst = [sbt(pa, "Vst%d" % i, [128, 8, 4, 65], BF16) for i in range(2)]
            Va = sbt(pa, "Va", [128, 12, 128], BF16)
            QTa = [sbt(pa, "QTa%d" % i, [128, 4, 512], BF16) for i in range(2)]
            KTa = sbt(pa, "KTa", [128, 2, 1536], BF16)
            Gst = [sbt(pa, "Gst%d" % i, [128, 4, 512], BF16) for i in range(2)]
            bfB = sbt(pa, "bfB", [128, 8], F32)
            sinkB = sbt(pa, "sinkB", [128, 8], F32)
            carryB = sbt(pa, "carryB", [128, 8], F32)
            tri = sbt(pa, "tri", [128, 128], F32)
            fz = [sbt(pa, "fz%d" % i, [128, 8], F32) for i in range(2)]
            cumt = [sbt(pa, "cumt%d" % i, [128, 8], F32) for i in range(2)]
            r1t = [sbt(pa, "r1t%d" % i, [128, 8], F32) for i in range(2)]
            ssb = [sbt(pa, "ssb%d" % i, [128, 256], F32) for i in range(5)]
            pbf = [sbt(pa, "pbf%d" % i, [128, 256], BF16) for i in range(5)]
            pTs = [sbt(pa, "pTs%d" % i, [128, 2, 128], BF16) for i in range(5)]
            swst = sbt(pa, "swst", [128, NT * 8, 6], F32)
            rinvA = [sbt(pa, "rinvA%d" % i, [128, 8], F32) for i in range(2)]
            Oatm = [sbt(pa, "Oatm%d" % i, [128, 512], BF16) for i in range(2)]
            OaTst = [sbt(pa, "OaTst%d" % i, [128, 4, 512], BF16) for i in range(1)]

            S_.cp("vector", Wka2[:, :, 0:64], Win[:, :, 576:640], WIN, ["Wka2a"])
            S_.cp("vector", Wka2[:, :, 64:128], Win[:, :, 512:576], WIN, ["Wka2b"])
            S_.dma("sync", bfB[:], bcast_rows(b_forget, 128), [], ["bfB"])
            S_.dma("sync", sinkB[:], bcast_rows(sinks, 128), [], ["sinkB"])
            S_.memset("vector", carryB[:], 0.0, ["carryB"])
            S_.memset("vector", ssA[:], 0.0, ["ssA"])
            S_.memset("vector", swst[:], 0.0, ["swst"])
            S_.memset("gpsimd", tri[:], 1.0, ["tri"])
            S_.op("gpsimd", lambda e: e.affine_select(out=tri[:], in_=tri[:], pattern=[[1, 128]],
                                                      compare_op=ALU.is_ge, fill=0.0, base=0, channel_multiplier=-1),
                  ["tri"], ["tri"])
            for i in range(2):
                S_.memset("vector", Qtm[i][:], 1.0, [("Qtm", i)])
                S_.memset("gpsimd", Ktm[i][:], 1.0, [("Ktm", i)])
                S_.memset("gpsimd", Vst[i][:], 1.0, [("Vst", i)])

            scale_q = 0.125
            psbf = [p[:].bitcast(BF16) for p in psb]

            def emit_H(i):
                if i >= NT:
                    return
                xb = xt[i % 2]
                S_.dma("sync", xb[:], x[i * 128:(i + 1) * 128, :], [], [("xt", i % 2)])
                if i < 32:
                    next(conv, None)
                S_.act(junk[:], xb[:], AF.Square, [("xt", i % 2), "ssA"], ["junk", ("ss", i)], accum=ssA[:, i:i + 1])
                S_.ts("vector", rsA[:, i:i + 1], ssA[:, i:i + 1], 1.0 / D, EPS, ALU.mult, ALU.add, [("ss", i)], [("rs", i)])
                S_.act(rsA[:, i:i + 1], rsA[:, i:i + 1], AF.Ln, [("rs", i)], [("rs", i)])
                S_.act(rsA[:, i:i + 1], rsA[:, i:i + 1], AF.Exp, [("rs", i)], [("rs", i)], scale=-0.5)
                tf = tmpf[0]
                S_.stt(tf[:], xb[:], rsA[:, i:i + 1], A1[:], ALU.mult, ALU.mult,
                       [("xt", i % 2), ("rs", i)] + full(A1), [("tmpf", 0)])
                S_.tt("gpsimd", hb[i % 2][:], tf[:], B1[:], ALU.add, [("tmpf", 0)] + full(B1), [("hb", i % 2)])

            def emit_T(c, t):
                i = 4 * c + t
                h_ = hb[i % 2]
                for k in range(8):
                    S_.tr(psbf[0][:, k * 128:(k + 1) * 128], h_[:, k * 128:(k + 1) * 128], identb[:],
                          [("hb", i % 2), "identb"], [PS(0)])
                S_.cp("scalar" if t % 2 == 0 else "vector", hT[0][:, :, t * 128:(t + 1) * 128],
                      psbf[0][:].rearrange("p (k n) -> p k n", k=8), [PS(0)], [("hT", 0, t)])

            def emit_P(c, t):
                i = 4 * c + t
                hTc = hT[0]
                rhT = [("hT", 0, t)]

                def tokproj(ps_ap, key, c0, c1):
                    for k in range(8):
                        S_.mm(ps_ap, hTc[:, k, t * 128:(t + 1) * 128], Win[:, k, c0:c1],
                              k == 0, k == 7, rhT + WIN, [key])
                q_ = Qtm[i % 2]
                k_ = Ktm[i % 2]
                f_ = fz[i % 2]
                tokproj(psb[3][:, 0:8], PS(3), 2304, 2312)
                S_.tt("vector", f_[:], psb[3][:, 0:8], bfB[:], ALU.add, [PS(3), "bfB"], [("fz", i % 2)])
                S_.act(f_[:], f_[:], AF.Exp, [("fz", i % 2)], [("fz", i % 2)], scale=-1.0)
                S_.act(f_[:], f_[:], AF.Ln, [("fz", i % 2)], [("fz", i % 2)], bias=1.0)
                tokproj(psb[1][:], PS(1), 768, 1280)
                S_.act(q_[:, :, 0:64], psb[1][:].rearrange("p (h d) -> p h d", h=8), AF.Copy,
                       [PS(1)], [("Qtm", i % 2)], scale=scale_q)
                tokproj(psb[2][:], PS(2), 1280, 1792)
                S_.cp("vector", k_[:, :, 0:64], psb[2][:].rearrange("p (h d) -> p h d", h=8), [PS(2)], [("Ktm", i % 2)])
                tokproj(psb[3][:], PS(3), 1792, 2304)
                vs = Vst[c % 2]
                S_.cp("scalar", vs[:, :, t, 0:64], psb[3][:].rearrange("p (h d) -> p h d", h=8), [PS(3)], [("Vst", c % 2)])
                tokproj(psb[1][:, 0:128], PS(1), 640, 768)
                S_.cp("vector", Va[:, i % 12, :], psb[1][:, 0:128], [PS(1)], [("Va", i % 12)])
                S_.mm(psb[2][:, 0:8], tri[:], f_[:], True, True, ["tri", ("fz", i % 2)], [PS(2)])
                S_.mm(psb[2][:, 8:16], onesf[:], f_[:], True, True, ["onesf", ("fz", i % 2)], [PS(2)])
                cm = cumt[i % 2]
                S_.tt("vector", cm[:], carryB[:], psb[2][:, 0:8], ALU.subtract, ["carryB", PS(2)], [("cumt", i % 2)])
                S_.tt("vector", carryB[:], carryB[:], psb[2][:, 8:16], ALU.subtract, ["carryB", PS(2)], ["carryB"])
                r1 = r1t[i % 2]
                S_.cp("vector", q_[:, :, 64], cm[:], [("cumt", i % 2)], [("Qtm", i % 2)])
                S_.tt("vector", r1[:], cm[:], q_[:, :, 64], ALU.subtract, [("cumt", i % 2), ("Qtm", i % 2)], [("r1t", i % 2)])
                S_.cp("vector", q_[:, :, 65], r1[:], [("r1t", i % 2)], [("Qtm", i % 2)])
                S_.tt("vector", r1[:], r1[:], q_[:, :, 65], ALU.subtract, [("r1t", i % 2), ("Qtm", i % 2)], [("r1t", i % 2)])
                S_.cp("vector", q_[:, :, 66], r1[:], [("r1t", i % 2)], [("Qtm", i % 2)])
                S_.ts("vector", k_[:, :, 67:70], q_[:, :, 64:67], -1.0, None, ALU.mult, None,
                      [("Qtm", i % 2)], [("Ktm", i % 2)])

            def emit_C2(c, t):
                i = 4 * c + t
                q_ = Qtm[i % 2]
                k_ = Ktm[i % 2]
                for (src, dstst, key, bank, ceng) in ((q_, QBst[0], "QBst", 4, "scalar"), (k_, KBst[0], "KBst", 6, "vector")):
                    for h in range(8):
                        S_.tr(psbf[bank][0:70, h * 128:(h + 1) * 128], src[:, h, :], identb[:],
                              [("Qtm" if key == "QBst" else "Ktm", i % 2), "identb"], [PS(bank)])
                    S_.cp(ceng, dstst[:, :, t * 128:(t + 1) * 128],
                          psbf[bank][0:70, :].rearrange("p (h n) -> p h n", h=8), [PS(bank)], [(key, 0)])

            def emit_chunk_stores(c):
                for h in range(8):
                    pass
                S_.dma("gpsimd", QB[:, :, c * 512:(c + 1) * 512].rearrange("h r n -> r h n"), QBst[0][:],
                       [("QBst", 0)], [("QB", c)])
                S_.dma("gpsimd", KB[:, :, c * 512:(c + 1) * 512].rearrange("h r n -> r h n"), KBst[0][:],
                       [("KBst", 0)], [("KB", c)])
                S_.dma("gpsimd", VB[:, :, c * 260:(c + 1) * 260].rearrange("h p n -> p h n"),
                       Vst[c % 2][:].rearrange("p h t d -> p h (t d)"), [("Vst", c % 2)], [("VB", c)])

            def emit_featgroups(c, swa_gen):
                hTc = hT[0]
                rh = [("hT", 0, t) for t in range(4)]
                groups = []
                for j in range(4):
                    groups.append(("qa", j))
                groups.append(("ka", 0))
                groups.append(("ka", 1))
                for m in range(16):
                    groups.append(("g", m))
                for gi, (kind, j) in enumerate(groups):
                    bank = 1 + gi % 3
                    if kind == "qa":
                        lw = lambda k: Win[:, k, j * 128:(j + 1) * 128]
                        rw = WIN
                    elif kind == "ka":
                        lw = (lambda k: Win[:, k, 512:640]) if j == 0 else (lambda k: Wka2[:, k, :])
                        rw = WIN if j == 0 else WKA2
                    else:
                        lw = lambda k: Win[:, k, 2312 + j * 128:2312 + (j + 1) * 128]
                        rw = WIN
                    for k in range(8):
                        S_.mm(psb[bank][:], lw(k), hTc[:, k, :], k == 0, k == 7, rh + rw, [PS(bank)])
                    if kind == "qa":
                        S_.act(QTa[c % 2][:, j, :], psb[bank][:], AF.Copy, [PS(bank)], [("QTa", c % 2, j)], scale=scale_q)
                    elif kind == "ka":
                        S_.cp("vector", KTa[:, j, (c % 3) * 512:(c % 3 + 1) * 512], psb[bank][:], [PS(bank)], [("KTa", j, c % 3)])
                    else:
                        gb = Gst[(j // 4) % 2]
                        S_.act(gb[:, j % 4, :], psb[bank][:], AF.Tanh, [PS(bank)], [("Gst", (j // 4) % 2)], scale=0.5)
                        if j % 4 == 3:
                            S_.dma("gpsimd", GT[(j - 3) * 128:(j + 1) * 128, c * 512:(c + 1) * 512].rearrange("(m p) n -> p m n", p=128),
                                   gb[:], [("Gst", (j // 4) % 2)], [("GT", c, j // 4)])
                    if swa_gen is not None:
                        for _ in range(5):
                            next(swa_gen, None)

            def swa_chunk(c):
                units = [(t, hq) for t in range(4) for hq in range(8)]
                st1 = {}

                def stage1(u):
                    t, hq = units[u]
                    i = 4 * c + t
                    j, b = hq // 2, 64 * (hq % 2)
                    kv = hq // 4
                    var = 0 if (kv == 0) == (b == 0) else 1
                    nk = 256 if i > 0 else 128
                    k0 = (i - 1) * 128 if i > 0 else 0
                    sl = u % 5
                    tiles_ = ([i - 1] if i > 0 else []) + [i]
                    for bk, ti in enumerate(tiles_):
                        off = ((ti // 4) % 3) * 512 + (ti % 4) * 128
                        S_.mm(psb[5][:, bk * 128:(bk + 1) * 128], QTa[c % 2][b:b + 64, j, t * 128:(t + 1) * 128],
                              KTa[b:b + 64, var, off:off + 128], True, True,
                              [("QTa", c % 2, j), ("KTa", var, (ti // 4) % 3)], [PS(5)])
                    col = i * 8 + hq
                    S_.tt("vector", ssb[sl][:, 0:nk], psb[5][:, 0:nk], biasT[:, hq, 256 - nk:256], ALU.add,
                          [PS(5), ("biasT", hq)], [("ssb", sl)])
                    S_.rmax(swst[:, col, 0:1], ssb[sl][:, 0:nk], [("ssb", sl), "swst"], [("swst", col)])
                    S_.ts("vector", swst[:, col, 2:3], swst[:, col, 0:1], sinkB[:, hq:hq + 1], -1.0, ALU.max, ALU.mult,
                          [("swst", col), "sinkB"], [("swst", col)])
                    S_.act(pbf[sl][:, 0:nk], ssb[sl][:, 0:nk], AF.Exp, [("ssb", sl), ("swst", col)],
                           [("pbf", sl), ("swst", col)], bias=swst[:, col, 2:3], accum=swst[:, col, 3:4])
                    S_.act(swst[:, col, 4:5], sinkB[:, hq:hq + 1], AF.Exp, ["sinkB", ("swst", col)], [("swst", col)],
                           bias=swst[:, col, 2:3])
                    st1[u] = (nk, sl)

                def stage2(u):
                    t, hq = units[u]
                    nk, sl = st1[u]
                    nb = nk // 128
                    i = 4 * c + t
                    col = i * 8 + hq
                    S_.tt("vector", swst[:, col, 5:6], swst[:, col, 3:4], swst[:, col, 4:5], ALU.add,
                          [("swst", col)], [("swst", col)])
                    S_.recip(rinvA[i % 2][:, hq:hq + 1], swst[:, col, 5:6], [("swst", col)], [("rinvA", i % 2, hq)])
                    for bk in range(nb):
                        S_.tr(psbf[6][:, bk * 128:(bk + 1) * 128], pbf[sl][:, bk * 128:(bk + 1) * 128], identb[:],
                              [("pbf", sl), "identb"], [PS(6)])
                    S_.cp("scalar" if u % 2 else "vector", pTs[sl][:, 0:nb, :],
                          psbf[6][:, 0:nb * 128].rearrange("p (b n) -> p b n", b=nb), [PS(6)], [("pTs", sl)])

                def stage3(u):
                    t, hq = units[u]
                    i = 4 * c + t
                    nk, sl = st1[u]
                    nb = nk // 128
                    kv = hq // 4
                    for bk in range(nb):
                        ktile = i - (nb - 1) + bk
                        S_.mm(psb[7][:, hq * 64:(hq + 1) * 64], pTs[sl][:, bk, :], Va[:, ktile % 12, kv * 64:(kv + 1) * 64],
                              bk == 0, bk == nb - 1, [("pTs", sl), ("Va", ktile % 12)], [("ps7", hq)])
                    if hq == 7:
                        oa = Oatm[i % 2]
                        S_.tt("vector", oa[:].rearrange("p (h d) -> p h d", h=8),
                              psb[7][:].rearrange("p (h d) -> p h d", h=8),
                              rinvA[i % 2][:].unsqueeze(2).to_broadcast([128, 8, 64]), ALU.mult,
                              [("ps7", h) for h in range(8)] + [("rinvA", i % 2, h) for h in range(8)], [("Oatm", i % 2)])
                        for j in range(4):
                            S_.tr(psbf[4][:, j * 128:(j + 1) * 128], oa[:, j * 128:(j + 1) * 128], identb[:],
                                  [("Oatm", i % 2), "identb"], [PS(4)])
                        S_.cp("scalar", OaTst[0][:, :, t * 128:(t + 1) * 128],
                              psbf[4][:, 0:512].rearrange("p (j n) -> p j n", j=4), [PS(4)], [("OaTst", 0)])
                        if t == 3:
                            S_.dma("gpsimd", OaT[:, c * 512:(c + 1) * 512].rearrange("(j p) n -> p j n", p=128),
                                   OaTst[0][:], [("OaTst", 0)], [("OaT", c)])

                n = len(units)
                for step in range(n + 4):
                    if step < n:
                        stage1(step)
                        yield
                    if 0 <= step - 2 < n:
                        stage2(step - 2)
                        yield
                    if 0 <= step - 4 < n:
                        stage3(step - 4)
                        yield

            prev_swa = None
            emit_H(0)
            for c in range(NCH):
                for t in range(4):
                    emit_T(c, t)
                    emit_H(4 * c + t + 1)
                    if t > 0:
                        emit_C2(c, t - 1)
                    emit_P(c, t)
                emit_C2(c, 3)
                emit_chunk_stores(c)
                emit_featgroups(c, prev_swa)
                if prev_swa is not None:
                    for _ in prev_swa:
                        pass
                prev_swa = swa_chunk(c)
            for _ in prev_swa:
                pass
        S_.barrier(barscr)
        pab.close()

        pbx = contextlib.ExitStack()
        with pbx:
            KBh = [sbt(pbx, "KBh%d" % i, [70, S], BF16) for i in range(2)]
            QBh = [sbt(pbx, "QBh%d" % i, [70, S], BF16) for i in range(2)]
            VBh = [sbt(pbx, "VBh%d" % i, [128, NT * 65], BF16) for i in range(2)]
            PTt = [sbt(pbx, "PTt%d" % i, [128, 512], BF16) for i in range(6)]
            cmask = sbt(pbx, "cmask", [128, 128], BF16)
            otf = [sbt(pbx, "otf%d" % i, [64, 512], F32) for i in range(2)]
            rinv = [sbt(pbx, "rinv%d" % i, [65, 512], F32) for i in range(2)]
            obst = [sbt(pbx, "obst%d" % i, [64, 512], BF16) for i in range(2)]
            S_.memset("gpsimd", cmask[:], -30000.0, ["cmask"])
            S_.op("gpsimd", lambda e: e.affine_select(out=cmask[:], in_=cmask[:], pattern=[[-1, 128]],
                                                      compare_op=ALU.is_gt, fill=0.0, base=0, channel_multiplier=1),
                  ["cmask"], ["cmask"])
            units = []
            for h in range(8):
                for c in range(NCH):
                    for kt in range(4 * c + 4):
                        units.append((h, c, kt))
            loaded = set()
            STB = [0, 1, 2, 3, 4]

            def load_head(h):
                if h in loaded or h >= 8:
                    return
                loaded.add(h)
                S_.dma("sync", KBh[h % 2][:], KB[h], [("KB", c) for c in range(NCH)], [("KBh", h % 2)])
                S_.dma("sync", QBh[h % 2][:], QB[h], [("QB", c) for c in range(NCH)], [("QBh", h % 2)])
                S_.dma("sync", VBh[h % 2][:], VB[h], [("VB", c) for c in range(NCH)], [("VBh", h % 2)])

            def st_mm(u):
                h, c, kt = units[u]
                j = kt - 4 * c
                q0 = 128 * j if j > 0 else 0
                bank = STB[u % 5]
                S_.mm(psb[bank][:, q0:512], KBh[h % 2][:, kt * 128:(kt + 1) * 128],
                      QBh[h % 2][:, c * 512 + q0:(c + 1) * 512], True, j < 0,
                      [("KBh", h % 2), ("QBh", h % 2)], [PS(bank)])
                if j >= 0:
                    S_.mm(psb[bank][:, q0:q0 + 128], identb[:], cmask[:], False, True, ["identb", "cmask"], [PS(bank)])

            def exp_pv(u):
                h, c, kt = units[u]
                j = kt - 4 * c
                q0 = 128 * j if j > 0 else 0
                bank = STB[u % 5]
                pt = PTt[u % 6]
                S_.act(pt[:, q0:512], psb[bank][:, q0:512], AF.Exp, [PS(bank)], [("PTt", u % 6)])
                ob = 5 + (h * NCH + c) % 2
                last = kt == 4 * c + 3
                S_.mm(psb[ob][0:65, q0:512], VBh[h % 2][:, kt * 65:(kt + 1) * 65], pt[:, q0:512], kt == 0, last,
                      [("VBh", h % 2), ("PTt", u % 6)], [PS(ob)])
                if last:
                    pp = (h * NCH + c) % 2

                    def normalize(h=h, c=c, pp=pp, ob=ob):
                        S_.act(rinv[pp][64:65, :], psb[ob][64:65, :], AF.Ln, [PS(ob)], [("rinv", pp)])
                        S_.act(rinv[pp][64:65, :], rinv[pp][64:65, :], AF.Exp, [("rinv", pp)], [("rinv", pp)], scale=-1.0)
                        S_.mm(psb[7][0:64, :], onesf[64:65, 0:64], rinv[pp][64:65, :], True, True,
                              ["onesf", ("rinv", pp)], [PS(7)])
                        S_.cp("vector", otf[pp][:], psb[ob][0:64, :], [PS(ob)], [("otf", pp)])
                        S_.tt("vector", obst[pp][:], otf[pp][:], psb[7][0:64, :], ALU.mult,
                              [("otf", pp), PS(7)], [("obst", pp)])
                        S_.dma("gpsimd", ObT[h * 64:(h + 1) * 64, c * 512:(c + 1) * 512], obst[pp][:],
                               [("obst", pp)], [("ObT", c)])
                    pendB.append([3, normalize])

            pendB = []

            def tickB():
                for p_ in [q_ for q_ in pendB if q_[0] <= 0]:
                    pendB.remove(p_)
                    p_[1]()
                for p_ in pendB:
                    p_[0] -= 1

            load_head(0)
            load_head(1)
            n = len(units)
            LOOK = 4
            for u in range(n + LOOK):
                if u < n:
                    st_mm(u)
                if u - LOOK >= 0:
                    tickB()
                    exp_pv(u - LOOK)
                    hh, cc, kk = units[u - LOOK]
                    if kk == 4 * cc + 3 and (hh * NCH + cc) % 2 == 0:
                        next(conv, None)
                    if cc == NCH - 1 and kk == 4 * cc + 3:
                        load_head(hh + 2)
            while pendB:
                tickB()
        S_.barrier(barscr)

        pc = contextlib.ExitStack()
        with pc:
            Wpa = sbt(pc, "Wpa", [128, 4, D], BF16)
            Wpb = sbt(pc, "Wpb", [128, 4, D], BF16)
            Wout = sbt(pc, "Wout", [128, 8, D], BF16)
            Wr = sbt(pc, "Wr", [128, 8, 36], F32)
            brB = sbt(pc, "brB", [128, 36], F32)
            oaC = [sbt(pc, "oaC%d" % i, [128, 4, 512], BF16) for i in range(2)]
            obC = [sbt(pc, "obC%d" % i, [128, 4, 512], BF16) for i in range(2)]
            gtC = [sbt(pc, "gtC%d" % i, [128, 16, 512], BF16) for i in range(2)]
            xc = [sbt(pc, "xc%d" % i, [128, D], F32) for i in range(2)]
            x1t = [sbt(pc, "x1t%d" % i, [128, D], F32) for i in range(2)]
            h2f = [sbt(pc, "h2f%d" % i, [128, D], F32) for i in range(4)]
            h2b = [sbt(pc, "h2b%d" % i, [128, D], BF16) for i in range(2)]
            h2T = [sbt(pc, "h2T%d" % i, [128, 8, 128], F32) for i in range(2)]
            mT = [sbt(pc, "mT%d" % i, [128, 8, 512], BF16) for i in range(2)]
            ga = [sbt(pc, "ga%d" % i, [128, 512], F32) for i in range(2)]
            gb_ = [sbt(pc, "gb%d" % i, [128, 512], F32) for i in range(2)]
            ss2 = sbt(pc, "ss2", [128, NT], F32)
            rs2 = sbt(pc, "rs2", [128, NT], F32)
            junk2 = sbt(pc, "junk2", [128, D], BF16)
            lg = [sbt(pc, "lg%d" % i, [128, 36], F32) for i in range(2)]
            rt = sbt(pc, "rt", [128, NT, 16], F32)
            msk = [sbt(pc, "msk%d" % i, [128, 32], F32) for i in range(2)]
            top8 = [sbt(pc, "top8%d" % i, [128, 8], F32) for i in range(2)]
            gex = [sbt(pc, "gex%d" % i, [128, 4], F32) for i in range(2)]
            Mb = [sbt(pc, "Mb%d" % i, [128, 32], BF16) for i in range(2)]
            lstrict = sbt(pc, "lstrict", [128, 128], BF16)
            rk = [sbt(pc, "rk%d" % i, [128, 32], F32) for i in range(2)]
            rj = [sbt(pc, "rj%d" % i, [128, 32], F32) for i in range(2)]

            A2 = sbt(pc, "A2c", [128, D], F32)
            B2 = sbt(pc, "B2c", [128, D], F32)
            Gm = sbt(pc, "Gmc", [128, D], F32)
            for q, tl in ((0, A2), (1, B2), (2, Gm)):
                S_.dma("sync", tl[:], MODS[q], [("MODS", q)], [(tl.name, 0), (tl.name, 1)])
            S_.dma("gpsimd", Wpa[:], wp_a.rearrange("(j p) n -> p j n", p=128), [], ["Wpa"])
            S_.dma("gpsimd", Wpb[:], wp_b.rearrange("(j p) n -> p j n", p=128), [], ["Wpb"])
            for k in range(8):
                S_.dma("sync", xc[k % 2][:], w_out[k * 128:(k + 1) * 128, :], [], [("xc", k % 2)])
                S_.stt(Wout[:, k, :], xc[k % 2][:], 0.5, Gm[:], ALU.mult, ALU.mult, [("xc", k % 2)] + full(Gm), [("Wout", k)])
            WOUT = [("Wout", k) for k in range(8)]
            S_.dma("sync", Wr[:], wr.rearrange("(k p) n -> p k n", p=128), [], ["Wr"])
            S_.dma("sync", brB[:], bcast_rows(br, 128), [], ["brB"])
            S_.memset("vector", ss2[:], 0.0, ["ss2"])
            S_.memset("vector", rt[:], 0.0, ["rt"])
            S_.memset("gpsimd", lstrict[:], 1.0, ["lstrict"])
            S_.op("gpsimd", lambda e: e.affine_select(out=lstrict[:], in_=lstrict[:], pattern=[[1, 128]],
                                                      compare_op=ALU.is_gt, fill=0.0, base=0, channel_multiplier=-1),
                  ["lstrict"], ["lstrict"])

            def load_chunk(c):
                if c >= NCH:
                    return
                S_.dma("sync", oaC[c % 2][:], OaT[:, c * 512:(c + 1) * 512].rearrange("(j p) n -> p j n", p=128),
                       [("OaT", c)], [("oaC", c % 2)])
                S_.dma("sync", obC[c % 2][:], ObT[:, c * 512:(c + 1) * 512].rearrange("(j p) n -> p j n", p=128),
                       [("ObT", c)], [("obC", c % 2)])
                S_.dma("sync", gtC[c % 2][:], GT[:, c * 512:(c + 1) * 512].rearrange("(m p) n -> p m n", p=128),
                       [("GT", c, q) for q in range(4)], [("gtC", c % 2)])

            def c_merge(c, m):
                pa_, pb_ = (0, 1) if m % 2 == 0 else (2, 3)
                for j in range(4):
                    S_.mm(psb[pa_][:], Wpa[:, j, m * 128:(m + 1) * 128], oaC[c % 2][:, j, :], j == 0, j == 3,
                          ["Wpa", ("oaC", c % 2)], [PS(pa_)])
                for j in range(4):
                    S_.mm(psb[pb_][:], Wpb[:, j, m * 128:(m + 1) * 128], obC[c % 2][:, j, :], j == 0, j == 3,
                          ["Wpb", ("obC", c % 2)], [PS(pb_)])
                S_.stt(ga[m % 2][:], gtC[c % 2][:, m, :], 1.0, psb[pa_][:], ALU.add, ALU.mult,
                       [PS(pa_), ("gtC", c % 2)], [("ga", m % 2)])
                S_.stt(gb_[m % 2][:], gtC[c % 2][:, 8 + m, :], 1.0, psb[pb_][:], ALU.add, ALU.mult,
                       [PS(pb_), ("gtC", c % 2)], [("gb", m % 2)])
                S_.tt("gpsimd", mT[c % 2][:, m, :], ga[m % 2][:], gb_[m % 2][:], ALU.add,
                      [("ga", m % 2), ("gb", m % 2)], [("mT", c % 2, m)])

            def c_W(c, t):
                i = 4 * c + t
                xb = xc[i % 2]
                S_.dma("sync", xb[:], x[i * 128:(i + 1) * 128, :], [], [("xc", i % 2)])
                next(conv, None)
                x1 = x1t[i % 2]
                for hf in range(2):
                    bank = 4 + hf
                    for m in range(8):
                        S_.mm(psb[bank][:], mT[c % 2][:, m, t * 128:(t + 1) * 128], Wout[:, m, hf * 512:(hf + 1) * 512],
                              m == 0, m == 7, [("mT", c % 2, m), ("Wout", m)], [PS(bank)])
                    S_.tt("vector", x1[:, hf * 512:(hf + 1) * 512], psb[bank][:], xb[:, hf * 512:(hf + 1) * 512],
                          ALU.add, [PS(bank), ("xc", i % 2)], [("x1t", i % 2, hf)])
                X1K = [("x1t", i % 2, 0), ("x1t", i % 2, 1)]
                S_.dma("gpsimd", X1[i * 128:(i + 1) * 128, :], x1[:], X1K, [("X1", i)])
                S_.act(junk2[:], x1[:], AF.Square, X1K + ["ss2"], ["junk2", ("ss2", i)], accum=ss2[:, i:i + 1])

            def c_W2(i):
                x1 = x1t[i % 2]
                X1K = [("x1t", i % 2, 0), ("x1t", i % 2, 1)]
                S_.ts("vector", rs2[:, i:i + 1], ss2[:, i:i + 1], 1.0 / D, EPS, ALU.mult, ALU.add, [("ss2", i)], [("rs2", i)])
                S_.act(rs2[:, i:i + 1], rs2[:, i:i + 1], AF.Ln, [("rs2", i)], [("rs2", i)])
                S_.act(rs2[:, i:i + 1], rs2[:, i:i + 1], AF.Exp, [("rs2", i)], [("rs2", i)], scale=-0.5)
                hf_ = h2f[i % 4]
                S_.stt(hf_[:], x1[:], rs2[:, i:i + 1], A2[:], ALU.mult, ALU.mult, X1K + [("rs2", i)] + full(A2), [("h2f", i % 4)])
                S_.tt("gpsimd", hf_[:], hf_[:], B2[:], ALU.add, [("h2f", i % 4)] + full(B2), [("h2f", i % 4)])
                S_.cp("gpsimd", h2b[i % 2][:].rearrange("t (k p) -> t k p", k=8),
                      hf_[:].rearrange("t (p k) -> t k p", k=8), [("h2f", i % 4)], [("h2b", i % 2)])
                S_.dma("gpsimd", H2P[i * 128:(i + 1) * 128, :], h2b[i % 2][:], [("h2b", i % 2)], [("H2P", i)])

            def c_R1(i):
                hf_ = h2f[i % 4]
                for rnd in range(2):
                    for kk in range(4):
                        k = rnd * 4 + kk
                        S_.tr(psb[6][:, kk * 128:(kk + 1) * 128], hf_[:, k * 128:(k + 1) * 128], identf[:],
                              [("h2f", i % 4), "identf"], [PS(6)])
                    S_.cp("scalar" if rnd == 0 else "vector", h2T[i % 2][:, rnd * 4:rnd * 4 + 4, :],
                          psb[6][:].rearrange("p (k n) -> p k n", k=4), [PS(6)], [("h2T", i % 2, rnd)])
                for k in range(8):
                    S_.mm(psb[7][:, 0:36], h2T[i % 2][:, k, :], Wr[:, k, :], k == 0, k == 7,
                          [("h2T", i % 2, k // 4), "Wr"], [PS(7)])
                L = lg[i % 2]
                LK = ("lg", i % 2)
                S_.tt("vector", L[:], psb[7][:, 0:36], brB[:], ALU.add, [PS(7), "brB"], [LK])
                RTK = ("rt", i)
                S_.rmax(rt[:, i, 0:1], L[:, 0:4], [LK, "rt"], [RTK])
                S_.ts("vector", rt[:, i, 1:2], rt[:, i, 0:1], -1.0, None, ALU.mult, None, [RTK], [RTK])
                S_.act(gex[i % 2][:], L[:, 0:4], AF.Exp, [LK, RTK], [("gex", i % 2), RTK], bias=rt[:, i, 1:2], accum=rt[:, i, 2:3])
                S_.recip(rt[:, i, 3:4], rt[:, i, 2:3], [RTK], [RTK])
                S_.ts("vector", gex[i % 2][:], L[:, 0:4], rt[:, i, 0:1], None, ALU.is_equal, None, [LK, RTK, ("gex", i % 2)], [("gex", i % 2)])
                S_.ts("vector", gex[i % 2][:], gex[i % 2][:], -1.0, 1e30, ALU.add, ALU.mult, [("gex", i % 2)], [("gex", i % 2)])
                mk = msk[i % 2]
                S_.tt("vector", mk[:].rearrange("p (g e) -> p g e", g=4), L[:, 4:36].rearrange("p (g e) -> p g e", g=4),
                      gex[i % 2][:].unsqueeze(2).to_broadcast([128, 4, 8]), ALU.add, [LK, ("gex", i % 2)], [("msk", i % 2)])
                S_.max8(top8[i % 2][:], mk[:], [("msk", i % 2)], [("top8", i % 2)])
                S_.ts("vector", M1[:, i, :], mk[:], top8[i % 2][:, 0:1], None, ALU.is_equal, None, [("msk", i % 2), ("top8", i % 2)], [("M1", i)])
                S_.ts("vector", M2[:, i, :], mk[:], top8[i % 2][:, 1:2], None, ALU.is_equal, None, [("msk", i % 2), ("top8", i % 2)], [("M2", i)])
                S_.tt("vector", rt[:, i, 4:5], top8[i % 2][:, 1:2], top8[i % 2][:, 0:1], ALU.subtract, [("top8", i % 2), RTK], [RTK])
                S_.act(rt[:, i, 5:6], rt[:, i, 4:5], AF.Exp, [RTK], [RTK])
                S_.ts("vector", rt[:, i, 6:7], rt[:, i, 5:6], 1.0, None, ALU.add, None, [RTK], [RTK])
                S_.recip(rt[:, i, 6:7], rt[:, i, 6:7], [RTK], [RTK])
                S_.tt("vector", W1[:, i:i + 1], rt[:, i, 6:7], rt[:, i, 3:4], ALU.mult, [RTK], [("W1", i)])
                S_.tt("vector", W2[:, i:i + 1], rt[:, i, 3:4], W1[:, i:i + 1], ALU.subtract, [RTK, ("W1", i)], [("W2", i)])
                mb = Mb[i % 2]
                S_.tt("vector", mb[:], M1[:, i, :], M2[:, i, :], ALU.add, [("M1", i), ("M2", i)], [("Mb", i % 2)])

            def c_R2(i):
                mb = Mb[i % 2]
                S_.mm(psb[7][:, 64:96], lstrict[:], mb[:], True, False, ["lstrict", ("Mb", i % 2)], [PS(7)])
                S_.mm(psb[7][:, 64:96], onesb[:], Macc[:], False, True, ["onesb", "Macc"], [PS(7)])
                S_.cp("vector", rk[i % 2][:], psb[7][:, 64:96], [PS(7)], [("rk", i % 2)])
                S_.tt("gpsimd", Macc[:], Macc[:], mb[:], ALU.add, ["Macc", ("Mb", i % 2)], ["Macc"])
                for (Mx, Rx, nm) in ((M1, R1, "R1"), (M2, R2, "R2")):
                    S_.tt("vector", rj[i % 2][:], Mx[:, i, :], rk[i % 2][:], ALU.mult,
                          [("M1" if nm == "R1" else "M2", i), ("rk", i % 2)], [("rj", i % 2)])
                    S_.rsum(Rx[:, i:i + 1], rj[i % 2][:], [("rj", i % 2)], [(nm, i)])

            pend = []

            def tick():
                due = [p_ for p_ in pend if p_[0] <= 0]
                for p_ in due:
                    pend.remove(p_)
                    p_[1](p_[2])
                for p_ in pend:
                    p_[0] -= 1

            load_chunk(0)
            for c in range(NCH):
                load_chunk(c + 1)
                for m in range(8):
                    c_merge(c, m)
                    tick()
                for t in range(4):
                    i = 4 * c + t
                    c_W(c, t)
                    tick()
                    pend.append([0, c_W2, i])
                    pend.append([2, c_R1, i])
                    pend.append([3, c_R2, i])
            while pend:
                tick()
        S_.barrier(barscr)

        pd = contextlib.ExitStack()
        with pd:
            cnt = sbt(pd, "cnt", [128, 32], F32)
            thr = sbt(pd, "thr", [128, 32], F32)
            cmp_ = sbt(pd, "cmp", [128, 32, 32], F32)
            ntl = sbt(pd, "ntl", [128, 32], F32)
            incl = sbt(pd, "incl", [128, 32], F32)
            base = sbt(pd, "base", [128, 32], F32)
            ones32 = sbt(pd, "ones32", [128, 32], F32)
            tio = sbt(pd, "tio", [128, NMT], F32)
            cmp2 = sbt(pd, "cmp2", [128, NMT, 32], F32)
            etf = sbt(pd, "etf", [128, NMT], F32)
            pio = sbt(pd, "pio", [128, 1], F32)
            widx = sbt(pd, "widx", [128, NMT], I32)
            posf = sbt(pd, "posf", [128, 2, NT], F32)
            posi = sbt(pd, "posi", [128, 2, NT], I32)
            mbig = sbt(pd, "mbig", [128, NT, 32], F32)
            h2g = [sbt(pd, "h2g%d" % i, [128, D], BF16) for i in range(4)]
            hs = [sbt(pd, "hs%d" % i, [128, D], BF16) for i in range(3)]
            hsT = [sbt(pd, "hsT%d" % i, [128, 8, 128], BF16) for i in range(2)]
            Wg_ = [sbt(pd, "Wg%d" % i, [128, 8, 256], BF16) for i in range(3)]
            Wu_ = [sbt(pd, "Wu%d" % i, [128, 8, 256], BF16) for i in range(3)]
            Wd_ = [sbt(pd, "Wd%d" % i, [128, 2, D], BF16) for i in range(3)]
            sa = [sbt(pd, "sa%d" % i, [128, 256], F32) for i in range(2)]
            hid = [sbt(pd, "hid%d" % i, [128, 256], BF16) for i in range(2)]
            hidT = [sbt(pd, "hidT%d" % i, [128, 2, 128], BF16) for i in range(2)]
            yt = [sbt(pd, "yt%d" % i, [128, D], F32) for i in range(2)]
            y1 = [sbt(pd, "y1_%d" % i, [128, D], F32) for i in range(3)]
            y2 = [sbt(pd, "y2_%d" % i, [128, D], F32) for i in range(3)]
            xf = [sbt(pd, "xf%d" % i, [128, D], F32) for i in range(3)]
            ssf = sbt(pd, "ssf", [128, NT], F32)
            rsf = sbt(pd, "rsf", [128, NT], F32)
            junk3 = sbt(pd, "junk3", [128, D], BF16)

            Gf = sbt(pd, "Gfd", [128, D], F32)
            Gfin = sbt(pd, "Gfin", [128, D], F32)
            S_.dma("sync", Gf[:], MODS[3], [("MODS", 3)], [("Gfd", 0), ("Gfd", 1)])
            S_.dma("sync", Gfin[:], bcast_rows(g_fin, 128), [], ["Gfin"])
            for _ in conv:
                pass
            WBK = [("WB", jn) for jn in range(96)]
            allM = [("M1", i) for i in range(NT)] + [("M2", i) for i in range(NT)]
            S_.mm(psb[0][:, 0:32], onesb[:], Macc[:], True, True, ["onesb", "Macc"], [PS(0)])
            S_.cp("vector", cnt[:], psb[0][:, 0:32], [PS(0)], ["cnt"])
            S_.op("gpsimd", lambda e: e.iota(thr[:], pattern=[[128, 32]], base=0, channel_multiplier=0,
                                             allow_small_or_imprecise_dtypes=True), [], ["thr"])
            S_.op("gpsimd", lambda e: e.iota(tio[:], pattern=[[1, NMT]], base=0, channel_multiplier=0,
                                             allow_small_or_imprecise_dtypes=True), [], ["tio"])
            S_.op("gpsimd", lambda e: e.iota(pio[:], pattern=[[0, 1]], base=0, channel_multiplier=1,
                                             allow_small_or_imprecise_dtypes=True), [], ["pio"])
            S_.memset("vector", ones32[:], 1.0, ["ones32"])
            S_.tt("vector", cmp_[:], cnt[:].unsqueeze(2).to_broadcast([128, 32, 32]),
                  thr[:].unsqueeze(1).to_broadcast([128, 32, 32]), ALU.is_gt, ["cnt", "thr"], ["cmp"])
            S_.rsum(ntl[:], cmp_[:], ["cmp"], ["ntl"])
            S_.op("vector", lambda e: e.tensor_tensor_scan(out=incl[:], data0=ones32[:], data1=ntl[:], initial=0.0,
                                                           op0=ALU.mult, op1=ALU.add), ["ones32", "ntl"], ["incl"])
            S_.tt("vector", base[:], incl[:], ntl[:], ALU.subtract, ["incl", "ntl"], ["base"])
            S_.ts("vector", base[:], base[:], 128.0, None, ALU.mult, None, ["base"], ["base"])
            S_.tt("vector", cmp2[:], incl[:].unsqueeze(1).to_broadcast([128, NMT, 32]),
                  tio[:].unsqueeze(2).to_broadcast([128, NMT, 32]), ALU.is_le, ["incl", "tio"], ["cmp2"])
            S_.rsum(etf[:], cmp2[:], ["cmp2"], ["etf"])
            S_.ts("vector", etf[:], etf[:], 128.0, None, ALU.mult, None, ["etf"], ["etf"])
            S_.ts("vector", etf[:], etf[:], pio[:, 0:1], None, ALU.add, None, ["etf", "pio"], ["etf"])
            S_.cp("vector", widx[:], etf[:], ["etf"], ["widx"])
            for q, (Mx, Rx, nm) in enumerate(((M1, R1, "R1"), (M2, R2, "R2"))):
                S_.tt("vector", mbig[:], Mx[:], base[:].unsqueeze(1).to_broadcast([128, NT, 32]), ALU.mult,
                      [("M1" if q == 0 else "M2", i) for i in range(NT)] + ["base"], ["mbig"])
                S_.rsum(posf[:, q, :], mbig[:], ["mbig"], [("posf", q)])
                S_.tt("vector", posf[:, q, :], posf[:, q, :], Rx[:], ALU.add, [("posf", q)] + [(nm, i) for i in range(NT)], [("posf", q)])
                S_.cp("vector", posi[:, q, :], posf[:, q, :], [("posf", q)], [("posi", q)])
            for i in range(NT):
                S_.dma("sync", h2g[i % 4][:], H2P[i * 128:(i + 1) * 128, :], [("H2P", i)], [("h2g", i % 4)])
                for q in range(2):
                    S_.op("gpsimd", (lambda i, q: lambda e: e.indirect_dma_start(
                        out=H2S, out_offset=bass.IndirectOffsetOnAxis(ap=posi[:, q, i:i + 1], axis=0),
                        in_=h2g[i % 4][:], in_offset=None))(i, q),
                        [("h2g", i % 4), ("posi", q)], [("H2S", i, q)], dma=True)
            H2SK = [("H2S", i, q) for i in range(NT) for q in range(2)]
            YSK = [("YS", t) for t in range(NMT)]
            def d_load(t):
                if t >= NMT:
                    return
                S_.dma("sync", hs[t % 3][:], H2S[t * 128:(t + 1) * 128, :], H2SK, [("hs", t % 3)])
                wb3 = t % 3
                for (Wt, src, nm) in ((Wg_, WXB[0], "Wg"), (Wu_, WXB[1], "Wu"), (Wd_, WXB[2], "Wd")):
                    dst = Wt[wb3][:].rearrange("p a f -> p (a f)")
                    S_.op("gpsimd", (lambda dst, src, t: lambda e: e.indirect_dma_start(
                        out=dst, out_offset=None, in_=src,
                        in_offset=bass.IndirectOffsetOnAxis(ap=widx[:, t:t + 1], axis=0),
                        bounds_check=S_.reg(e, 4095), oob_is_err=False))(dst, src, t),
                        ["widx"] + (WBK if t == 0 else []), [(nm, wb3)], dma=True)

            def d_trA(t):
                if t >= NMT:
                    return
                b = t % 2
                for k in range(8):
                    S_.tr(psbf[b][:, k * 128:(k + 1) * 128], hs[t % 3][:, k * 128:(k + 1) * 128], identb[:],
                          [("hs", t % 3), "identb"], [PS(b)])
                S_.cp("vector", hsT[b][:], psbf[b][:].rearrange("p (k n) -> p k n", k=8), [PS(b)], [("hsT", b)])

            def d_au(t):
                b = t % 2
                wb3 = t % 3
                bank = 2 + b
                for k in range(8):
                    S_.mm(psb[bank][:, 0:256], hsT[b][:, k, :], Wg_[wb3][:, k, :], k == 0, k == 7,
                          [("hsT", b), ("Wg", wb3)], [PS(bank)])
                for k in range(8):
                    S_.mm(psb[bank][:, 256:512], hsT[b][:, k, :], Wu_[wb3][:, k, :], k == 0, k == 7,
                          [("hsT", b), ("Wu", wb3)], [PS(bank)])
                S_.act(sa[b][:], psb[bank][:, 0:256], AF.Silu, [PS(bank)], [("sa", b)])
                S_.tt("vector", hid[b][:], sa[b][:], psb[bank][:, 256:512], ALU.mult, [("sa", b), PS(bank)], [("hid", b)])

            def d_down(t):
                if t < 0:
                    return
                b = t % 2
                wb3 = t % 3
                hv = hid[b][:].rearrange("s (p j) -> s j p", j=2)
                for j in range(2):
                    S_.tr(psbf[4][:, j * 128:(j + 1) * 128], hv[:, j, :], identb[:], [("hid", b), "identb"], [PS(4)])
                S_.cp("scalar", hidT[b][:], psbf[4][:, 0:256].rearrange("p (j n) -> p j n", j=2), [PS(4)], [("hidT", b)])
                for hf in range(2):
                    bk = 5 + hf
                    for j in range(2):
                        S_.mm(psb[bk][:], hidT[b][:, j, :], Wd_[wb3][:, j, hf * 512:(hf + 1) * 512], j == 0, j == 1,
                              [("hidT", b), ("Wd", wb3)], [PS(bk)])
                    S_.cp("scalar" if hf == 0 else "vector", yt[b][:, hf * 512:(hf + 1) * 512], psb[bk][:], [PS(bk)], [("yt", b, hf)])
                S_.dma("scalar", YS[t * 128:(t + 1) * 128, :], yt[b][:], [("yt", b, 0), ("yt", b, 1)], [("YS", t)])

            d_load(0)
            d_load(1)
            d_trA(0)
            for t in range(NMT):
                d_trA(t + 1)
                d_au(t)
                d_down(t - 1)
                d_load(t + 2)
            d_down(NMT - 1)
            S_.memset("vector", ssf[:], 0.0, ["ssf"])
            for i in range(NT):
                b = i % 3
                for q, yy in ((0, y1), (1, y2)):
                    S_.op("gpsimd", (lambda yy, q, i: lambda e: e.indirect_dma_start(
                        out=yy[i % 3][:], out_offset=None, in_=YS,
                        in_offset=bass.IndirectOffsetOnAxis(ap=posi[:, q, i:i + 1], axis=0)))(yy, q, i),
                        YSK + [("posi", q)], [("y%d" % q, b)], dma=True)
                S_.dma("sync", xf[b][:], X1[i * 128:(i + 1) * 128, :], [("X1", i)], [("xf", b)])
                S_.ts("vector", y1[b][:], y1[b][:], W1[:, i:i + 1], None, ALU.mult, None, [("y0", b), ("W1", i)], [("y0", b)])
                S_.stt(y1[b][:], y2[b][:], W2[:, i:i + 1], y1[b][:], ALU.mult, ALU.add, [("y1", b), ("y0", b), ("W2", i)], [("y0", b)])
                S_.tt("gpsimd", y1[b][:], y1[b][:], Gf[:], ALU.mult, [("y0", b)] + full(Gf), [("y0", b)])
                S_.tt("vector", xf[b][:], xf[b][:], y1[b][:], ALU.add, [("xf", b), ("y0", b)], [("xf", b)])
                S_.act(junk3[:], xf[b][:], AF.Square, [("xf", b), "ssf"], ["junk3", ("ssf", i)], accum=ssf[:, i:i + 1])
                S_.ts("vector", rsf[:, i:i + 1], ssf[:, i:i + 1], 1.0 / D, EPS, ALU.mult, ALU.add, [("ssf", i)], [("rsf", i)])
                S_.act(rsf[:, i:i + 1], rsf[:, i:i + 1], AF.Ln, [("rsf", i)], [("rsf", i)])
                S_.act(rsf[:, i:i + 1], rsf[:, i:i + 1], AF.Exp, [("rsf", i)], [("rsf", i)], scale=-0.5)
                S_.stt(y2[b][:], xf[b][:], rsf[:, i:i + 1], Gfin[:], ALU.mult, ALU.mult, [("xf", b), ("rsf", i), "Gfin", ("y1", b)], [("y1", b)])
                S_.dma("scalar", out[i * 128:(i + 1) * 128, :], y2[b][:], [("y1", b)], [("out", i)])
        S_.emit()
    return nc


def _t5_bucket_np(u):
    u = np.asarray(u)
    nf = np.maximum(u, 1).astype(np.float32)
    large = 16 + (np.log(nf / np.float32(16)) / np.float32(math.log(128 / 16)) * np.float32(16)).astype(np.int32)
    large = np.minimum(large, 31)
    return np.where(u < 16, u, large)


def make_in_maps(inputs, S, cores):
    f = lambda a: np.ascontiguousarray(np.asarray(a, dtype=np.float32))
    x = f(inputs["x"])
    c = f(inputs["c"])
    buck = _t5_bucket_np(np.arange(128))
    selb = np.zeros((32, 128), np.float32)
    selb[buck, np.arange(128)] = 1.0
    wr = np.concatenate([f(inputs["w_router_group"])[0]] + [f(inputs["w_router_expert"])[0, g] for g in range(4)], axis=1)
    br = np.concatenate([f(inputs["b_router_group"])[0].reshape(1, 4), f(inputs["b_router_expert"])[0].reshape(1, 32)], axis=1)
    shared = {
        "w_ada": f(inputs["w_ada"])[0], "b_ada": f(inputs["b_ada"])[0].reshape(1, -1),
        "g_mix": f(inputs["g_norm_mix"])[0].reshape(1, -1), "g_ffn": f(inputs["g_norm_ffn"])[0].reshape(1, -1),
        "g_fin": f(inputs["g_final"]).reshape(1, -1), "w_in": f(inputs["w_in"])[0],
        "sinks": f(inputs["sinks"])[0].reshape(1, 8), "b_forget": f(inputs["b_forget"])[0].reshape(1, 8),
        "rel_tab": f(inputs["rel_bias_table"]), "selb": selb,
        "wp_a": f(inputs["w_proj_swa"])[0], "wp_b": f(inputs["w_proj_fox"])[0], "w_out": f(inputs["w_out"])[0],
        "wr": f(wr), "br": f(br),
        "wg": f(inputs["w_gate_exp"])[0].reshape(4096, 2048), "wu": f(inputs["w_up_exp"])[0].reshape(4096, 2048),
        "wd": f(inputs["w_down_exp"])[0].reshape(4096, 2048),
    }
    maps = []
    for b in cores:
        m = dict(shared)
        m["x"] = np.ascontiguousarray(x[b, :S])
        m["ccol"] = np.ascontiguousarray(c[b].reshape(8, 128).T)
        maps.append(m)
    return maps


_NC_CACHE = {}


def kernel(**inputs):
    S = 4096
    if S not in _NC_CACHE:
        _NC_CACHE[S] = build_nc(S)
    nc = _NC_CACHE[S]
    in_maps = make_in_maps(inputs, S, list(range(8)))
    res = run_bass_kernel_spmd(nc, in_maps, core_ids=list(range(8)))
    return np.stack([np.asarray(r["out"], dtype=np.float32) for r in res.results], axis=0)
```
